# Optimizing a Trainium2 kernel written in Bass

```python
import math
import jax
import jax.numpy as jnp
from jax import lax
import numpy as np

D_MODEL = 2048
BATCH = 8
SEQ = 2048
DEPTH = 1

HEAD_DIM = 64
N_HEADS_TOTAL = D_MODEL // HEAD_DIM
MIX_WIDTH = N_HEADS_TOTAL * HEAD_DIM
N_NSA_HEADS = N_HEADS_TOTAL // 2
N_FOX_HEADS = N_HEADS_TOTAL - N_NSA_HEADS
NSA_GQA = 4
N_NSA_KV = N_NSA_HEADS // NSA_GQA
CMP_BLOCK = 32
CMP_STRIDE = 16
CMP_HIDDEN = 2 * HEAD_DIM
SLC_BLOCK = 64
SLC_TOP_N = 16
WINDOW = 512
Q_BLOCK = 128
SLC_Q_BLOCK = 64
REL_BUCKETS = 32
REL_MAX_DIST = 128
N_GROUPS = 4
EXPERTS_PER_GROUP = 8
N_EXPERTS = N_GROUPS * EXPERTS_PER_GROUP
EXPERT_TOP_K = 2
EXPERT_FF = D_MODEL // 4
NORM_EPS = 1e-6
NEG_INF = -1e30
FORCE_BONUS = 1e4

NSA_Q_COLS = N_NSA_HEADS * HEAD_DIM
NSA_KV_COLS = N_NSA_KV * HEAD_DIM
NSA_GATE_COLS = N_NSA_HEADS * 3
FOX_COLS = N_FOX_HEADS * HEAD_DIM
IN_SPLIT_SIZES = (NSA_Q_COLS,) + (NSA_KV_COLS,) * 6 + (NSA_GATE_COLS, FOX_COLS, FOX_COLS, FOX_COLS, N_FOX_HEADS)
IN_COLS = sum(IN_SPLIT_SIZES)

kernel_name = 'hybrid_nsa_fox_hmoe'


def rms_norm(x, w):
    xf = x.astype(jnp.float32)
    y = xf * lax.rsqrt(jnp.mean(xf * xf, axis=-1, keepdims=True) + NORM_EPS)
    return (y * w.astype(jnp.float32)).astype(x.dtype)


def t5_bucket(dist):
    n = jnp.maximum(dist, 0)
    max_exact = REL_BUCKETS // 2
    rel = jnp.log(jnp.maximum(n, 1).astype(jnp.float32) / max_exact) / math.log(REL_MAX_DIST / max_exact)
    large = max_exact + (rel * (REL_BUCKETS - max_exact)).astype(jnp.int32)
    large = jnp.minimum(large, REL_BUCKETS - 1)
    return jnp.where(n < max_exact, n, large)


def masked_softmax(s, mask):
    s = jnp.where(mask, s, NEG_INF)
    m = jnp.max(s, axis=-1, keepdims=True)
    e = jnp.where(mask, jnp.exp(s - m), 0.0)
    return e / jnp.maximum(jnp.sum(e, axis=-1, keepdims=True), 1e-30)


def compress_blocks(kv, pe, w1, w2):
    b, t, g, d = kv.shape
    nc = (t - CMP_BLOCK) // CMP_STRIDE + 1
    idx = jnp.arange(nc)[:, None] * CMP_STRIDE + jnp.arange(CMP_BLOCK)[None, :]
    blocks = kv[:, idx] + pe[None, None, :, None, :]
    blocks = jnp.moveaxis(blocks, 3, 2).reshape(b, nc, g, CMP_BLOCK * d)
    return jax.nn.silu(blocks @ w1) @ w2


def nsa_mixer(q, k_cmp, v_cmp, k_slc, v_slc, k_win, v_win, gate_logits,
              pe_k, pe_v, ck_w1, ck_w2, cv_w1, cv_w2, rel_table):
    b, t, _, d = q.shape
    g, r = N_NSA_KV, NSA_GQA
    scale = d ** -0.5
    qg = q.reshape(b, t, g, r, d)
    t_pos = jnp.arange(t)

    kc = compress_blocks(k_cmp, pe_k, ck_w1, ck_w2)
    vc = compress_blocks(v_cmp, pe_v, cv_w1, cv_w2)
    nc = kc.shape[1]
    c_end = jnp.arange(nc) * CMP_STRIDE + CMP_BLOCK - 1
    dist_c = t_pos[:, None] - c_end[None, :]
    bias_c = rel_table[t5_bucket(dist_c)].reshape(t, nc, g, r).transpose(2, 3, 0, 1)
    s_c = jnp.einsum('btgrd,bcgd->bgrtc', qg, kc, preferred_element_type=jnp.float32) * scale + bias_c
    p_cmp = masked_softmax(s_c, dist_c >= 0)
    o_cmp = jnp.einsum('bgrtc,bcgd->btgrd', p_cmp.astype(vc.dtype), vc)

    ns = t // SLC_BLOCK
    ratio = SLC_BLOCK // CMP_STRIDE
    span = CMP_BLOCK // CMP_STRIDE
    offs = (jnp.arange(ratio)[:, None] - jnp.arange(span)[None, :]).reshape(-1)
    cidx = jnp.arange(ns)[:, None] * ratio + offs[None, :]
    cvalid = (cidx >= 0) & (cidx < nc)
    p_grp = jnp.sum(p_cmp, axis=2)
    imp = jnp.sum(jnp.where(cvalid, jnp.take(p_grp, jnp.clip(cidx, 0, nc - 1), axis=-1), 0.0), axis=-1)
    blk = jnp.arange(ns)
    cur = t_pos // SLC_BLOCK
    forced = (blk[None, :] == 0) | (blk[None, :] == cur[:, None]) | (blk[None, :] == cur[:, None] - 1)
    blk_valid = blk[None, :] * SLC_BLOCK <= t_pos[:, None]
    sel_score = jnp.where(blk_valid, imp + jnp.where(forced, FORCE_BONUS, 0.0), NEG_INF)
    n_sel = min(SLC_TOP_N, ns)
    _, sel = lax.top_k(sel_score, n_sel)
    nk = n_sel * SLC_BLOCK

    k_t = jnp.moveaxis(k_slc, 2, 1)
    v_t = jnp.moveaxis(v_slc, 2, 1)
    b_idx = jnp.arange(b)[:, None, None]
    g_idx = jnp.arange(g)[None, :, None]
    table_g = rel_table.reshape(REL_BUCKETS, g, r).transpose(1, 0, 2)

    def slc_chunk(ci):
        t0 = ci * SLC_Q_BLOCK
        qc = lax.dynamic_slice_in_dim(qg, t0, SLC_Q_BLOCK, axis=1)
        selc = lax.dynamic_slice_in_dim(sel, t0, SLC_Q_BLOCK, axis=2)
        pos = (selc[..., None] * SLC_BLOCK + jnp.arange(SLC_BLOCK)).reshape(b, g, SLC_Q_BLOCK * nk)
        kg = k_t[b_idx, g_idx, pos].reshape(b, g, SLC_Q_BLOCK, nk, d)
        vg = v_t[b_idx, g_idx, pos].reshape(b, g, SLC_Q_BLOCK, nk, d)
        dist = (t0 + jnp.arange(SLC_Q_BLOCK))[:, None] - pos.reshape(b, g, SLC_Q_BLOCK, nk)
        bias = table_g[jnp.arange(g)[None, :, None, None], t5_bucket(dist)]
        s = jnp.einsum('bqgrd,bgqkd->bgrqk', qc, kg, preferred_element_type=jnp.float32) * scale
        s = s + jnp.moveaxis(bias, 4, 2)
        p = masked_softmax(s, (dist >= 0)[:, :, None])
        return jnp.einsum('bgrqk,bgqkd->bqgrd', p.astype(vg.dtype), vg)

    o_slc = lax.map(slc_chunk, jnp.arange(t // SLC_Q_BLOCK))
    o_slc = jnp.moveaxis(o_slc, 0, 1).reshape(b, t, g, r, d)

    kp = jnp.pad(k_win, ((0, 0), (WINDOW, 0), (0, 0), (0, 0)))
    vp = jnp.pad(v_win, ((0, 0), (WINDOW, 0), (0, 0), (0, 0)))
    span_w = WINDOW + Q_BLOCK
    j_loc = jnp.arange(span_w)
    dist_w = jnp.arange(Q_BLOCK)[:, None] + WINDOW - j_loc[None, :]
    band = (dist_w >= 0) & (dist_w < WINDOW)
    bias_w = rel_table[t5_bucket(dist_w)].reshape(Q_BLOCK, span_w, g, r).transpose(2, 3, 0, 1)

    def win_block(bi):
        t0 = bi * Q_BLOCK
        qb = lax.dynamic_slice_in_dim(qg, t0, Q_BLOCK, axis=1)
        kb = lax.dynamic_slice_in_dim(kp, t0, span_w, axis=1)
        vb = lax.dynamic_slice_in_dim(vp, t0, span_w, axis=1)
        mask = band & (t0 - WINDOW + j_loc >= 0)[None, :]
        s = jnp.einsum('bqgrd,bkgd->bgrqk', qb, kb, preferred_element_type=jnp.float32) * scale + bias_w
        p = masked_softmax(s, mask)
        return jnp.einsum('bgrqk,bkgd->bqgrd', p.astype(vb.dtype), vb)

    o_win = lax.map(win_block, jnp.arange(t // Q_BLOCK))
    o_win = jnp.moveaxis(o_win, 0, 1).reshape(b, t, g, r, d)

    gates = jax.nn.sigmoid(gate_logits.astype(jnp.float32)).reshape(b, t, g, r, 3).astype(q.dtype)
    o = gates[..., 0:1] * o_cmp + gates[..., 1:2] * o_slc + gates[..., 2:3] * o_win
    return o.reshape(b, t, g * r * d)


def fox_mixer(q, k, v, f_logit, f_bias):
    b, t, h, d = q.shape
    scale = d ** -0.5
    log_f = jax.nn.log_sigmoid(f_logit.astype(jnp.float32) + f_bias.astype(jnp.float32))
    c = jnp.moveaxis(jnp.cumsum(log_f, axis=1), 1, 2)
    outs = []
    for bi in range(t // Q_BLOCK):
        t0, t1 = bi * Q_BLOCK, (bi + 1) * Q_BLOCK
        s = jnp.einsum('bqhd,bkhd->bhqk', q[:, t0:t1], k[:, :t1], preferred_element_type=jnp.float32) * scale
        s = s + c[:, :, t0:t1, None] - c[:, :, None, :t1]
        mask = jnp.arange(t0, t1)[:, None] >= jnp.arange(t1)[None, :]
        p = masked_softmax(s, mask)
        outs.append(jnp.einsum('bhqk,bkhd->bqhd', p.astype(v.dtype), v[:, :t1]))
    return jnp.concatenate(outs, axis=1).reshape(b, t, h * d)


def hier_moe(xn, wg, bg, we, be, w_gate, w_up, w_down):
    b, t, dm = xn.shape
    xf = xn.reshape(b * t, dm)
    glog = (xf @ wg + bg).astype(jnp.float32)
    pg = jax.nn.softmax(glog, axis=-1)
    gsel = jnp.argmax(glog, axis=-1)
    p_gsel = jnp.take_along_axis(pg, gsel[:, None], axis=-1)
    elog = (xf @ we + be).astype(jnp.float32).reshape(b * t, N_GROUPS, EXPERTS_PER_GROUP)
    elog_g = jnp.take_along_axis(elog, gsel[:, None, None], axis=1)[:, 0]
    top_v, top_i = lax.top_k(elog_g, EXPERT_TOP_K)
    w_top = jax.nn.softmax(top_v, axis=-1) * p_gsel
    eid = gsel[:, None] * EXPERTS_PER_GROUP + top_i
    combine = jnp.sum(jax.nn.one_hot(eid, N_EXPERTS, dtype=jnp.float32) * w_top[..., None], axis=1)
    combine = combine.astype(xf.dtype)
    y = jnp.zeros_like(xf)
    for e in range(N_EXPERTS):
        he = jax.nn.silu(xf @ w_gate[e]) * (xf @ w_up[e])
        y = y + combine[:, e:e + 1] * (he @ w_down[e])
    return y.reshape(b, t, dm)


def setup_inputs(seed: int = 0) -> dict:
    key = jax.random.key(seed)
    ks = jax.random.split(key, 24)

    def nrm(k, shape, scale):
        return jax.random.normal(k, shape, jnp.float32) * scale

    L = DEPTH
    return {
        'x': nrm(ks[0], (BATCH, SEQ, D_MODEL), 1.0),
        'attn_norm_w': 1.0 + nrm(ks[1], (L, D_MODEL), 0.01),
        'w_in': nrm(ks[2], (L, D_MODEL, IN_COLS), D_MODEL ** -0.5),
        'cmp_pe_k': nrm(ks[3], (L, CMP_BLOCK, HEAD_DIM), 0.1),
        'cmp_pe_v': nrm(ks[4], (L, CMP_BLOCK, HEAD_DIM), 0.1),
        'cmp_k_w1': nrm(ks[5], (L, CMP_BLOCK * HEAD_DIM, CMP_HIDDEN), (CMP_BLOCK * HEAD_DIM) ** -0.5),
        'cmp_k_w2': nrm(ks[6], (L, CMP_HIDDEN, HEAD_DIM), CMP_HIDDEN ** -0.5),
        'cmp_v_w1': nrm(ks[7], (L, CMP_BLOCK * HEAD_DIM, CMP_HIDDEN), (CMP_BLOCK * HEAD_DIM) ** -0.5),
        'cmp_v_w2': nrm(ks[8], (L, CMP_HIDDEN, HEAD_DIM), CMP_HIDDEN ** -0.5),
        'rel_bias_table': nrm(ks[9], (REL_BUCKETS, N_NSA_HEADS), 0.5),
        'fox_forget_b': 2.0 + nrm(ks[10], (L, N_FOX_HEADS), 0.5),
        'nsa_out_norm_w': 1.0 + nrm(ks[11], (L, N_NSA_HEADS * HEAD_DIM), 0.01),
        'fox_out_norm_w': 1.0 + nrm(ks[12], (L, N_FOX_HEADS * HEAD_DIM), 0.01),
        'w_out': nrm(ks[13], (L, MIX_WIDTH, D_MODEL), MIX_WIDTH ** -0.5),
        'ffn_norm_w': 1.0 + nrm(ks[14], (L, D_MODEL), 0.01),
        'router_group_w': nrm(ks[15], (L, D_MODEL, N_GROUPS), D_MODEL ** -0.5),
        'router_group_b': nrm(ks[16], (L, N_GROUPS), 0.01),
        'router_expert_w': nrm(ks[17], (L, D_MODEL, N_EXPERTS), D_MODEL ** -0.5),
        'router_expert_b': nrm(ks[18], (L, N_EXPERTS), 0.01),
        'expert_w_gate': nrm(ks[19], (L, N_EXPERTS, D_MODEL, EXPERT_FF), D_MODEL ** -0.5),
        'expert_w_up': nrm(ks[20], (L, N_EXPERTS, D_MODEL, EXPERT_FF), D_MODEL ** -0.5),
        'expert_w_down': nrm(ks[21], (L, N_EXPERTS, EXPERT_FF, D_MODEL), EXPERT_FF ** -0.5),
        'final_norm_w': 1.0 + nrm(ks[22], (D_MODEL,), 0.01),
    }


def reference(x, attn_norm_w, w_in, cmp_pe_k, cmp_pe_v, cmp_k_w1, cmp_k_w2, cmp_v_w1, cmp_v_w2,
              rel_bias_table, fox_forget_b, nsa_out_norm_w, fox_out_norm_w, w_out, ffn_norm_w,
              router_group_w, router_group_b, router_expert_w, router_expert_b,
              expert_w_gate, expert_w_up, expert_w_down, final_norm_w):
    b, t, _ = x.shape
    split_at = [int(v) for v in np.cumsum(IN_SPLIT_SIZES)[:-1]]
    h = x
    for layer in range(DEPTH):
        xn = rms_norm(h, attn_norm_w[layer])
        proj = xn @ w_in[layer]
        (nq, kcmp, vcmp, kslc, vslc, kwin, vwin, ngate, fq, fk, fv, ff) = jnp.split(proj, split_at, axis=-1)
        o_nsa = nsa_mixer(
            nq.reshape(b, t, N_NSA_HEADS, HEAD_DIM),
            kcmp.reshape(b, t, N_NSA_KV, HEAD_DIM), vcmp.reshape(b, t, N_NSA_KV, HEAD_DIM),
            kslc.reshape(b, t, N_NSA_KV, HEAD_DIM), vslc.reshape(b, t, N_NSA_KV, HEAD_DIM),
            kwin.reshape(b, t, N_NSA_KV, HEAD_DIM), vwin.reshape(b, t, N_NSA_KV, HEAD_DIM),
            ngate, cmp_pe_k[layer], cmp_pe_v[layer], cmp_k_w1[layer], cmp_k_w2[layer],
            cmp_v_w1[layer], cmp_v_w2[layer], rel_bias_table)
        o_fox = fox_mixer(
            fq.reshape(b, t, N_FOX_HEADS, HEAD_DIM), fk.reshape(b, t, N_FOX_HEADS, HEAD_DIM),
            fv.reshape(b, t, N_FOX_HEADS, HEAD_DIM), ff, fox_forget_b[layer])
        mixed = jnp.concatenate([rms_norm(o_nsa, nsa_out_norm_w[layer]),
                                 rms_norm(o_fox, fox_out_norm_w[layer])], axis=-1)
        h = h + mixed @ w_out[layer]
        hn = rms_norm(h, ffn_norm_w[layer])
        h = h + hier_moe(hn, router_group_w[layer], router_group_b[layer], router_expert_w[layer],
                         router_expert_b[layer], expert_w_gate[layer], expert_w_up[layer], expert_w_down[layer])
    return rms_norm(h, final_norm_w)
```

```python
import math
import os
from contextlib import ExitStack

import ml_dtypes
import numpy as np

import concourse.bass as bass
import concourse.mybir as mybir
from concourse.bass_utils import run_bass_kernel_spmd

F32 = mybir.dt.float32
BF16 = mybir.dt.bfloat16
I32 = mybir.dt.int32
AF = mybir.ActivationFunctionType
ALU = mybir.AluOpType
AX = mybir.AxisListType

T = 2048
D = 2048
NT = 16
HD = 64
NEG = -30000.0
IN_COLS = 5696
C_NQ, C_KCMP, C_VCMP, C_KSLC, C_VSLC, C_KWIN, C_VWIN, C_GATE, C_FQ, C_FK, C_FV, C_FF = (
    0, 1024, 1280, 1536, 1792, 2048, 2304, 2560, 2608, 3632, 4656, 5680)
NSLOT = 64
VOFF = 2112
VLEN = 4608


class Eng:
    def __init__(self, name, e, sem, strict_self=True):
        self.name = name; self.e = e; self.sem = sem; self.cnt = 0
        self.waited = {}; self.strict_self = strict_self


class Buf:
    def __init__(self, t, name=""):
        self.t = t; self.name = name; self.w = {}; self.r = {}

    def __getitem__(self, idx):
        return self.t[idx]


class K:
    def __init__(self, nc, es, n_dma_sems=40):
        self.nc = nc; self.es = es

        def mk(name, e, strict=True):
            return Eng(name, e, es.enter_context(nc.semaphore("sem_" + name)), strict)
        self.pe = mk("pe", nc.tensor, strict=False)
        relax = os.environ.get("K_RELAX", "") .split(",")
        self.act = mk("act", nc.scalar, strict="act" not in relax)
        self.dve = mk("dve", nc.vector, strict="dve" not in relax)
        self.pool = mk("pool", nc.gpsimd, strict="pool" not in relax)
        self.sp = mk("sp", nc.sync)
        self.dsems = [[es.enter_context(nc.semaphore(f"dsem{i}")), 0] for i in range(n_dma_sems)]
        self.dnext = 0
        self.nwaits = 0; self.nops = 0; self.ndma = 0

    def sb(self, name, shape, dt, es=None):
        return Buf((es or self.es).enter_context(self.nc.sbuf_tensor(name, list(shape), dt)), name)

    def ps(self, name, shape, dt):
        return Buf(self.es.enter_context(self.nc.psum_tensor(name, list(shape), dt)), name)

    def dram(self, name, shape, dt, kind="Internal"):
        return Buf(self.nc.dram_tensor(name, list(shape), dt, kind=kind), name)

    def view(self, buf, name=""):
        return Buf(buf.t, name or buf.name)

    def _wait(self, E, need, keep_last=False):
        pend = []
        for key, (sem, val) in need.items():
            if sem is E.sem and not E.strict_self:
                continue
            if E.waited.get(key, 0) >= val:
                continue
            pend.append((key, sem, val))
        last = None
        if keep_last and pend:
            last = pend.pop()
        for key, sem, val in pend:
            E.e.wait_ge(sem, val); E.waited[key] = val; self.nwaits += 1
        if last is not None:
            E.waited[last[0]] = last[2]
        return last

    @staticmethod
    def _merge(need, d):
        for kk, (s, v) in d.items():
            if kk not in need or need[kk][1] < v:
                need[kk] = (s, v)

    def _deps(self, reads, writes):
        need = {}
        for b in reads:
            self._merge(need, b.w)
        for b in writes:
            self._merge(need, b.w); self._merge(need, b.r)
        return need

    def op(self, E, fn, reads=(), writes=()):
        last = self._wait(E, self._deps(reads, writes), keep_last=True)
        ins = fn()
        if last is not None:
            ins._wait_ge(last[1], last[2])
        E.cnt += 1; self.nops += 1
        ins.then_inc(E.sem, 1)
        tok = (E.sem, E.cnt); key = id(E.sem)
        for b in reads:
            b.r[key] = tok
        for b in writes:
            b.w = {key: tok}; b.r = {}
        return ins

    def dma(self, E, out_ap, in_ap, reads=(), writes=(), fn=None, sems=None, **kw):
        need = self._deps(reads, writes)
        if sems is not None:
            ds = sems[0][sems[1][0] % len(sems[0])]; sems[1][0] += 1
        else:
            ds = self.dsems[self.dnext]; self.dnext = (self.dnext + 1) % len(self.dsems)
        if ds[1] > 0:
            self._merge(need, {id(ds[0]): (ds[0], ds[1])})
        self._wait(E, need)
        if fn is None:
            ins = E.e.dma_start(out=out_ap, in_=in_ap, **kw)
        else:
            ins = fn()
        ds[1] += 16; self.ndma += 1
        ins.then_inc(ds[0], 16)
        tok = (ds[0], ds[1]); key = id(ds[0])
        for b in reads:
            b.r[key] = tok
        for b in writes:
            b.w = {key: tok}; b.r = {}
        return ins

    def barrier(self):
        engs = [self.pe, self.act, self.dve, self.pool, self.sp]
        for E in engs:
            need = {}
            for F in engs:
                if F is not E and F.cnt > 0:
                    need[id(F.sem)] = (F.sem, F.cnt)
            for ds in self.dsems:
                if ds[1] > 0:
                    need[id(ds[0])] = (ds[0], ds[1])
            self._wait(E, need)

    def finish(self, bufs):
        need = {}
        for b in bufs:
            self._merge(need, b.w)
        self._wait(self.sp, need)


class Scope:
    def __init__(self, k):
        self.k = k; self.es = ExitStack()

    def __enter__(self):
        self.es.__enter__()
        return self.es

    def __exit__(self, *a):
        if a[0] is None:
            self.k.barrier()
        return self.es.__exit__(*a)


def _t5_bucket(n):
    n = np.maximum(n, 0)
    rel = np.log(np.maximum(n, 1).astype(np.float32) / np.float32(16)) / np.float32(math.log(128 / 16))
    large = 16 + (rel * np.float32(16)).astype(np.int32)
    large = np.minimum(large, 31)
    return np.where(n < 16, n, large)


def _consts():
    bf = ml_dtypes.bfloat16
    c = {}
    c["ident_bf"] = np.eye(128, dtype=np.float32).astype(bf)
    c["ident_f"] = np.eye(128, dtype=np.float32)
    i = np.arange(128)[:, None]; j = np.arange(128)[None, :]
    c["caus"] = np.where(i <= j, 0.0, NEG).astype(bf)
    c["w4m"] = np.where(i > j, 0.0, NEG).astype(bf)
    c["stri"] = (i < j).astype(np.float32).astype(bf)
    up = np.zeros((128, 4, 512), np.float32)
    for jo in range(4):
        for to in range(4):
            if jo < to:
                up[:, jo, to * 128:(to + 1) * 128] = 1.0
            elif jo == to:
                up[:, jo, to * 128:(to + 1) * 128] = (i <= j)
    c["upat"] = up
    m = np.arange(VLEN) - VOFF
    oh = np.zeros((33, VLEN), np.float32)
    bk = _t5_bucket(m)
    for idx in range(VLEN):
        if m[idx] >= 0:
            oh[bk[idx], idx] = 1.0
        else:
            oh[32, idx] = 1.0
    c["ohv"] = oh
    sel31 = np.zeros((32, 32), np.float32); sel31[31, :] = 1.0
    c["sel31"] = sel31
    am = np.zeros((128, 32), np.float32)
    for jj in range(32):
        for a in range(4):
            for b in range(2):
                cc = jj * 4 + a - b
                if 0 <= cc < 127:
                    am[cc, jj] += 1.0
    c["amat"] = am.astype(bf)
    em = np.zeros((32, 2048), np.float32)
    for jj in range(32):
        em[jj, jj * 64:(jj + 1) * 64] = 1.0
    c["emat"] = em.astype(bf)
    t = np.arange(1024, 2048)
    blk = np.arange(32)[None, :]
    cur = (t // 64)[:, None]
    valid = (blk * 64 <= t[:, None])
    forced = (blk == 0) | (blk == cur) | (blk == cur - 1)
    add = np.where(valid, np.where(forced, 1e4, 0.0), -1e30).astype(np.float32)
    c["selvalid"] = valid.astype(np.float32).reshape(8, 128, 32).transpose(1, 0, 2).copy()
    c["seladd"] = add.reshape(8, 128, 32).transpose(1, 0, 2).copy()
    c["ones_d"] = np.ones((3, 8, 2048), np.float32).astype(bf)
    c["iota_p"] = np.arange(128, dtype=np.float32).reshape(128, 1)
    return c


CONST_SPECS = [("ident_bf", [128, 128], BF16), ("ident_f", [128, 128], F32), ("caus", [128, 128], BF16),
               ("w4m", [128, 128], BF16), ("stri", [128, 128], BF16), ("upat", [128, 4, 512], F32),
               ("ohv", [33, VLEN], F32), ("sel31", [32, 32], F32), ("amat", [128, 32], BF16),
               ("emat", [32, 2048], BF16), ("selvalid", [128, 8, 32], F32), ("seladd", [128, 8, 32], F32),
               ("ones_d", [3, 8, 2048], BF16), ("iota_p", [128, 1], F32)]

PARAM_SPECS = [("attn_norm_w", [1, 2048]), ("w_in", [1, 2048, IN_COLS]), ("cmp_pe_k", [1, 32, 64]),
               ("cmp_pe_v", [1, 32, 64]), ("cmp_k_w1", [1, 2048, 128]), ("cmp_k_w2", [1, 128, 64]),
               ("cmp_v_w1", [1, 2048, 128]), ("cmp_v_w2", [1, 128, 64]), ("rel_bias_table", [32, 16]),
               ("fox_forget_b", [1, 16]), ("nsa_out_norm_w", [1, 1024]), ("fox_out_norm_w", [1, 1024]),
               ("w_out", [1, 2048, 2048]), ("ffn_norm_w", [1, 2048]), ("router_group_w", [1, 2048, 4]),
               ("router_group_b", [1, 4]), ("router_expert_w", [1, 2048, 32]), ("router_expert_b", [1, 32]),
               ("expert_w_gate", [32, 2048, 512]), ("expert_w_up", [32, 2048, 512]),
               ("expert_w_down", [32, 512, 2048]), ("final_norm_w", [2048])]


def build_nc(stop_after=None, debug=False):
    nc = bass.Bass("TRN2", target_bir_lowering=False)
    P = {}
    x_d = Buf(nc.dram_tensor("x", [T, D], F32, kind="ExternalInput"), "x")
    for name, shape in PARAM_SPECS:
        P[name] = Buf(nc.dram_tensor(name, shape, F32, kind="ExternalInput"), name)
    CD = {}
    for name, shape, dt in CONST_SPECS:
        CD[name] = Buf(nc.dram_tensor("c_" + name, shape, dt, kind="ExternalInput"), name)
    out_d = Buf(nc.dram_tensor("out", [T, D], F32, kind="ExternalOutput"), "out")
    dbg = {}
    V, S, G, TE = nc.vector, nc.scalar, nc.gpsimd, nc.tensor

    with ExitStack() as es:
        k = K(nc, es)
        pe, act, dve, pool, sp = k.pe, k.act, k.dve, k.pool, k.sp

        o_scr = k.dram("o_scr", [T, 2048], BF16)
        vscr = k.dram("vscr", [16, VLEN], BF16)
        brd = k.dram("brd", [16 * 128, VLEN], BF16)
        cscr = k.dram("cscr", [16, 6, T], BF16)
        h1_scr = k.dram("h1_scr", [T, D], F32)
        hn_scr = k.dram("hn_scr", [T, D], BF16)
        xslot = k.dram("xslot", [NSLOT * 128, D], BF16)
        yslot = k.dram("yslot", [NSLOT * 128, D], F32)
        if debug:
            for nm, shp, dt in [("d_o", [T, 2048], BF16), ("d_h1", [T, D], F32)]:
                dbg[nm] = Buf(nc.dram_tensor(nm, shp, dt, kind="ExternalOutput"), nm)

        wbf = {"g": k.dram("wbf_g", [32, 2048, 512], BF16), "u": k.dram("wbf_u", [32, 2048, 512], BF16),
               "d": k.dram("wbf_d", [32, 512, 2048], BF16)}
        pre_sems = ([[es.enter_context(nc.semaphore(f"presem{i}")), 0] for i in range(6)], [0])
        pre_list = []
        pre_views = []
        for e_ in range(32):
            for key_, pn_ in [("g", "expert_w_gate"), ("u", "expert_w_up"), ("d", "expert_w_down")]:
                vw = k.view(wbf[key_], f"wbf_{key_}{e_}")
                pre_views.append(vw)
                pre_list.append((vw, wbf[key_].t[e_], P[pn_], P[pn_].t[e_]))
        pre_pos = [0]

        def precast_step(n):
            for _ in range(n):
                if pre_pos[0] >= len(pre_list):
                    return
                vw, dst_ap, sb_, src_ap = pre_list[pre_pos[0]]; pre_pos[0] += 1
                k.dma(pool, dst_ap, src_ap, reads=[sb_], writes=[vw], sems=pre_sems)

        o_views = []

        def o_store(dst_ap, src_b, src_ap):
            v_ = k.view(o_scr, "o_st")
            k.dma(sp, dst_ap, src_ap, reads=[src_b], writes=[v_])
            o_views.append(v_)

        def done(bufs):
            k.finish(bufs)
            print("ops", k.nops, "waits", k.nwaits, "dmas", k.ndma, flush=True)
            return nc

        PB = [k.ps(f"pb{i}", [128, 512], F32) for i in range(7)]
        PT = k.ps("pt_bf", [128, 1024], BF16)

        ident_bf = k.sb("ident_bf", [128, 128], BF16)
        ident_f = k.sb("ident_f", [128, 128], F32)
        caus = k.sb("caus", [128, 128], BF16)
        for b_, nm in [(ident_bf, "ident_bf"), (ident_f, "ident_f"), (caus, "caus")]:
            k.dma(sp, b_[:], CD[nm][:], reads=[CD[nm]], writes=[b_])
        rstd_tmp = k.sb("rstd_tmp", [128, 4], F32)
        ssn = k.sb("ssn", [128, NT], F32)
        ssf = k.sb("ssf", [128, NT], F32)
        sstmp = k.sb("sstmp", [128, 2], F32)
        eps_t = k.sb("eps_t", [128, 1], F32)
        junk = k.sb("junk", [128, 2048], BF16)
        k.op(dve, lambda: V.memset(ssn[:], 0.0), writes=[ssn])
        k.op(dve, lambda: V.memset(ssf[:], 0.0), writes=[ssf])
        k.op(dve, lambda: V.memset(eps_t[:], 1e-6), writes=[eps_t])

        def evac(E, out_ap, in_ap, reads, writes, scale=None):
            if E is act:
                if scale is None:
                    return k.op(act, lambda: S.copy(out=out_ap, in_=in_ap), reads=reads, writes=writes)
                return k.op(act, lambda: S.activation(out=out_ap, in_=in_ap, func=AF.Copy, scale=scale), reads=reads, writes=writes)
            if scale is None:
                return k.op(dve, lambda: V.tensor_copy(out=out_ap, in_=in_ap), reads=reads, writes=writes)
            return k.op(dve, lambda: V.tensor_scalar(out=out_ap, in0=in_ap, scalar1=scale, scalar2=None, op0=ALU.mult),
                        reads=reads, writes=writes)

        def mm(out_b, out_ap, l_b, l_ap, r_b, r_ap, start, stop, extra_reads=()):
            return k.op(pe, lambda: TE.matmul(out_ap, l_ap, r_ap, start=start, stop=stop),
                        reads=[l_b, r_b] + list(extra_reads), writes=[out_b])

        def transpose(out_b, out_ap, in_b, in_ap, id_b, kp=128):
            return k.op(pe, lambda: TE.transpose(out_ap, in_ap, id_b[0:kp, 0:kp]), reads=[in_b, id_b], writes=[out_b])

        def rstd_from_ss(ss_ap, ss_b, out_ap, out_b, n):
            k.op(act, lambda: S.activation(out=rstd_tmp[:, 0:1], in_=ss_ap, func=AF.Ln, scale=1.0 / n, bias=eps_t[:, 0:1]),
                 reads=[ss_b, eps_t], writes=[rstd_tmp])
            k.op(act, lambda: S.activation(out=out_ap, in_=rstd_tmp[:, 0:1], func=AF.Exp, scale=-0.5),
                 reads=[rstd_tmp], writes=[out_b])

        def tt(out_b, out_ap, a_b, a_ap, b_b, b_ap, op, E=None):
            return k.op(E or dve, lambda: (E or dve).e.tensor_tensor(out=out_ap, in0=a_ap, in1=b_ap, op=op), reads=[a_b, b_b], writes=[out_b])

        def ts(out_b, out_ap, a_b, a_ap, s1, op0, s2=None, op1=None, sreads=()):
            if op1 is None:
                return k.op(dve, lambda: V.tensor_scalar(out=out_ap, in0=a_ap, scalar1=s1, scalar2=None, op0=op0),
                            reads=[a_b] + list(sreads), writes=[out_b])
            return k.op(dve, lambda: V.tensor_scalar(out=out_ap, in0=a_ap, scalar1=s1, scalar2=s2, op0=op0, op1=op1),
                        reads=[a_b] + list(sreads), writes=[out_b])

        def stt(out_b, out_ap, a_b, a_ap, sc, b_b, b_ap, op0, op1, sreads=()):
            return k.op(dve, lambda: V.scalar_tensor_tensor(out=out_ap, in0=a_ap, scalar=sc, in1=b_ap, op0=op0, op1=op1),
                        reads=[a_b, b_b] + list(sreads), writes=[out_b])

        with Scope(k) as esA:
            xnT = k.sb("xnT", [128, 16, T], BF16, es=esA)
            with Scope(k) as e1:
                anw_bc = k.sb("anw_bc", [128, D], F32, es=e1)
                k.dma(sp, anw_bc[:], bass.AP(P["attn_norm_w"].t, 0, [[0, 128], [1, D]]), reads=[P["attn_norm_w"]], writes=[anw_bc])
                xb = [k.sb(f"xb{i}", [128, D], F32, es=e1) for i in range(2)]
                xn_tm = [k.sb(f"xn_tm{i}", [128, D], BF16, es=e1) for i in range(2)]
                ssx = k.sb("ssx", [128, NT], F32, es=e1)
                rsx = k.sb("rsx", [128, NT], F32, es=e1)
                for i in range(NT):
                    xs = xb[i % 2]; xt = xn_tm[i % 2]
                    k.dma(sp, xs[:], x_d[i * 128:(i + 1) * 128, :], reads=[x_d], writes=[xs])
                    k.op(act, lambda: S.activation(out=junk[:], in_=xs[:], func=AF.Square, accum_out=ssx[:, i:i + 1]),
                         reads=[xs], writes=[junk, ssx])
                    rstd_from_ss(ssx[:, i:i + 1], ssx, rsx[:, i:i + 1], rsx, D)
                    stt(xt, xt[:], xs, xs[:], rsx[:, i:i + 1], anw_bc, anw_bc[:], ALU.mult, ALU.mult, sreads=[rsx])
                    for c4 in range(4):
                        for cc in range(4):
                            c = c4 * 4 + cc
                            transpose(PT, PT[:, cc * 128:(cc + 1) * 128], xt, xt[:, c * 128:(c + 1) * 128], ident_bf)
                        evac(act if c4 % 2 else dve, xnT[:, c4 * 4:(c4 + 1) * 4, i * 128:(i + 1) * 128],
                             PT[:, 0:512].rearrange("p (c t) -> p c t", c=4), [PT], [xnT])

            if stop_after == "xn":
                k.dma(sp, dbg["d_o"].t[:].rearrange("(p c) t -> p c t", c=16), xnT[:], reads=[xnT], writes=[dbg["d_o"]])
                return done([dbg["d_o"]])
            with Scope(k) as e2:
                tab = k.sb("tab", [33, 16], F32, es=e2)
                sel31 = k.sb("sel31", [32, 32], F32, es=e2)
                ohv = k.sb("ohv", [33, VLEN], F32, es=e2)
                vsb = k.sb("vsb", [16, VLEN], BF16, es=e2)
                k.dma(sp, tab[0:32, :], P["rel_bias_table"][:], reads=[P["rel_bias_table"]], writes=[tab])
                k.dma(sp, sel31[:], CD["sel31"][:], reads=[CD["sel31"]], writes=[sel31])
                k.dma(sp, ohv[:], CD["ohv"][:], reads=[CD["ohv"]], writes=[ohv])
                mm(PB[0], PB[0][0:32, 0:16], sel31, sel31[:], tab, tab[0:32, :], True, True)
                tt(tab, tab[0:32, :], tab, tab[0:32, :], PB[0], PB[0][0:32, 0:16], ALU.subtract)
                k.op(dve, lambda: V.memset(tab[32:33, :], NEG), writes=[tab])
                for q in range(VLEN // 512):
                    pb = PB[q % 2]
                    mm(pb, pb[0:16, :], tab, tab[:], ohv, ohv[:, q * 512:(q + 1) * 512], True, True)
                    evac(dve, vsb[:, q * 512:(q + 1) * 512], pb[0:16, :], [pb], [vsb])
                k.dma(sp, vscr[:], vsb[:], reads=[vsb], writes=[vscr])
                for h in range(16):
                    k.dma(sp, brd[h * 128:(h + 1) * 128, :], bass.AP(vscr.t, h * VLEN, [[0, 128], [1, VLEN]]), reads=[vscr], writes=[brd])

            wst_box = [None]

            def cast(E, out_b, out_ap, in_b, in_ap):
                return k.op(E, lambda: E.e.tensor_copy(out=out_ap, in_=in_ap), reads=[in_b], writes=[out_b])

            def load_w(buf, c0, col0, ncols):
                wst = wst_box[0]
                stv = wst[:, 0:16 * ncols].rearrange("p (c n) -> p c n", c=16)
                src = P["w_in"].t[0, :, col0:col0 + ncols].rearrange("(c p) n -> p c n", p=128)
                k.dma(sp, stv, src, reads=[P["w_in"]], writes=[wst])
                cast(pool, buf, buf[:, :, c0:c0 + ncols], wst, stv)

            rot = [0]

            def proj_fm(wbuf, ncols, dst_b, dst_fn, scale):
                for tc in range(4):
                    pb = PB[rot[0] % 3]; rot[0] += 1
                    for c in range(16):
                        mm(pb, pb[0:ncols, :], wbuf, wbuf[:, c, 0:ncols], xnT, xnT[:, c, tc * 512:(tc + 1) * 512], c == 0, c == 15)
                    evac(act if rot[0] % 2 else dve, dst_fn(tc), pb[0:ncols, :], [pb], [dst_b], scale)

            with Scope(k) as eC:
                wst_box[0] = k.sb("wstC", [128, 16 * 16], F32, es=eC)
                wf = k.sb("wf", [128, 16, 16], BF16, es=eC)
                fb_bc = k.sb("fb_bc", [128, 16], F32, es=eC)
                logf = k.sb("logf", [128, NT, 16], F32, es=eC)
                upat = k.sb("upat", [128, 4, 512], F32, es=eC)
                onesf = k.sb("onesf", [128, 512], F32, es=eC)
                cT = k.sb("cT", [16, T], F32, es=eC)
                r1 = k.sb("r1", [16, T], F32, es=eC)
                parts = k.sb("parts", [16, 6, T], BF16, es=eC)
                ztmp = k.sb("ztmp", [128, 16], F32, es=eC)
                load_w(wf, 0, C_FF, 16)
                k.dma(sp, fb_bc[:], bass.AP(P["fox_forget_b"].t, 0, [[0, 128], [1, 16]]), reads=[P["fox_forget_b"]], writes=[fb_bc])
                k.dma(sp, upat[:], CD["upat"][:], reads=[CD["upat"]], writes=[upat])
                k.op(dve, lambda: V.memset(onesf[:], 1.0), writes=[onesf])
                if stop_after == "wf":
                    k.dma(sp, dbg["d_o"].t[0:128, 0:256], wf[:].rearrange("p a b -> p (a b)"), reads=[wf], writes=[dbg["d_o"]])
                    k.dma(pool, dbg["d_o"].t[128:256, 0:16], fb_bc[:], reads=[fb_bc], writes=[dbg["d_o"]])
                    return done([dbg["d_o"]])
                for i in range(NT):
                    pb = PB[i % 2]
                    for c in range(16):
                        mm(pb, pb[:, 0:16], xnT, xnT[:, c, i * 128:(i + 1) * 128], wf, wf[:, c, :], c == 0, c == 15)
                    tt(ztmp, ztmp[:], pb, pb[:, 0:16], fb_bc, fb_bc[:], ALU.add)
                    k.op(act, lambda: S.activation(out=ztmp[:], in_=ztmp[:], func=AF.Exp, scale=-1.0), reads=[ztmp], writes=[ztmp])
                    k.op(act, lambda: S.activation(out=ztmp[:], in_=ztmp[:], func=AF.Ln, scale=1.0, bias=1.0), reads=[ztmp], writes=[ztmp])
                    ts(logf, logf[:, i, :], ztmp, ztmp[:], -1.0, ALU.mult)
                for q in range(4):
                    pb = PB[q % 2]
                    n = 4 * q + 4
                    for j in range(n):
                        jo = j - 4 * q
                        if jo >= 0:
                            mm(pb, pb[0:16, :], logf, logf[:, j, :], upat, upat[:, jo, :], j == 0, j == n - 1)
                        else:
                            mm(pb, pb[0:16, :], logf, logf[:, j, :], onesf, onesf[:], j == 0, False)
                    evac(dve, cT[:, q * 512:(q + 1) * 512], pb[0:16, :], [pb], [cT])
                if stop_after == "lf":
                    k.dma(sp, dbg["d_h1"].t[0:128, 0:256], logf[:].rearrange("p a b -> p (a b)"), reads=[logf], writes=[dbg["d_h1"]])
                    k.dma(sp, dbg["d_h1"].t[128:144, :], cT[:], reads=[cT], writes=[dbg["d_h1"]])
                    return done([dbg["d_h1"]])
                k.op(dve, lambda: V.tensor_copy(out=parts[:, 0, :], in_=cT[:]), reads=[cT], writes=[parts])
                tt(r1, r1[:], cT, cT[:], parts, parts[:, 0, :], ALU.subtract)
                k.op(dve, lambda: V.tensor_copy(out=parts[:, 1, :], in_=r1[:]), reads=[r1], writes=[parts])
                tt(r1, r1[:], r1, r1[:], parts, parts[:, 1, :], ALU.subtract)
                k.op(dve, lambda: V.tensor_copy(out=parts[:, 2, :], in_=r1[:]), reads=[r1], writes=[parts])
                ts(parts, parts[:, 3:6, :], parts, parts[:, 0:3, :], -1.0, ALU.mult)
                k.dma(sp, cscr[:], parts[:], reads=[parts], writes=[cscr])

            if stop_after == "cs":
                k.dma(sp, dbg["d_o"].t[0:96, :].rearrange("(h k) t -> h k t", k=6), cscr[:], reads=[cscr], writes=[dbg["d_o"]])
                return done([dbg["d_o"]])
            with Scope(k) as eF:
                NH = 4
                wst_box[0] = k.sb("wstF", [128, 16 * 256], F32, es=eF)
                FQ = k.sb("FQ", [128, NH, T], BF16, es=eF)
                FK = k.sb("FK", [128, NH, T], BF16, es=eF)
                k.op(pool, lambda: G.memset(FQ[:], 0.0), writes=[FQ])
                k.op(pool, lambda: G.memset(FK[:], 0.0), writes=[FK])
                FV = k.sb("FV", [128, NT, NH, 65], BF16, es=eF)
                OJ = k.sb("OJ", [128, NT, NH * 64], BF16, es=eF)
                wq = [k.sb(f"wq{i}", [128, 16, 128], BF16, es=eF) for i in range(2)]
                wv = k.sb("wv", [128, 16, NH * 64], BF16, es=eF)
                pt_sb = [k.sb(f"pt_sb{i}", [128, 512], BF16, es=eF) for i in range(3)]
                rz = k.sb("rz", [128, 8], F32, es=eF)
                of32s = [k.sb(f"of32_{i}", [128, 64], F32, es=eF) for i in range(2)]
                FQh = [k.view(FQ, f"FQ{h}") for h in range(NH)]
                FKh = [k.view(FK, f"FK{h}") for h in range(NH)]
                FQc = k.view(FQ, "FQc"); FKc = k.view(FK, "FKc")
                for v_ in FQh + [FQc]:
                    v_.w = dict(FQ.w)
                for v_ in FKh + [FKc]:
                    v_.w = dict(FK.w)
                k.op(dve, lambda: V.memset(FV[:, :, :, 64:65], 1.0), writes=[FV])
                POb = PB[3:7]
                for fp in range(16 // NH):
                    h0 = fp * NH
                    for pj in range(NH // 2):
                        h = h0 + 2 * pj
                        for wb, col, DST, DSTh, sc in [(wq[0], C_FQ, FQ, FQh, 0.125), (wq[1], C_FK, FK, FKh, None)]:
                            load_w(wb, 0, col + h * 64, 128)
                            for tc in range(4):
                                pb = PB[rot[0] % 3]; rot[0] += 1
                                for c in range(16):
                                    mm(pb, pb[:, :], wb, wb[:, c, :], xnT, xnT[:, c, tc * 512:(tc + 1) * 512], c == 0, c == 15)
                                evac(act, DST[0:64, 2 * pj, tc * 512:(tc + 1) * 512], pb[0:64, :], [pb], [DSTh[2 * pj]], sc)
                                evac(dve, DST[64:128, 2 * pj + 1, tc * 512:(tc + 1) * 512], pb[64:128, :], [pb], [DSTh[2 * pj + 1]], sc)
                    load_w(wv, 0, C_FV + h0 * 64, NH * 64)
                    for i in range(NT):
                        pb = PB[rot[0] % 3]; rot[0] += 1
                        for c in range(16):
                            mm(pb, pb[:, 0:NH * 64], xnT, xnT[:, c, i * 128:(i + 1) * 128], wv, wv[:, c, :], c == 0, c == 15)
                        evac(act if i % 2 else dve, FV[:, i, :, 0:64], pb[:, 0:NH * 64].rearrange("p (h d) -> p h d", h=NH), [pb], [FV])
                    for par, r0 in [(0, 64), (1, 0)]:
                        hs = slice(h0 + par, h0 + NH, 2); ls = slice(par, NH, 2); nh2 = NH // 2
                        k.dma(sp, FQ[r0:r0 + 3, ls, :], cscr.t[hs, 0:3, :].rearrange("h k t -> k h t"), reads=[cscr], writes=[FQc])
                        k.dma(sp, FQ[r0 + 3:r0 + 6, ls, :], CD["ones_d"].t[:, 0:nh2, :], reads=[CD["ones_d"]], writes=[FQc])
                        k.dma(sp, FK[r0:r0 + 3, ls, :], CD["ones_d"].t[:, 0:nh2, :], reads=[CD["ones_d"]], writes=[FKc])
                        k.dma(sp, FK[r0 + 3:r0 + 6, ls, :], cscr.t[hs, 3:6, :].rearrange("h k t -> k h t"), reads=[cscr], writes=[FKc])
                    recs = [(hl, qc, kb) for hl in range(NH) for qc in range(4) for kb in range(4 * qc + 4)]

                    def geom(rec):
                        hl, qc, kb = rec
                        dg = kb - 4 * qc
                        q0 = qc * 512 + (dg * 128 if dg > 0 else 0)
                        return hl, qc, kb, dg, q0, (qc + 1) * 512 - q0

                    def fox_qk(rec, slot):
                        hl, qc, kb, dg, q0, n = geom(rec)
                        pb = PB[slot % 3]
                        mm(pb, pb[:, 0:n], FKh[hl], FK[:, hl, kb * 128:(kb + 1) * 128], FQh[hl], FQ[:, hl, q0:q0 + n],
                           True, dg < 0, extra_reads=[FQc, FKc])
                        if dg >= 0:
                            mm(pb, pb[:, 0:128], ident_bf, ident_bf[:], caus, caus[:], False, True)

                    def fox_pv(rec, slot):
                        hl, qc, kb, dg, q0, n = geom(rec)
                        pb = PB[slot % 3]; ptb = pt_sb[slot % 3]
                        k.op(act, lambda: S.activation(out=ptb[:, 0:n], in_=pb[:, 0:n], func=AF.Exp), reads=[pb], writes=[ptb])
                        j0 = max(dg, 0)
                        for jq in range(j0, 4):
                            po = POb[jq]
                            cs = (jq - j0) * 128
                            mm(po, po[:, 0:65], ptb, ptb[:, cs:cs + 128], FV, FV[:, kb, hl, :], kb == 0, kb == 4 * qc + jq)
                        if kb == 4 * qc + 3:
                            for jq in range(4):
                                po = POb[jq]; i = qc * 4 + jq
                                k.op(dve, lambda: V.reciprocal(out=rz[:, jq:jq + 1], in_=po[:, 64:65]), reads=[po], writes=[rz])
                                of = of32s[jq % 2]
                                ts(of, of[:], po, po[:, 0:64], rz[:, jq:jq + 1], ALU.mult, sreads=[rz])
                                k.op(act, lambda: S.activation(out=junk[:, 0:64], in_=of[:], func=AF.Square, accum_out=sstmp[:, 0:1]),
                                     reads=[of], writes=[junk, sstmp])
                                tt(ssf, ssf[:, i:i + 1], ssf, ssf[:, i:i + 1], sstmp, sstmp[:, 0:1], ALU.add)
                                k.op(dve, lambda: V.tensor_copy(out=OJ[:, i, hl * 64:(hl + 1) * 64], in_=of[:]), reads=[of], writes=[OJ])
                            if qc % 2 == 1:
                                precast_step(1)

                    fox_qk(recs[0], 0)
                    for t_ in range(len(recs)):
                        if t_ + 1 < len(recs):
                            fox_qk(recs[t_ + 1], t_ + 1)
                        fox_pv(recs[t_], t_)
                    for i in range(NT):
                        c0 = 1024 + h0 * 64
                        o_store(o_scr[i * 128:(i + 1) * 128, c0:c0 + NH * 64], OJ, OJ[:, i, :])

            if stop_after == "fox":
                k.dma(sp, dbg["d_o"][:], o_scr[:], reads=o_views, writes=[dbg["d_o"]])
                return done([dbg["d_o"]])

            with Scope(k) as eN:
                wst_box[0] = k.sb("wstN", [128, 16 * 144], F32, es=eN)
                QT = k.sb("QT", [128, 4, T], BF16, es=eN)
                KTS = k.sb("KTS", [128, T], BF16, es=eN)
                KTW = k.sb("KTW", [128, T], BF16, es=eN)
                for b_ in (QT, KTS, KTW):
                    k.op(pool, lambda: G.memset(b_[:], 0.0), writes=[b_])
                CK = k.sb("CK", [64, T], BF16, es=eN)
                CV = k.sb("CV", [64, T], BF16, es=eN)
                VS = k.sb("VS", [128, NT, 65], BF16, es=eN)
                VW = k.sb("VW", [128, NT, 65], BF16, es=eN)
                GT = k.sb("GT", [128, NT, 12], F32, es=eN)
                W1K = k.sb("W1K", [64, 32, 128], BF16, es=eN)
                W1V = k.sb("W1V", [64, 32, 128], BF16, es=eN)
                W2K = k.sb("W2K", [128, 64], BF16, es=eN)
                W2V = k.sb("W2V", [128, 64], BF16, es=eN)
                pe_ld = k.sb("pe_ld", [32, 128], F32, es=eN)
                PET = k.sb("PET", [64, 2, 32], BF16, es=eN)
                b1 = k.sb("b1", [128, 2], F32, es=eN)
                HK = k.sb("HK", [128, 128], BF16, es=eN)
                HV = k.sb("HV", [128, 128], BF16, es=eN)
                KC = k.sb("KC", [128, 128], BF16, es=eN)
                VCX = k.sb("VCX", [128, 97], BF16, es=eN)
                B0 = k.sb("B0", [128, 4, 128], BF16, es=eN)
                B1 = k.sb("B1", [128, 4, 128], BF16, es=eN)
                W4X = k.sb("W4X", [128, 4, 128], BF16, es=eN)
                CB = [k.sb(f"CB{i}", [128, 4, 128], BF16, es=eN) for i in range(2)]
                NM4 = [k.sb(f"NM4{i}", [128, 4, 128], BF16, es=eN) for i in range(2)]
                emat = k.sb("emat", [128, T], BF16, es=eN)
                for b_ in NM4 + [emat]:
                    k.op(pool, lambda: G.memset(b_[:], 0.0), writes=[b_])
                selvalid = k.sb("selvalid", [128, 8, 32], F32, es=eN)
                seladd = k.sb("seladd", [128, 8, 32], F32, es=eN)
                OJn = k.sb("OJn", [128, NT, 256], BF16, es=eN)
                OA = k.sb("OA", [128, 4, 64], F32, es=eN)
                wqn = [k.sb(f"wqn{i}", [128, 16, 64], BF16, es=eN) for i in range(2)]
                wtm = k.sb("wtm", [128, 16, 144], BF16, es=eN)
                ptn = [k.sb(f"ptn{i}", [128, 512], BF16, es=eN) for i in range(3)]
                sm = k.sb("sm", [128, 64], F32, es=eN)
                imp = k.sb("imp", [128, 32], F32, es=eN)
                score = k.sb("score", [128, 32], F32, es=eN)
                score2 = k.sb("score2", [128, 32], F32, es=eN)
                mx = k.sb("mx", [128, 16], F32, es=eN)
                negmb = k.sb("negmb", [128, 32], BF16, es=eN)
                gtmp = k.sb("gtmp", [128, 12], F32, es=eN)
                k.dma(sp, emat[0:32, :], CD["emat"][:], reads=[CD["emat"]], writes=[emat])
                for b_, nm in [(selvalid, "selvalid"), (seladd, "seladd")]:
                    k.dma(sp, b_[:], CD[nm][:], reads=[CD[nm]], writes=[b_])
                k.dma(sp, W4X[:, 0, :], CD["w4m"][:], reads=[CD["w4m"]], writes=[W4X])
                for r in range(1, 4):
                    k.op(dve, lambda: V.tensor_copy(out=W4X[:, r, :], in_=W4X[:, 0, :]), reads=[W4X], writes=[W4X])
                wstn = wst_box[0]
                for (Wd, pn) in [(W1K, "cmp_k_w1"), (W1V, "cmp_v_w1")]:
                    for hf in range(2):
                        stv = wstn[0:64, 0:2048].rearrange("p (l h) -> p l h", l=16)
                        k.dma(sp, stv, P[pn].t[0, hf * 1024:(hf + 1) * 1024, :].rearrange("(l d) h -> d l h", d=64), reads=[P[pn]], writes=[wstn])
                        cast(pool, Wd, Wd[:, hf * 16:(hf + 1) * 16, :], wstn, stv)
                for (Wd, pn) in [(W2K, "cmp_k_w2"), (W2V, "cmp_v_w2")]:
                    k.dma(sp, wstn[:, 0:64], P[pn].t[0], reads=[P[pn]], writes=[wstn])
                    cast(pool, Wd, Wd[:], wstn, wstn[:, 0:64])
                k.dma(sp, pe_ld[:, 0:64], P["cmp_pe_k"].t[0], reads=[P["cmp_pe_k"]], writes=[pe_ld])
                k.dma(sp, pe_ld[:, 64:128], P["cmp_pe_v"].t[0], reads=[P["cmp_pe_v"]], writes=[pe_ld])
                for kv in range(2):
                    transpose(PB[0], PB[0][0:64, 0:32], pe_ld, pe_ld[:, kv * 64:(kv + 1) * 64], ident_f, kp=32)
                    evac(dve, PET[:, kv, :], PB[0][0:64, 0:32], [PB[0]], [PET])
                    W1 = W1K if kv == 0 else W1V
                    for l in range(32):
                        mm(PB[1], PB[1][:, 0:1], W1, W1[:, l, :], PET, PET[:, kv, l:l + 1], l == 0, l == 31)
                    evac(dve, b1[:, kv:kv + 1], PB[1][:, 0:1], [PB[1]], [b1])
                k.op(dve, lambda: V.memset(VS[:, :, 64:65], 1.0), writes=[VS])
                k.op(dve, lambda: V.memset(VW[:, :, 64:65], 1.0), writes=[VW])
                k.op(dve, lambda: V.memset(KC[:], 0.0), writes=[KC])
                k.op(dve, lambda: V.memset(VCX[:], 0.0), writes=[VCX])
                k.op(dve, lambda: V.memset(VCX[:, 64:65], 1.0), writes=[VCX])
                k.dma(sp, VCX[:, 65:97], CD["amat"][:], reads=[CD["amat"]], writes=[VCX])
                POb = PB[3:7]

                if stop_after == "nsa0":
                    k.dma(sp, dbg["d_o"][:], o_scr[:], reads=o_views, writes=[dbg["d_o"]])
                    return done([dbg["d_o"]])
                for g in range(4):
                    for r in range(4):
                        wb = wqn[r % 2]
                        load_w(wb, 0, C_NQ + (4 * g + r) * 64, 64)
                        proj_fm(wb, 64, QT, lambda tc: QT[0:64, r, tc * 512:(tc + 1) * 512], 0.125)
                    for ii, (col, dst) in enumerate([(C_KSLC, KTS), (C_KWIN, KTW), (C_KCMP, CK), (C_VCMP, CV)]):
                        wb = wqn[ii % 2]
                        load_w(wb, 0, col + g * 64, 64)
                        proj_fm(wb, 64, dst, lambda tc: dst[0:64, tc * 512:(tc + 1) * 512], None)
                    if os.environ.get("NSA_SKIP") == "qk":
                        k.dma(sp, dbg["d_o"][:], o_scr[:], reads=o_views, writes=[dbg["d_o"]])
                        return done([dbg["d_o"]])
                    wstn_ = wst_box[0]
                    stv_all = wstn_[:, 0:16 * 144].rearrange("p (c n) -> p c n", c=16)
                    for (c0_, col_, n_) in [(0, C_VSLC + g * 64, 64), (64, C_VWIN + g * 64, 64), (128, C_GATE + g * 12, 16)]:
                        k.dma(sp, stv_all[:, :, c0_:c0_ + n_], P["w_in"].t[0, :, col_:col_ + n_].rearrange("(c p) n -> p c n", p=128),
                              reads=[P["w_in"]], writes=[wstn_])
                    cast(pool, wtm, wtm[:], wstn_, stv_all)
                    for i in range(NT):
                        pb = PB[rot[0] % 3]; rot[0] += 1
                        for c in range(16):
                            mm(pb, pb[:, 0:144], xnT, xnT[:, c, i * 128:(i + 1) * 128], wtm, wtm[:, c, :], c == 0, c == 15)
                        evac(dve, VS[:, i, 0:64], pb[:, 0:64], [pb], [VS])
                        evac(dve, VW[:, i, 0:64], pb[:, 64:128], [pb], [VW])
                        evac(dve, gtmp[:], pb[:, 128:140], [pb], [gtmp])
                        k.op(act, lambda: S.activation(out=gtmp[:], in_=gtmp[:], func=AF.Exp, scale=-1.0), reads=[gtmp], writes=[gtmp])
                        ts(gtmp, gtmp[:], gtmp, gtmp[:], 1.0, ALU.add)
                        k.op(dve, lambda: V.reciprocal(out=GT[:, i, :], in_=gtmp[:]), reads=[gtmp], writes=[GT])
                    if os.environ.get("NSA_SKIP") == "proj":
                        k.dma(sp, dbg["d_o"][:], o_scr[:], reads=o_views, writes=[dbg["d_o"]])
                        return done([dbg["d_o"]])
                    for kv, (SRC, W1, H) in enumerate([(CK, W1K, HK), (CV, W1V, HV)]):
                        pb = PB[rot[0] % 3]; rot[0] += 1
                        for l in range(32):
                            mm(pb, pb[:, 0:127], W1, W1[:, l, :], SRC, SRC[:, l:l + 2017:16], l == 0, l == 31)
                        k.op(act, lambda: S.activation(out=H[:, 0:127], in_=pb[:, 0:127], func=AF.Silu, bias=b1[:, kv:kv + 1]),
                             reads=[pb, b1], writes=[H])
                    pb = PB[rot[0] % 3]; rot[0] += 1
                    mm(pb, pb[0:64, 0:127], W2K, W2K[:], HK, HK[:, 0:127], True, True)
                    evac(dve, KC[0:64, 0:127], pb[0:64, 0:127], [pb], [KC])
                    pb = PB[rot[0] % 3]; rot[0] += 1
                    mm(pb, pb[0:127, 0:64], HV, HV[:, 0:127], W2V, W2V[:], True, True)
                    evac(dve, VCX[0:127, 0:64], pb[0:127, 0:64], [pb], [VCX])
                    base = (4 * g) * 128 * VLEN + VOFF
                    if os.environ.get("NSA_SKIP") == "cmp":
                        k.dma(sp, dbg["d_o"][:], o_scr[:], reads=o_views, writes=[dbg["d_o"]])
                        return done([dbg["d_o"]])
                    k.dma(sp, B0[:], bass.AP(brd.t, base, [[VLEN - 1, 128], [128 * VLEN, 4], [1, 128]]), reads=[brd], writes=[B0])
                    k.dma(sp, B1[:], bass.AP(brd.t, base + 128, [[VLEN - 1, 128], [128 * VLEN, 4], [1, 128]]), reads=[brd], writes=[B1])

                    if stop_after == "nsa1":
                        k.dma(sp, dbg["d_o"][:], o_scr[:], reads=o_views, writes=[dbg["d_o"]])
                        return done([dbg["d_o"]])

                    def cb_load(qb):
                        cb = CB[qb % 2]
                        k.dma(sp, cb[:], bass.AP(brd.t, base + 128 * qb - 31, [[VLEN - 16, 128], [128 * VLEN, 4], [1, 128]]), reads=[brd], writes=[cb])

                    def n_qk(rec, slot):
                        kind, qb, kb = rec
                        pb = PB[slot % 3]
                        qap = QT[:, :, qb * 128:(qb + 1) * 128]
                        if kind == "cmp":
                            if qb + 1 < NT:
                                cb_load(qb + 1)
                            cb = CB[qb % 2]
                            mm(pb, pb[:, :], KC, KC[:], QT, qap, True, False)
                            mm(pb, pb[:, :], ident_bf, ident_bf[:], cb, cb[:], False, True)
                            return
                        sel = kind == "slc"
                        KT = KTS if sel else KTW
                        extras = []
                        if kb == qb:
                            extras.append((ident_bf, ident_bf[:], B0, B0[:]))
                        elif kb == qb - 1:
                            extras.append((ident_bf, ident_bf[:], B1, B1[:]))
                        if (not sel) and kb == qb - 4:
                            extras.append((ident_bf, ident_bf[:], W4X, W4X[:]))
                        if sel and qb >= 8:
                            nm4 = NM4[qb % 2]
                            extras.append((emat, emat[:, kb * 128:(kb + 1) * 128], nm4, nm4[:]))
                        mm(pb, pb[:, :], KT, KT[:, kb * 128:(kb + 1) * 128], QT, qap, True, len(extras) == 0)
                        for ei, (lb, la, rb, ra) in enumerate(extras):
                            mm(pb, pb[:, :], lb, la, rb, ra, False, ei == len(extras) - 1)

                    def n_pv(rec, slot):
                        kind, qb, kb = rec
                        pb = PB[slot % 3]; ptb = ptn[slot % 3]
                        k.op(act, lambda: S.activation(out=ptb[:], in_=pb[:], func=AF.Exp), reads=[pb], writes=[ptb])
                        if kind == "cmp":
                            po = PB[(slot + 1) % 3]
                            for r in range(4):
                                mm(po, po[:, r * 97:(r + 1) * 97], ptb, ptb[:, r * 128:(r + 1) * 128], VCX, VCX[:], True, True)
                            ts(sm, sm[:, 0:4], po, po[:, 64:64 + 97 * 3 + 1:97], 1e-30, ALU.max)
                            k.op(dve, lambda: V.reciprocal(out=sm[:, 4:8], in_=sm[:, 0:4]), reads=[sm], writes=[sm])
                            tt(sm, sm[:, 8:12], sm, sm[:, 4:8], GT, GT[:, qb, 0:12:3], ALU.mult)
                            for r in range(4):
                                ts(OA, OA[:, r, :], po, po[:, r * 97:r * 97 + 64], sm[:, 8 + r:9 + r], ALU.mult, sreads=[sm])
                            if qb >= 8:
                                ts(imp, imp[:], po, po[:, 65:97], sm[:, 4:5], ALU.mult, sreads=[sm])
                                for r in range(1, 4):
                                    stt(imp, imp[:], po, po[:, r * 97 + 65:r * 97 + 97], sm[:, 4 + r:5 + r], imp, imp[:], ALU.mult, ALU.add, sreads=[sm])
                                tt(score, score[:], imp, imp[:], selvalid, selvalid[:, qb - 8, :], ALU.mult)
                                tt(score, score[:], score, score[:], seladd, seladd[:, qb - 8, :], ALU.add)
                                k.op(dve, lambda: V.max(out=mx[:, 0:8], in_=score[:]), reads=[score], writes=[mx])
                                k.op(dve, lambda: V.match_replace(out=score2[:], in_to_replace=mx[:, 0:8], in_values=score[:], imm_value=-3.0e38),
                                     reads=[score, mx], writes=[score2])
                                k.op(dve, lambda: V.max(out=mx[:, 8:16], in_=score2[:]), reads=[score2], writes=[mx])
                                ts(negmb, negmb[:], score, score[:], mx[:, 15:16], ALU.is_lt, NEG, ALU.mult, sreads=[mx])
                                transpose(PT, PT[0:32, 0:128], negmb, negmb[:], ident_bf)
                                nm4 = NM4[qb % 2]
                                for r in range(4):
                                    evac(dve if r % 2 else act, nm4[0:32, r, :], PT[0:32, 0:128], [PT], [nm4])
                            return
                        sel = kind == "slc"
                        VT = VS if sel else VW
                        kb_lo = 0 if sel else max(0, qb - 4)
                        for r in range(4):
                            mm(POb[r], POb[r][:, 0:65], ptb, ptb[:, r * 128:(r + 1) * 128], VT, VT[:, kb, :], kb == kb_lo, kb == qb)
                        if kb == qb:
                            gate_off = 1 if sel else 2
                            o0 = 16 if sel else 24
                            for r in range(4):
                                k.op(dve, lambda: V.reciprocal(out=sm[:, o0 + r:o0 + r + 1], in_=POb[r][:, 64:65]), reads=[POb[r]], writes=[sm])
                            tt(sm, sm[:, o0 + 4:o0 + 8], sm, sm[:, o0:o0 + 4], GT, GT[:, qb, gate_off:12:3], ALU.mult)
                            for r in range(4):
                                stt(OA, OA[:, r, :], POb[r], POb[r][:, 0:64], sm[:, o0 + 4 + r:o0 + 5 + r], OA, OA[:, r, :], ALU.mult, ALU.add, sreads=[sm])
                            if sel:
                                k.op(act, lambda: S.activation(out=junk[:, 0:256], in_=OA[:].rearrange("p r d -> p (r d)"), func=AF.Square,
                                                               accum_out=sstmp[:, 1:2]), reads=[OA], writes=[junk, sstmp])
                                tt(ssn, ssn[:, qb:qb + 1], ssn, ssn[:, qb:qb + 1], sstmp, sstmp[:, 1:2], ALU.add)
                                k.op(dve, lambda: V.tensor_copy(out=OJn[:, qb, :], in_=OA[:].rearrange("p r d -> p (r d)")), reads=[OA], writes=[OJn])
                                precast_step(1)

                    recs = []
                    for qb in range(NT):
                        recs.append(("cmp", qb, 0))
                        recs += [("win", qb, kb) for kb in range(max(0, qb - 4), qb + 1)]
                        recs += [("slc", qb, kb) for kb in range(0, qb + 1)]
                    slots = []
                    sl_ = 0
                    for rec in recs:
                        slots.append(sl_)
                        sl_ += 2 if rec[0] == "cmp" else 1
                    cb_load(0)
                    n_qk(recs[0], slots[0])
                    for t_ in range(len(recs)):
                        if t_ + 1 < len(recs):
                            n_qk(recs[t_ + 1], slots[t_ + 1])
                        n_pv(recs[t_], slots[t_])
                    for i in range(NT):
                        o_store(o_scr[i * 128:(i + 1) * 128, g * 256:(g + 1) * 256], OJn, OJn[:, i, :])

        if stop_after == "nsa":
            k.dma(sp, dbg["d_o"][:], o_scr[:], reads=o_views, writes=[dbg["d_o"]])
            return done([dbg["d_o"]])

        OH1a = k.sb("OH1a", [128, NT, 32], F32)
        OH2a = k.sb("OH2a", [128, NT, 32], F32)
        SELb = k.sb("SELb", [128, NT, 32], BF16)
        Wk = k.sb("Wk", [128, NT, 2], F32)
        ROWI = k.sb("ROWI", [128, NT, 2], I32)
        with Scope(k) as eO:
            WO = k.sb("WO", [128, 16, D], BF16, es=eO)
            onw_ld = k.sb("onw_ld", [16, 128], F32, es=eO)
            onw_col = k.sb("onw_col", [128, 16], F32, es=eO)
            rs_n = k.sb("rs_n", [128, NT], F32, es=eO)
            rs_f = k.sb("rs_f", [128, NT], F32, es=eO)
            fnw_bc = k.sb("fnw_bc", [128, D], F32, es=eO)
            WR = k.sb("WR", [128, 16, 36], F32, es=eO)
            RB = k.sb("RB", [128, 36], F32, es=eO)
            ot = [k.sb(f"ot{i}", [128, D], BF16, es=eO) for i in range(2)]
            oT = k.sb("oT", [128, 16, 128], BF16, es=eO)
            xs2 = [k.sb(f"xs2{i}", [128, D], F32, es=eO) for i in range(2)]
            h1t = k.sb("h1t", [128, D], F32, es=eO)
            hn32 = k.sb("hn32", [128, D], F32, es=eO)
            hnb = k.sb("hnb", [128, D], BF16, es=eO)
            hnT = k.sb("hnT", [128, 16, 128], F32, es=eO)
            lg = k.sb("lg", [128, 36], F32, es=eO)
            rt = k.sb("rt", [128, 64], F32, es=eO)
            elg = k.sb("elg", [128, 8], F32, es=eO)
            oh = k.sb("oh", [128, 24], F32, es=eO)
            wso = [k.sb(f"wso{i}", [128, D], F32, es=eO) for i in range(2)]
            for c in range(16):
                k.dma(sp, wso[c % 2][:], P["w_out"].t[0, c * 128:(c + 1) * 128, :], reads=[P["w_out"]], writes=[wso[c % 2]])
                k.op(pool, lambda: G.tensor_copy(out=WO[:, c, :], in_=wso[c % 2][:]), reads=[wso[c % 2]], writes=[WO])
            k.dma(sp, onw_ld[0:8, :], P["nsa_out_norm_w"].t[0].rearrange("(c p) -> c p", p=128), reads=[P["nsa_out_norm_w"]], writes=[onw_ld])
            k.dma(sp, onw_ld[8:16, :], P["fox_out_norm_w"].t[0].rearrange("(c p) -> c p", p=128), reads=[P["fox_out_norm_w"]], writes=[onw_ld])
            transpose(PB[0], PB[0][:, 0:16], onw_ld, onw_ld[:], ident_f, kp=16)
            evac(dve, onw_col[:], PB[0][:, 0:16], [PB[0]], [onw_col])
            k.dma(sp, fnw_bc[:], bass.AP(P["ffn_norm_w"].t, 0, [[0, 128], [1, D]]), reads=[P["ffn_norm_w"]], writes=[fnw_bc])
            with nc.allow_non_contiguous_dma(reason="small router weights"):
                k.dma(sp, WR[:, :, 0:4], P["router_group_w"].t[0].rearrange("(c p) n -> p c n", p=128), reads=[P["router_group_w"]], writes=[WR])
                k.dma(sp, WR[:, :, 4:36], P["router_expert_w"].t[0].rearrange("(c p) n -> p c n", p=128), reads=[P["router_expert_w"]], writes=[WR])
            k.dma(sp, RB[:, 0:4], bass.AP(P["router_group_b"].t, 0, [[0, 128], [1, 4]]), reads=[P["router_group_b"]], writes=[RB])
            k.dma(sp, RB[:, 4:36], bass.AP(P["router_expert_b"].t, 0, [[0, 128], [1, 32]]), reads=[P["router_expert_b"]], writes=[RB])
            for i in range(NT):
                rstd_from_ss(ssn[:, i:i + 1], ssn, rs_n[:, i:i + 1], rs_n, 1024)
                rstd_from_ss(ssf[:, i:i + 1], ssf, rs_f[:, i:i + 1], rs_f, 1024)
            for i in range(NT):
                o_t = ot[i % 2]; xs = xs2[i % 2]
                k.dma(sp, o_t[:], o_scr[i * 128:(i + 1) * 128, :], reads=o_views, writes=[o_t])
                k.dma(sp, xs[:], x_d[i * 128:(i + 1) * 128, :], reads=[x_d], writes=[xs])
                for c4 in range(4):
                    for cc in range(4):
                        c = c4 * 4 + cc
                        transpose(PT, PT[:, cc * 128:(cc + 1) * 128], o_t, o_t[:, c * 128:(c + 1) * 128], ident_bf)
                    for cc in range(4):
                        c = c4 * 4 + cc
                        ts(oT, oT[:, c, :], PT, PT[:, cc * 128:(cc + 1) * 128], onw_col[:, c:c + 1], ALU.mult, sreads=[onw_col])
                for dmb in range(4):
                    pn = PB[(2 * dmb) % 4]; pf = PB[(2 * dmb + 1) % 4]
                    for c in range(8):
                        mm(pn, pn[:], oT, oT[:, c, :], WO, WO[:, c, dmb * 512:(dmb + 1) * 512], c == 0, c == 7)
                    for c in range(8, 16):
                        mm(pf, pf[:], oT, oT[:, c, :], WO, WO[:, c, dmb * 512:(dmb + 1) * 512], c == 8, c == 15)
                    sl = slice(dmb * 512, (dmb + 1) * 512)
                    stt(h1t, h1t[:, sl], pn, pn[:], rs_n[:, i:i + 1], xs, xs[:, sl], ALU.mult, ALU.add, sreads=[rs_n])
                    stt(h1t, h1t[:, sl], pf, pf[:], rs_f[:, i:i + 1], h1t, h1t[:, sl], ALU.mult, ALU.add, sreads=[rs_f])
                k.dma(sp, h1_scr[i * 128:(i + 1) * 128, :], h1t[:], reads=[h1t], writes=[h1_scr])
                k.op(act, lambda: S.activation(out=junk[:], in_=h1t[:], func=AF.Square, accum_out=sstmp[:, 0:1]), reads=[h1t], writes=[junk, sstmp])
                rstd_from_ss(sstmp[:, 0:1], sstmp, rt[:, 0:1], rt, D)
                stt(hn32, hn32[:], h1t, h1t[:], rt[:, 0:1], fnw_bc, fnw_bc[:], ALU.mult, ALU.mult, sreads=[rt])
                k.op(act, lambda: S.copy(out=hnb[:], in_=hn32[:]), reads=[hn32], writes=[hnb])
                k.dma(sp, hn_scr[i * 128:(i + 1) * 128, :], hnb[:], reads=[hnb], writes=[hn_scr])
                for c4 in range(4):
                    pb = PB[4 + c4 % 2]
                    for cc in range(4):
                        c = c4 * 4 + cc
                        transpose(pb, pb[:, cc * 128:(cc + 1) * 128], hn32, hn32[:, c * 128:(c + 1) * 128], ident_f)
                    evac(act if c4 % 2 else dve, hnT[:, c4 * 4:(c4 + 1) * 4, :], pb[:].rearrange("p (c t) -> p c t", c=4), [pb], [hnT])
                pl = PB[6]
                for c in range(16):
                    mm(pl, pl[:, 0:36], hnT, hnT[:, c, :], WR, WR[:, c, :], c == 0, c == 15)
                tt(lg, lg[:], pl, pl[:, 0:36], RB, RB[:], ALU.add)
                k.op(dve, lambda: V.reduce_max(out=rt[:, 1:2], in_=lg[:, 0:4], axis=AX.X), reads=[lg], writes=[rt])
                ts(oh, oh[:, 0:4], lg, lg[:, 0:4], rt[:, 1:2], ALU.is_ge, sreads=[rt])
                ts(rt, rt[:, 2:3], rt, rt[:, 1:2], -1.0, ALU.mult)
                k.op(act, lambda: S.activation(out=rt[:, 8:12], in_=lg[:, 0:4], func=AF.Exp, bias=rt[:, 2:3], accum_out=rt[:, 3:4]),
                     reads=[lg, rt], writes=[rt])
                k.op(dve, lambda: V.reciprocal(out=rt[:, 4:5], in_=rt[:, 3:4]), reads=[rt], writes=[rt])
                ts(elg, elg[:], lg, lg[:, 4:12], oh[:, 0:1], ALU.mult, sreads=[oh])
                for gg in range(1, 4):
                    stt(elg, elg[:], lg, lg[:, 4 + gg * 8:12 + gg * 8], oh[:, gg:gg + 1], elg, elg[:], ALU.mult, ALU.add, sreads=[oh])
                k.op(dve, lambda: V.reduce_max(out=rt[:, 5:6], in_=elg[:], axis=AX.X), reads=[elg], writes=[rt])
                ts(oh, oh[:, 8:16], elg, elg[:], rt[:, 5:6], ALU.is_ge, sreads=[rt])
                stt(elg, elg[:], oh, oh[:, 8:16], -1.0e30, elg, elg[:], ALU.mult, ALU.add)
                k.op(dve, lambda: V.reduce_max(out=rt[:, 6:7], in_=elg[:], axis=AX.X), reads=[elg], writes=[rt])
                ts(oh, oh[:, 16:24], elg, elg[:], rt[:, 6:7], ALU.is_ge, sreads=[rt])
                tt(rt, rt[:, 7:8], rt, rt[:, 6:7], rt, rt[:, 5:6], ALU.subtract)
                k.op(act, lambda: S.activation(out=rt[:, 12:13], in_=rt[:, 7:8], func=AF.Exp), reads=[rt], writes=[rt])
                ts(rt, rt[:, 13:14], rt, rt[:, 12:13], 1.0, ALU.add)
                k.op(dve, lambda: V.reciprocal(out=rt[:, 14:15], in_=rt[:, 13:14]), reads=[rt], writes=[rt])
                tt(Wk, Wk[:, i, 0:1], rt, rt[:, 14:15], rt, rt[:, 4:5], ALU.mult)
                tt(rt, rt[:, 15:16], rt, rt[:, 14:15], rt, rt[:, 12:13], ALU.mult)
                tt(Wk, Wk[:, i, 1:2], rt, rt[:, 15:16], rt, rt[:, 4:5], ALU.mult)
                for gg in range(4):
                    ts(OH1a, OH1a[:, i, gg * 8:(gg + 1) * 8], oh, oh[:, 8:16], oh[:, gg:gg + 1], ALU.mult, sreads=[oh])
                    ts(OH2a, OH2a[:, i, gg * 8:(gg + 1) * 8], oh, oh[:, 16:24], oh[:, gg:gg + 1], ALU.mult, sreads=[oh])
                tt(SELb, SELb[:, i, :], OH1a, OH1a[:, i, :], OH2a, OH2a[:, i, :], ALU.add)

        if stop_after == "oproj":
            k.dma(sp, dbg["d_h1"][:], h1_scr[:], reads=[h1_scr], writes=[dbg["d_h1"]])
            return done([dbg["d_h1"]])

        IDXI = k.sb("IDXI", [128, NSLOT], I32)
        with Scope(k) as eR:
            onesb = k.sb("onesb", [128, 128], BF16, es=eR)
            stri = k.sb("stri", [128, 128], BF16, es=eR)
            ncnt = k.sb("ncnt", [128, 32], F32, es=eR)
            tl = k.sb("tl", [128, 32], F32, es=eR)
            cA = k.sb("cA", [128, 32], F32, es=eR)
            cBb = k.sb("cBb", [128, 32], F32, es=eR)
            basef = k.sb("basef", [128, 32], F32, es=eR)
            rowf = k.sb("rowf", [128, 32], F32, es=eR)
            tmp32 = k.sb("tmp32", [128, 32], F32, es=eR)
            ROWF = k.sb("ROWF", [128, NT, 2], F32, es=eR)
            esl_f = k.sb("esl_f", [1, NSLOT + 1], F32, es=eR)
            nfl_f = k.sb("nfl_f", [1, NSLOT], F32, es=eR)
            k.op(dve, lambda: V.memset(onesb[:], 1.0), writes=[onesb])
            k.dma(sp, stri[:], CD["stri"][:], reads=[CD["stri"]], writes=[stri])
            pcnt = PB[0]
            for i in range(NT):
                mm(pcnt, pcnt[:, 0:32], onesb, onesb[:], SELb, SELb[:, i, :], i == 0, i == NT - 1)
            evac(dve, ncnt[:], pcnt[:, 0:32], [pcnt], [ncnt])
            k.op(dve, lambda: V.memset(tl[:], 0.0), writes=[tl])
            for j in range(16):
                stt(tl, tl[:], ncnt, ncnt[:], float(128 * j), tl, tl[:], ALU.is_gt, ALU.add)
            k.op(dve, lambda: V.tensor_copy(out=cA[:], in_=tl[:]), reads=[tl], writes=[cA])
            src, dst = cA, cBb
            for sft in [1, 2, 4, 8, 16]:
                k.op(dve, lambda: V.tensor_copy(out=dst[:, 0:sft], in_=src[:, 0:sft]), reads=[src], writes=[dst])
                tt(dst, dst[:, sft:32], src, src[:, sft:32], src, src[:, 0:32 - sft], ALU.add)
                src, dst = dst, src
            cum = src
            tt(basef, basef[:], cum, cum[:], tl, tl[:], ALU.subtract)
            ts(basef, basef[:], basef, basef[:], 128.0, ALU.mult)
            for i in range(NT):
                pp = PB[1 + i % 2]
                for j in range(i):
                    mm(pp, pp[:, 0:32], onesb, onesb[:], SELb, SELb[:, j, :], j == 0, False)
                mm(pp, pp[:, 0:32], stri, stri[:], SELb, SELb[:, i, :], i == 0, True)
                tt(rowf, rowf[:], pp, pp[:, 0:32], basef, basef[:], ALU.add)
                for kk, OH in enumerate([OH1a, OH2a]):
                    tt(tmp32, tmp32[:], rowf, rowf[:], OH, OH[:, i, :], ALU.mult)
                    k.op(dve, lambda: V.reduce_sum(out=ROWF[:, i, kk:kk + 1], in_=tmp32[:], axis=AX.X), reads=[tmp32], writes=[ROWF])
            k.op(dve, lambda: V.tensor_copy(out=ROWI[:], in_=ROWF[:]), reads=[ROWF], writes=[ROWI])
            k.op(dve, lambda: V.memset(esl_f[:], -1.0), writes=[esl_f])
            for s in range(NSLOT):
                k.op(dve, lambda: V.tensor_scalar(out=tmp32[0:1, :], in0=cum[0:1, :], scalar1=float(s), scalar2=None, op0=ALU.is_le,
                                                  op1=ALU.add, accum_out=esl_f[0:1, s + 1:s + 2]), reads=[cum], writes=[tmp32, esl_f])
            ts(esl_f, esl_f[0:1, 1:NSLOT + 1], esl_f, esl_f[0:1, 1:NSLOT + 1], 31.0, ALU.min)
            k.op(dve, lambda: V.memset(nfl_f[:], 1.0), writes=[nfl_f])
            tt(nfl_f, nfl_f[0:1, 2:NSLOT], esl_f, esl_f[0:1, 3:NSLOT + 1], esl_f, esl_f[0:1, 1:NSLOT - 1], ALU.not_equal)
            onesrow = k.sb("onesrow", [1, 128], F32, es=eR)
            iop = k.sb("iop", [128, 1], F32, es=eR)
            idxf = k.sb("idxf", [128, NSLOT], F32, es=eR)
            k.op(dve, lambda: V.memset(onesrow[:], 1.0), writes=[onesrow])
            k.dma(sp, iop[:], CD["iota_p"][:], reads=[CD["iota_p"]], writes=[iop])
            mm(PB[3], PB[3][:, 0:NSLOT], onesrow, onesrow[:], esl_f, esl_f[0:1, 1:NSLOT + 1], True, True)
            mm(PB[4], PB[4][:, 0:NSLOT], onesrow, onesrow[:], nfl_f, nfl_f[:], True, True)
            ts(idxf, idxf[:], PB[3], PB[3][:, 0:NSLOT], 128.0, ALU.mult, iop[:, 0:1], ALU.add, sreads=[iop])
            ts(idxf, idxf[:], idxf, idxf[:], -100000.0, ALU.add)
            tt(idxf, idxf[:], idxf, idxf[:], PB[4], PB[4][:, 0:NSLOT], ALU.mult)
            ts(idxf, idxf[:], idxf, idxf[:], 100000.0, ALU.add)
            k.op(dve, lambda: V.tensor_copy(out=IDXI[:], in_=idxf[:]), reads=[idxf], writes=[IDXI])
            hb = [k.sb(f"hb{i}", [128, D], BF16, es=eR) for i in range(2)]
            for i in range(NT):
                hbt = hb[i % 2]
                k.dma(sp, hbt[:], hn_scr[i * 128:(i + 1) * 128, :], reads=[hn_scr], writes=[hbt])
                for kk in range(2):
                    k.dma(pool, None, None, reads=[hbt, ROWI], writes=[xslot],
                          fn=lambda: G.indirect_dma_start(out=xslot[:], out_offset=bass.IndirectOffsetOnAxis(ap=ROWI[:, i, kk:kk + 1], axis=0),
                                                          in_=hbt[:], in_offset=None))

        with Scope(k) as eM:
            WGs = [k.sb(f"WG{i}", [128, 16, 512], BF16, es=eM) for i in range(2)]
            WUs = [k.sb(f"WU{i}", [128, 16, 512], BF16, es=eM) for i in range(2)]
            WDs = [k.sb(f"WD{i}", [128, 4, D], BF16, es=eM) for i in range(2)]
            xgbs = [k.sb(f"xgb{i}", [128, D], BF16, es=eM) for i in range(2)]
            xgTs = [k.sb(f"xgT{i}", [128, 16, 128], BF16, es=eM) for i in range(2)]
            hTs = [k.sb(f"hT{i}", [128, 4, 128], BF16, es=eM) for i in range(2)]
            sgs = [k.sb(f"sg{i}", [128, 128], F32, es=eM) for i in range(2)]
            ybts = [k.sb(f"ybt{i}", [128, D], F32, es=eM) for i in range(2)]
            PTh = [k.view(PT, "PTa"), k.view(PT, "PTb")]
            precast_step(1000)
            bc_reg = G.to_reg(4095)

            def load_w_slot(s):
                for Wb, src in [(WGs[s % 2], wbf["g"]), (WUs[s % 2], wbf["u"]), (WDs[s % 2], wbf["d"])]:
                    src2d = src.t[:].rearrange("e (p c) f -> (e p) (c f)", p=128)
                    k.dma(pool, None, None, reads=pre_views + [IDXI], writes=[Wb],
                          fn=lambda: G.indirect_dma_start(out=Wb[:].rearrange("p c f -> p (c f)"), out_offset=None, in_=src2d,
                                                          in_offset=bass.IndirectOffsetOnAxis(ap=IDXI[:, s:s + 1], axis=0),
                                                          bounds_check=bc_reg, oob_is_err=False))

            def load_x_dma(s):
                xgb = xgbs[s % 2]
                k.dma(sp, xgb[:], xslot[s * 128:(s + 1) * 128, :], reads=[xslot], writes=[xgb])

            def load_x_tr(s):
                xgb = xgbs[s % 2]; xgT = xgTs[s % 2]
                for c4 in range(4):
                    for cc in range(4):
                        c = c4 * 4 + cc
                        transpose(PT, PT[:, cc * 128:(cc + 1) * 128], xgb, xgb[:, c:D:16], ident_bf)
                    evac(act if c4 % 2 else dve, xgT[:, c4 * 4:(c4 + 1) * 4, :], PT[:, 0:512].rearrange("p (c t) -> p c t", c=4), [PT], [xgT])

            load_x_dma(0)
            load_x_dma(1)
            load_w_slot(0)
            load_x_tr(0)
            for s in range(NSLOT):
                xgT = xgTs[s % 2]; ybt = ybts[s % 2]; hT = hTs[s % 2]
                WG, WU, WD = WGs[s % 2], WUs[s % 2], WDs[s % 2]
                if s + 1 < NSLOT:
                    load_x_tr(s + 1)
                    if s + 2 < NSLOT:
                        load_x_dma(s + 2)
                    load_w_slot(s + 1)
                for c2 in range(4):
                    pg = PB[(2 * c2) % 4]; pu = PB[(2 * c2 + 1) % 4]; sg = sgs[c2 % 2]
                    for c in range(16):
                        mm(pg, pg[:, 0:128], WG, WG[:, c, c2:512:4], xgT, xgT[:, c, :], c == 0, c == 15)
                    for c in range(16):
                        mm(pu, pu[:, 0:128], WU, WU[:, c, c2:512:4], xgT, xgT[:, c, :], c == 0, c == 15)
                    k.op(act, lambda: S.activation(out=sg[:], in_=pg[:, 0:128], func=AF.Silu), reads=[pg], writes=[sg])
                    tt(hT, hT[:, c2, :], sg, sg[:], pu, pu[:, 0:128], ALU.mult)
                for dmb in range(4):
                    pd = PB[4 + dmb % 3]
                    for c2 in range(4):
                        mm(pd, pd[:], hT, hT[:, c2, :], WD, WD[:, c2, dmb * 512:(dmb + 1) * 512], c2 == 0, c2 == 3)
                    evac(act if dmb % 2 else dve, ybt[:, dmb * 512:(dmb + 1) * 512], pd[:], [pd], [ybt])
                k.dma(sp, yslot[s * 128:(s + 1) * 128, :], ybt[:], reads=[ybt], writes=[yslot])

        with Scope(k) as eZ:
            fw_bc = k.sb("fw_bc", [128, D], F32, es=eZ)
            k.dma(sp, fw_bc[:], bass.AP(P["final_norm_w"].t, 0, [[0, 128], [1, D]]), reads=[P["final_norm_w"]], writes=[fw_bc])
            h1b = [k.sb(f"h1b{i}", [128, D], F32, es=eZ) for i in range(2)]
            g0 = [k.sb(f"g0{i}", [128, D], F32, es=eZ) for i in range(2)]
            g1 = [k.sb(f"g1{i}", [128, D], F32, es=eZ) for i in range(2)]
            ob = [k.sb(f"ob{i}", [128, D], F32, es=eZ) for i in range(2)]
            rf = k.sb("rf", [128, 4], F32, es=eZ)
            for i in range(NT):
                hh = h1b[i % 2]; a0 = g0[i % 2]; a1 = g1[i % 2]; oo = ob[i % 2]
                k.dma(sp, hh[:], h1_scr[i * 128:(i + 1) * 128, :], reads=[h1_scr], writes=[hh])
                for kk, gb in enumerate([a0, a1]):
                    k.dma(pool, None, None, reads=[yslot, ROWI], writes=[gb],
                          fn=lambda: G.indirect_dma_start(out=gb[:], out_offset=None, in_=yslot[:],
                                                          in_offset=bass.IndirectOffsetOnAxis(ap=ROWI[:, i, kk:kk + 1], axis=0)))
                stt(hh, hh[:], a0, a0[:], Wk[:, i, 0:1], hh, hh[:], ALU.mult, ALU.add, sreads=[Wk])
                stt(hh, hh[:], a1, a1[:], Wk[:, i, 1:2], hh, hh[:], ALU.mult, ALU.add, sreads=[Wk])
                k.op(act, lambda: S.activation(out=junk[:], in_=hh[:], func=AF.Square, accum_out=rf[:, 0:1]), reads=[hh], writes=[junk, rf])
                rstd_from_ss(rf[:, 0:1], rf, rf[:, 1:2], rf, D)
                stt(oo, oo[:], hh, hh[:], rf[:, 1:2], fw_bc, fw_bc[:], ALU.mult, ALU.mult, sreads=[rf])
                k.dma(sp, out_d[i * 128:(i + 1) * 128, :], oo[:], reads=[oo], writes=[out_d])
        return done([out_d])


_NC_CACHE = {}


def _in_maps(inputs, n_cores=8):
    consts = _consts()
    maps = []
    for c in range(n_cores):
        m = {"x": np.ascontiguousarray(inputs["x"][c])}
        for name, shape in PARAM_SPECS:
            m[name] = np.ascontiguousarray(np.asarray(inputs[name], dtype=np.float32).reshape(shape))
        for name, shape, dt in CONST_SPECS:
            m["c_" + name] = consts[name]
        maps.append(m)
    return maps


def kernel(**inputs):
    if "nc" not in _NC_CACHE:
        _NC_CACHE["nc"] = build_nc()
    nc = _NC_CACHE["nc"]
    res = run_bass_kernel_spmd(nc, _in_maps(inputs), core_ids=list(range(8)))
    return np.stack([np.asarray(r["out"], dtype=np.float32) for r in res.results], axis=0)
```

```python
import math
import os
from contextlib import ExitStack

import ml_dtypes
import numpy as np

import concourse.bass as bass
import concourse.mybir as mybir
from concourse.bass_utils import run_bass_kernel_spmd

F32 = mybir.dt.float32
BF16 = mybir.dt.bfloat16
I32 = mybir.dt.int32
AF = mybir.ActivationFunctionType
ALU = mybir.AluOpType
AX = mybir.AxisListType

T = 2048
D = 2048
NT = 16
HD = 64
NEG = -30000.0
IN_COLS = 5696
C_NQ, C_KCMP, C_VCMP, C_KSLC, C_VSLC, C_KWIN, C_VWIN, C_GATE, C_FQ, C_FK, C_FV, C_FF = (
    0, 1024, 1280, 1536, 1792, 2048, 2304, 2560, 2608, 3632, 4656, 5680)
NSLOT = 64
VOFF = 2112
VLEN = 4608


class Eng:
    def __init__(self, name, e, sem, strict_self=True):
        self.name = name; self.e = e; self.sem = sem; self.cnt = 0
        self.waited = {}; self.strict_self = strict_self


class Buf:
    def __init__(self, t, name=""):
        self.t = t; self.name = name; self.w = {}; self.r = {}

    def __getitem__(self, idx):
        return self.t[idx]


class K:
    def __init__(self, nc, es, n_dma_sems=40):
        self.nc = nc; self.es = es

        def mk(name, e, strict=True):
            return Eng(name, e, es.enter_context(nc.semaphore("sem_" + name)), strict)
        self.pe = mk("pe", nc.tensor, strict=False)
        relax = os.environ.get("K_RELAX", "") .split(",")
        self.act = mk("act", nc.scalar, strict="act" not in relax)
        self.dve = mk("dve", nc.vector, strict="dve" not in relax)
        self.pool = mk("pool", nc.gpsimd, strict="pool" not in relax)
        self.sp = mk("sp", nc.sync)
        self.dsems = [[es.enter_context(nc.semaphore(f"dsem{i}")), 0] for i in range(n_dma_sems)]
        self.dnext = 0
        self.nwaits = 0; self.nops = 0; self.ndma = 0

    def sb(self, name, shape, dt, es=None):
        return Buf((es or self.es).enter_context(self.nc.sbuf_tensor(name, list(shape), dt)), name)

    def ps(self, name, shape, dt):
        return Buf(self.es.enter_context(self.nc.psum_tensor(name, list(shape), dt)), name)

    def dram(self, name, shape, dt, kind="Internal"):
        return Buf(self.nc.dram_tensor(name, list(shape), dt, kind=kind), name)

    def view(self, buf, name=""):
        return Buf(buf.t, name or buf.name)

    def _wait(self, E, need, keep_last=False):
        pend = []
        for key, (sem, val) in need.items():
            if sem is E.sem and not E.strict_self:
                continue
            if E.waited.get(key, 0) >= val:
                continue
            pend.append((key, sem, val))
        last = None
        if keep_last and pend:
            last = pend.pop()
        for key, sem, val in pend:
            E.e.wait_ge(sem, val); E.waited[key] = val; self.nwaits += 1
        if last is not None:
            E.waited[last[0]] = last[2]
        return last

    @staticmethod
    def _merge(need, d):
        for kk, (s, v) in d.items():
            if kk not in need or need[kk][1] < v:
                need[kk] = (s, v)

    def _deps(self, reads, writes):
        need = {}
        for b in reads:
            self._merge(need, b.w)
        for b in writes:
            self._merge(need, b.w); self._merge(need, b.r)
        return need

    def op(self, E, fn, reads=(), writes=()):
        last = self._wait(E, self._deps(reads, writes), keep_last=True)
        ins = fn()
        if last is not None:
            ins._wait_ge(last[1], last[2])
        E.cnt += 1; self.nops += 1
        ins.then_inc(E.sem, 1)
        tok = (E.sem, E.cnt); key = id(E.sem)
        for b in reads:
            b.r[key] = tok
        for b in writes:
            b.w = {key: tok}; b.r = {}
        return ins

    def dma(self, E, out_ap, in_ap, reads=(), writes=(), fn=None, sems=None, **kw):
        need = self._deps(reads, writes)
        if sems is not None:
            ds = sems[0][sems[1][0] % len(sems[0])]; sems[1][0] += 1
        else:
            ds = self.dsems[self.dnext]; self.dnext = (self.dnext + 1) % len(self.dsems)
        if ds[1] > 0:
            self._merge(need, {id(ds[0]): (ds[0], ds[1])})
        self._wait(E, need)
        if fn is None:
            ins = E.e.dma_start(out=out_ap, in_=in_ap, **kw)
        else:
            ins = fn()
        ds[1] += 16; self.ndma += 1
        ins.then_inc(ds[0], 16)
        tok = (ds[0], ds[1]); key = id(ds[0])
        for b in reads:
            b.r[key] = tok
        for b in writes:
            b.w = {key: tok}; b.r = {}
        return ins

    def barrier(self):
        engs = [self.pe, self.act, self.dve, self.pool, self.sp]
        for E in engs:
            need = {}
            for F in engs:
                if F is not E and F.cnt > 0:
                    need[id(F.sem)] = (F.sem, F.cnt)
            for ds in self.dsems:
                if ds[1] > 0:
                    need[id(ds[0])] = (ds[0], ds[1])
            self._wait(E, need)

    def finish(self, bufs):
        need = {}
        for b in bufs:
            self._merge(need, b.w)
        self._wait(self.sp, need)


class Scope:
    def __init__(self, k):
        self.k = k; self.es = ExitStack()

    def __enter__(self):
        self.es.__enter__()
        return self.es

    def __exit__(self, *a):
        if a[0] is None:
            self.k.barrier()
        return self.es.__exit__(*a)


def _t5_bucket(n):
    n = np.maximum(n, 0)
    rel = np.log(np.maximum(n, 1).astype(np.float32) / np.float32(16)) / np.float32(math.log(128 / 16))
    large = 16 + (rel * np.float32(16)).astype(np.int32)
    large = np.minimum(large, 31)
    return np.where(n < 16, n, large)


def _consts():
    bf = ml_dtypes.bfloat16
    c = {}
    c["ident_bf"] = np.eye(128, dtype=np.float32).astype(bf)
    c["ident_f"] = np.eye(128, dtype=np.float32)
    i = np.arange(128)[:, None]; j = np.arange(128)[None, :]
    c["caus"] = np.where(i <= j, 0.0, NEG).astype(bf)
    c["w4m"] = np.where(i > j, 0.0, NEG).astype(bf)
    c["stri"] = (i < j).astype(np.float32).astype(bf)
    up = np.zeros((128, 4, 512), np.float32)
    for jo in range(4):
        for to in range(4):
            if jo < to:
                up[:, jo, to * 128:(to + 1) * 128] = 1.0
            elif jo == to:
                up[:, jo, to * 128:(to + 1) * 128] = (i <= j)
    c["upat"] = up
    m = np.arange(VLEN) - VOFF
    oh = np.zeros((33, VLEN), np.float32)
    bk = _t5_bucket(m)
    for idx in range(VLEN):
        if m[idx] >= 0:
            oh[bk[idx], idx] = 1.0
        else:
            oh[32, idx] = 1.0
    c["ohv"] = oh
    sel31 = np.zeros((32, 32), np.float32); sel31[31, :] = 1.0
    c["sel31"] = sel31
    am = np.zeros((128, 32), np.float32)
    for jj in range(32):
        for a in range(4):
            for b in range(2):
                cc = jj * 4 + a - b
                if 0 <= cc < 127:
                    am[cc, jj] += 1.0
    c["amat"] = am.astype(bf)
    em = np.zeros((32, 2048), np.float32)
    for jj in range(32):
        em[jj, jj * 64:(jj + 1) * 64] = 1.0
    c["emat"] = em.astype(bf)
    t = np.arange(1024, 2048)
    blk = np.arange(32)[None, :]
    cur = (t // 64)[:, None]
    valid = (blk * 64 <= t[:, None])
    forced = (blk == 0) | (blk == cur) | (blk == cur - 1)
    add = np.where(valid, np.where(forced, 1e4, 0.0), -1e30).astype(np.float32)
    c["selvalid"] = valid.astype(np.float32).reshape(8, 128, 32).transpose(1, 0, 2).copy()
    c["seladd"] = add.reshape(8, 128, 32).transpose(1, 0, 2).copy()
    c["ones_d"] = np.ones((3, 8, 2048), np.float32).astype(bf)
    c["iota_p"] = np.arange(128, dtype=np.float32).reshape(128, 1)
    return c


CONST_SPECS = [("ident_bf", [128, 128], BF16), ("ident_f", [128, 128], F32), ("caus", [128, 128], BF16),
               ("w4m", [128, 128], BF16), ("stri", [128, 128], BF16), ("upat", [128, 4, 512], F32),
               ("ohv", [33, VLEN], F32), ("sel31", [32, 32], F32), ("amat", [128, 32], BF16),
               ("emat", [32, 2048], BF16), ("selvalid", [128, 8, 32], F32), ("seladd", [128, 8, 32], F32),
               ("ones_d", [3, 8, 2048], BF16), ("iota_p", [128, 1], F32)]

PARAM_SPECS = [("attn_norm_w", [1, 2048]), ("w_in", [1, 2048, IN_COLS]), ("cmp_pe_k", [1, 32, 64]),
               ("cmp_pe_v", [1, 32, 64]), ("cmp_k_w1", [1, 2048, 128]), ("cmp_k_w2", [1, 128, 64]),
               ("cmp_v_w1", [1, 2048, 128]), ("cmp_v_w2", [1, 128, 64]), ("rel_bias_table", [32, 16]),
               ("fox_forget_b", [1, 16]), ("nsa_out_norm_w", [1, 1024]), ("fox_out_norm_w", [1, 1024]),
               ("w_out", [1, 2048, 2048]), ("ffn_norm_w", [1, 2048]), ("router_group_w", [1, 2048, 4]),
               ("router_group_b", [1, 4]), ("router_expert_w", [1, 2048, 32]), ("router_expert_b", [1, 32]),
               ("expert_w_gate", [32, 2048, 512]), ("expert_w_up", [32, 2048, 512]),
               ("expert_w_down", [32, 512, 2048]), ("final_norm_w", [2048])]


def build_nc(stop_after=None, debug=False):
    nc = bass.Bass("TRN2", target_bir_lowering=False)
    P = {}
    x_d = Buf(nc.dram_tensor("x", [T, D], F32, kind="ExternalInput"), "x")
    for name, shape in PARAM_SPECS:
        P[name] = Buf(nc.dram_tensor(name, shape, F32, kind="ExternalInput"), name)
    CD = {}
    for name, shape, dt in CONST_SPECS:
        CD[name] = Buf(nc.dram_tensor("c_" + name, shape, dt, kind="ExternalInput"), name)
    out_d = Buf(nc.dram_tensor("out", [T, D], F32, kind="ExternalOutput"), "out")
    dbg = {}
    V, S, G, TE = nc.vector, nc.scalar, nc.gpsimd, nc.tensor

    with ExitStack() as es:
        k = K(nc, es)
        pe, act, dve, pool, sp = k.pe, k.act, k.dve, k.pool, k.sp

        o_scr = k.dram("o_scr", [T, 2048], BF16)
        vscr = k.dram("vscr", [16, VLEN], BF16)
        brd = k.dram("brd", [16 * 128, VLEN], BF16)
        cscr = k.dram("cscr", [16, 6, T], BF16)
        h1_scr = k.dram("h1_scr", [T, D], F32)
        hn_scr = k.dram("hn_scr", [T, D], BF16)
        xslot = k.dram("xslot", [NSLOT * 128, D], BF16)
        yslot = k.dram("yslot", [NSLOT * 128, D], F32)
        if debug:
            for nm, shp, dt in [("d_o", [T, 2048], BF16), ("d_h1", [T, D], F32)]:
                dbg[nm] = Buf(nc.dram_tensor(nm, shp, dt, kind="ExternalOutput"), nm)

        wbf = {"g": k.dram("wbf_g", [32, 2048, 512], BF16), "u": k.dram("wbf_u", [32, 2048, 512], BF16),
               "d": k.dram("wbf_d", [32, 512, 2048], BF16)}
        pre_sems = ([[es.enter_context(nc.semaphore(f"presem{i}")), 0] for i in range(6)], [0])
        pre_list = []
        pre_views = []
        for e_ in range(32):
            for key_, pn_ in [("g", "expert_w_gate"), ("u", "expert_w_up"), ("d", "expert_w_down")]:
                vw = k.view(wbf[key_], f"wbf_{key_}{e_}")
                pre_views.append(vw)
                pre_list.append((vw, wbf[key_].t[e_], P[pn_], P[pn_].t[e_]))
        pre_pos = [0]

        def precast_step(n):
            for _ in range(n):
                if pre_pos[0] >= len(pre_list):
                    return
                vw, dst_ap, sb_, src_ap = pre_list[pre_pos[0]]; pre_pos[0] += 1
                k.dma(pool, dst_ap, src_ap, reads=[sb_], writes=[vw], sems=pre_sems)

        o_views = []

        def o_store(dst_ap, src_b, src_ap):
            v_ = k.view(o_scr, "o_st")
            k.dma(sp, dst_ap, src_ap, reads=[src_b], writes=[v_])
            o_views.append(v_)

        def done(bufs):
            k.finish(bufs)
            print("ops", k.nops, "waits", k.nwaits, "dmas", k.ndma, flush=True)
            return nc

        PB = [k.ps(f"pb{i}", [128, 512], F32) for i in range(7)]
        PT = k.ps("pt_bf", [128, 1024], BF16)

        ident_bf = k.sb("ident_bf", [128, 128], BF16)
        ident_f = k.sb("ident_f", [128, 128], F32)
        caus = k.sb("caus", [128, 128], BF16)
        for b_, nm in [(ident_bf, "ident_bf"), (ident_f, "ident_f"), (caus, "caus")]:
            k.dma(sp, b_[:], CD[nm][:], reads=[CD[nm]], writes=[b_])
        rstd_tmp = k.sb("rstd_tmp", [128, 4], F32)
        ssn = k.sb("ssn", [128, NT], F32)
        ssf = k.sb("ssf", [128, NT], F32)
        sstmp = k.sb("sstmp", [128, 2], F32)
        eps_t = k.sb("eps_t", [128, 1], F32)
        junk = k.sb("junk", [128, 2048], BF16)
        k.op(dve, lambda: V.memset(ssn[:], 0.0), writes=[ssn])
        k.op(dve, lambda: V.memset(ssf[:], 0.0), writes=[ssf])
        k.op(dve, lambda: V.memset(eps_t[:], 1e-6), writes=[eps_t])

        def evac(E, out_ap, in_ap, reads, writes, scale=None):
            if E is act:
                if scale is None:
                    return k.op(act, lambda: S.copy(out=out_ap, in_=in_ap), reads=reads, writes=writes)
                return k.op(act, lambda: S.activation(out=out_ap, in_=in_ap, func=AF.Copy, scale=scale), reads=reads, writes=writes)
            if scale is None:
                return k.op(dve, lambda: V.tensor_copy(out=out_ap, in_=in_ap), reads=reads, writes=writes)
            return k.op(dve, lambda: V.tensor_scalar(out=out_ap, in0=in_ap, scalar1=scale, scalar2=None, op0=ALU.mult),
                        reads=reads, writes=writes)

        def mm(out_b, out_ap, l_b, l_ap, r_b, r_ap, start, stop, extra_reads=()):
            return k.op(pe, lambda: TE.matmul(out_ap, l_ap, r_ap, start=start, stop=stop),
                        reads=[l_b, r_b] + list(extra_reads), writes=[out_b])

        def transpose(out_b, out_ap, in_b, in_ap, id_b, kp=128):
            return k.op(pe, lambda: TE.transpose(out_ap, in_ap, id_b[0:kp, 0:kp]), reads=[in_b, id_b], writes=[out_b])

        def rstd_from_ss(ss_ap, ss_b, out_ap, out_b, n):
            k.op(act, lambda: S.activation(out=rstd_tmp[:, 0:1], in_=ss_ap, func=AF.Ln, scale=1.0 / n, bias=eps_t[:, 0:1]),
                 reads=[ss_b, eps_t], writes=[rstd_tmp])
            k.op(act, lambda: S.activation(out=out_ap, in_=rstd_tmp[:, 0:1], func=AF.Exp, scale=-0.5),
                 reads=[rstd_tmp], writes=[out_b])

        def tt(out_b, out_ap, a_b, a_ap, b_b, b_ap, op, E=None):
            return k.op(E or dve, lambda: (E or dve).e.tensor_tensor(out=out_ap, in0=a_ap, in1=b_ap, op=op), reads=[a_b, b_b], writes=[out_b])

        def ts(out_b, out_ap, a_b, a_ap, s1, op0, s2=None, op1=None, sreads=()):
            if op1 is None:
                return k.op(dve, lambda: V.tensor_scalar(out=out_ap, in0=a_ap, scalar1=s1, scalar2=None, op0=op0),
                            reads=[a_b] + list(sreads), writes=[out_b])
            return k.op(dve, lambda: V.tensor_scalar(out=out_ap, in0=a_ap, scalar1=s1, scalar2=s2, op0=op0, op1=op1),
                        reads=[a_b] + list(sreads), writes=[out_b])

        def stt(out_b, out_ap, a_b, a_ap, sc, b_b, b_ap, op0, op1, sreads=()):
            return k.op(dve, lambda: V.scalar_tensor_tensor(out=out_ap, in0=a_ap, scalar=sc, in1=b_ap, op0=op0, op1=op1),
                        reads=[a_b, b_b] + list(sreads), writes=[out_b])

        with Scope(k) as esA:
            xnT = k.sb("xnT", [128, 16, T], BF16, es=esA)
            with Scope(k) as e1:
                anw_bc = k.sb("anw_bc", [128, D], F32, es=e1)
                k.dma(sp, anw_bc[:], bass.AP(P["attn_norm_w"].t, 0, [[0, 128], [1, D]]), reads=[P["attn_norm_w"]], writes=[anw_bc])
                xb = [k.sb(f"xb{i}", [128, D], F32, es=e1) for i in range(2)]
                xn_tm = [k.sb(f"xn_tm{i}", [128, D], BF16, es=e1) for i in range(2)]
                ssx = k.sb("ssx", [128, NT], F32, es=e1)
                rsx = k.sb("rsx", [128, NT], F32, es=e1)
                for i in range(NT):
                    xs = xb[i % 2]; xt = xn_tm[i % 2]
                    k.dma(sp, xs[:], x_d[i * 128:(i + 1) * 128, :], reads=[x_d], writes=[xs])
                    k.op(act, lambda: S.activation(out=junk[:], in_=xs[:], func=AF.Square, accum_out=ssx[:, i:i + 1]),
                         reads=[xs], writes=[junk, ssx])
                    rstd_from_ss(ssx[:, i:i + 1], ssx, rsx[:, i:i + 1], rsx, D)
                    stt(xt, xt[:], xs, xs[:], rsx[:, i:i + 1], anw_bc, anw_bc[:], ALU.mult, ALU.mult, sreads=[rsx])
                    for c4 in range(4):
                        for cc in range(4):
                            c = c4 * 4 + cc
                            transpose(PT, PT[:, cc * 128:(cc + 1) * 128], xt, xt[:, c * 128:(c + 1) * 128], ident_bf)
                        evac(act if c4 % 2 else dve, xnT[:, c4 * 4:(c4 + 1) * 4, i * 128:(i + 1) * 128],
                             PT[:, 0:512].rearrange("p (c t) -> p c t", c=4), [PT], [xnT])

            if stop_after == "xn":
                k.dma(sp, dbg["d_o"].t[:].rearrange("(p c) t -> p c t", c=16), xnT[:], reads=[xnT], writes=[dbg["d_o"]])
                return done([dbg["d_o"]])
            with Scope(k) as e2:
                tab = k.sb("tab", [33, 16], F32, es=e2)
                sel31 = k.sb("sel31", [32, 32], F32, es=e2)
                ohv = k.sb("ohv", [33, VLEN], F32, es=e2)
                vsb = k.sb("vsb", [16, VLEN], BF16, es=e2)
                k.dma(sp, tab[0:32, :], P["rel_bias_table"][:], reads=[P["rel_bias_table"]], writes=[tab])
                k.dma(sp, sel31[:], CD["sel31"][:], reads=[CD["sel31"]], writes=[sel31])
                k.dma(sp, ohv[:], CD["ohv"][:], reads=[CD["ohv"]], writes=[ohv])
                mm(PB[0], PB[0][0:32, 0:16], sel31, sel31[:], tab, tab[0:32, :], True, True)
                tt(tab, tab[0:32, :], tab, tab[0:32, :], PB[0], PB[0][0:32, 0:16], ALU.subtract)
                k.op(dve, lambda: V.memset(tab[32:33, :], NEG), writes=[tab])
                for q in range(VLEN // 512):
                    pb = PB[q % 2]
                    mm(pb, pb[0:16, :], tab, tab[:], ohv, ohv[:, q * 512:(q + 1) * 512], True, True)
                    evac(dve, vsb[:, q * 512:(q + 1) * 512], pb[0:16, :], [pb], [vsb])
                k.dma(sp, vscr[:], vsb[:], reads=[vsb], writes=[vscr])
                for h in range(16):
                    k.dma(sp, brd[h * 128:(h + 1) * 128, :], bass.AP(vscr.t, h * VLEN, [[0, 128], [1, VLEN]]), reads=[vscr], writes=[brd])

            wst_box = [None]

            def cast(E, out_b, out_ap, in_b, in_ap):
                return k.op(E, lambda: E.e.tensor_copy(out=out_ap, in_=in_ap), reads=[in_b], writes=[out_b])

            def load_w(buf, c0, col0, ncols):
                wst = wst_box[0]
                stv = wst[:, 0:16 * ncols].rearrange("p (c n) -> p c n", c=16)
                src = P["w_in"].t[0, :, col0:col0 + ncols].rearrange("(c p) n -> p c n", p=128)
                k.dma(sp, stv, src, reads=[P["w_in"]], writes=[wst])
                cast(pool, buf, buf[:, :, c0:c0 + ncols], wst, stv)

            rot = [0]

            def proj_fm(wbuf, ncols, dst_b, dst_fn, scale):
                for tc in range(4):
                    pb = PB[rot[0] % 3]; rot[0] += 1
                    for c in range(16):
                        mm(pb, pb[0:ncols, :], wbuf, wbuf[:, c, 0:ncols], xnT, xnT[:, c, tc * 512:(tc + 1) * 512], c == 0, c == 15)
                    evac(act if rot[0] % 2 else dve, dst_fn(tc), pb[0:ncols, :], [pb], [dst_b], scale)

            with Scope(k) as eC:
                wst_box[0] = k.sb("wstC", [128, 16 * 16], F32, es=eC)
                wf = k.sb("wf", [128, 16, 16], BF16, es=eC)
                fb_bc = k.sb("fb_bc", [128, 16], F32, es=eC)
                logf = k.sb("logf", [128, NT, 16], F32, es=eC)
                upat = k.sb("upat", [128, 4, 512], F32, es=eC)
                onesf = k.sb("onesf", [128, 512], F32, es=eC)
                cT = k.sb("cT", [16, T], F32, es=eC)
                r1 = k.sb("r1", [16, T], F32, es=eC)
                parts = k.sb("parts", [16, 6, T], BF16, es=eC)
                ztmp = k.sb("ztmp", [128, 16], F32, es=eC)
                load_w(wf, 0, C_FF, 16)
                k.dma(sp, fb_bc[:], bass.AP(P["fox_forget_b"].t, 0, [[0, 128], [1, 16]]), reads=[P["fox_forget_b"]], writes=[fb_bc])
                k.dma(sp, upat[:], CD["upat"][:], reads=[CD["upat"]], writes=[upat])
                k.op(dve, lambda: V.memset(onesf[:], 1.0), writes=[onesf])
                if stop_after == "wf":
                    k.dma(sp, dbg["d_o"].t[0:128, 0:256], wf[:].rearrange("p a b -> p (a b)"), reads=[wf], writes=[dbg["d_o"]])
                    k.dma(pool, dbg["d_o"].t[128:256, 0:16], fb_bc[:], reads=[fb_bc], writes=[dbg["d_o"]])
                    return done([dbg["d_o"]])
                for i in range(NT):
                    pb = PB[i % 2]
                    for c in range(16):
                        mm(pb, pb[:, 0:16], xnT, xnT[:, c, i * 128:(i + 1) * 128], wf, wf[:, c, :], c == 0, c == 15)
                    tt(ztmp, ztmp[:], pb, pb[:, 0:16], fb_bc, fb_bc[:], ALU.add)
                    k.op(act, lambda: S.activation(out=ztmp[:], in_=ztmp[:], func=AF.Exp, scale=-1.0), reads=[ztmp], writes=[ztmp])
                    k.op(act, lambda: S.activation(out=ztmp[:], in_=ztmp[:], func=AF.Ln, scale=1.0, bias=1.0), reads=[ztmp], writes=[ztmp])
                    ts(logf, logf[:, i, :], ztmp, ztmp[:], -1.0, ALU.mult)
                for q in range(4):
                    pb = PB[q % 2]
                    n = 4 * q + 4
                    for j in range(n):
                        jo = j - 4 * q
                        if jo >= 0:
                            mm(pb, pb[0:16, :], logf, logf[:, j, :], upat, upat[:, jo, :], j == 0, j == n - 1)
                        else:
                            mm(pb, pb[0:16, :], logf, logf[:, j, :], onesf, onesf[:], j == 0, False)
                    evac(dve, cT[:, q * 512:(q + 1) * 512], pb[0:16, :], [pb], [cT])
                if stop_after == "lf":
                    k.dma(sp, dbg["d_h1"].t[0:128, 0:256], logf[:].rearrange("p a b -> p (a b)"), reads=[logf], writes=[dbg["d_h1"]])
                    k.dma(sp, dbg["d_h1"].t[128:144, :], cT[:], reads=[cT], writes=[dbg["d_h1"]])
                    return done([dbg["d_h1"]])
                k.op(dve, lambda: V.tensor_copy(out=parts[:, 0, :], in_=cT[:]), reads=[cT], writes=[parts])
                tt(r1, r1[:], cT, cT[:], parts, parts[:, 0, :], ALU.subtract)
                k.op(dve, lambda: V.tensor_copy(out=parts[:, 1, :], in_=r1[:]), reads=[r1], writes=[parts])
                tt(r1, r1[:], r1, r1[:], parts, parts[:, 1, :], ALU.subtract)
                k.op(dve, lambda: V.tensor_copy(out=parts[:, 2, :], in_=r1[:]), reads=[r1], writes=[parts])
                ts(parts, parts[:, 3:6, :], parts, parts[:, 0:3, :], -1.0, ALU.mult)
                k.dma(sp, cscr[:], parts[:], reads=[parts], writes=[cscr])

            if stop_after == "cs":
                k.dma(sp, dbg["d_o"].t[0:96, :].rearrange("(h k) t -> h k t", k=6), cscr[:], reads=[cscr], writes=[dbg["d_o"]])
                return done([dbg["d_o"]])
            with Scope(k) as eF:
                NH = 4
                wst_box[0] = k.sb("wstF", [128, 16 * 256], F32, es=eF)
                FQ = k.sb("FQ", [128, NH, T], BF16, es=eF)
                FK = k.sb("FK", [128, NH, T], BF16, es=eF)
                k.op(pool, lambda: G.memset(FQ[:], 0.0), writes=[FQ])
                k.op(pool, lambda: G.memset(FK[:], 0.0), writes=[FK])
                FV = k.sb("FV", [128, NT, NH, 65], BF16, es=eF)
                OJ = k.sb("OJ", [128, NT, NH * 64], BF16, es=eF)
                wq = [k.sb(f"wq{i}", [128, 16, 128], BF16, es=eF) for i in range(2)]
                wv = k.sb("wv", [128, 16, NH * 64], BF16, es=eF)
                pt_sb = [k.sb(f"pt_sb{i}", [128, 512], BF16, es=eF) for i in range(3)]
                rz = k.sb("rz", [128, 8], F32, es=eF)
                of32s = [k.sb(f"of32_{i}", [128, 64], F32, es=eF) for i in range(2)]
                FQh = [k.view(FQ, f"FQ{h}") for h in range(NH)]
                FKh = [k.view(FK, f"FK{h}") for h in range(NH)]
                FQc = k.view(FQ, "FQc"); FKc = k.view(FK, "FKc")
                for v_ in FQh + [FQc]:
                    v_.w = dict(FQ.w)
                for v_ in FKh + [FKc]:
                    v_.w = dict(FK.w)
                k.op(dve, lambda: V.memset(FV[:, :, :, 64:65], 1.0), writes=[FV])
                POb = PB[3:7]
                for fp in range(16 // NH):
                    h0 = fp * NH
                    for pj in range(NH // 2):
                        h = h0 + 2 * pj
                        for wb, col, DST, DSTh, sc in [(wq[0], C_FQ, FQ, FQh, 0.125), (wq[1], C_FK, FK, FKh, None)]:
                            load_w(wb, 0, col + h * 64, 128)
                            for tc in range(4):
                                pb = PB[rot[0] % 3]; rot[0] += 1
                                for c in range(16):
                                    mm(pb, pb[:, :], wb, wb[:, c, :], xnT, xnT[:, c, tc * 512:(tc + 1) * 512], c == 0, c == 15)
                                evac(act, DST[0:64, 2 * pj, tc * 512:(tc + 1) * 512], pb[0:64, :], [pb], [DSTh[2 * pj]], sc)
                                evac(dve, DST[64:128, 2 * pj + 1, tc * 512:(tc + 1) * 512], pb[64:128, :], [pb], [DSTh[2 * pj + 1]], sc)
                    load_w(wv, 0, C_FV + h0 * 64, NH * 64)
                    for i in range(NT):
                        pb = PB[rot[0] % 3]; rot[0] += 1
                        for c in range(16):
                            mm(pb, pb[:, 0:NH * 64], xnT, xnT[:, c, i * 128:(i + 1) * 128], wv, wv[:, c, :], c == 0, c == 15)
                        evac(act if i % 2 else dve, FV[:, i, :, 0:64], pb[:, 0:NH * 64].rearrange("p (h d) -> p h d", h=NH), [pb], [FV])
                    for par, r0 in [(0, 64), (1, 0)]:
                        hs = slice(h0 + par, h0 + NH, 2); ls = slice(par, NH, 2); nh2 = NH // 2
                        k.dma(sp, FQ[r0:r0 + 3, ls, :], cscr.t[hs, 0:3, :].rearrange("h k t -> k h t"), reads=[cscr], writes=[FQc])
                        k.dma(sp, FQ[r0 + 3:r0 + 6, ls, :], CD["ones_d"].t[:, 0:nh2, :], reads=[CD["ones_d"]], writes=[FQc])
                        k.dma(sp, FK[r0:r0 + 3, ls, :], CD["ones_d"].t[:, 0:nh2, :], reads=[CD["ones_d"]], writes=[FKc])
                        k.dma(sp, FK[r0 + 3:r0 + 6, ls, :], cscr.t[hs, 3:6, :].rearrange("h k t -> k h t"), reads=[cscr], writes=[FKc])
                    recs = [(hl, qc, kb) for hl in range(NH) for qc in range(4) for kb in range(4 * qc + 4)]

                    def geom(rec):
                        hl, qc, kb = rec
                        dg = kb - 4 * qc
                        q0 = qc * 512 + (dg * 128 if dg > 0 else 0)
                        return hl, qc, kb, dg, q0, (qc + 1) * 512 - q0

                    def fox_qk(rec, slot):
                        hl, qc, kb, dg, q0, n = geom(rec)
                        pb = PB[slot % 3]
                        mm(pb, pb[:, 0:n], FKh[hl], FK[:, hl, kb * 128:(kb + 1) * 128], FQh[hl], FQ[:, hl, q0:q0 + n],
                           True, dg < 0, extra_reads=[FQc, FKc])
                        if dg >= 0:
                            mm(pb, pb[:, 0:128], ident_bf, ident_bf[:], caus, caus[:], False, True)

                    def fox_pv(rec, slot):
                        hl, qc, kb, dg, q0, n = geom(rec)
                        pb = PB[slot % 3]; ptb = pt_sb[slot % 3]
                        k.op(act, lambda: S.activation(out=ptb[:, 0:n], in_=pb[:, 0:n], func=AF.Exp), reads=[pb], writes=[ptb])
                        j0 = max(dg, 0)
                        for jq in range(j0, 4):
                            po = POb[jq]
                            cs = (jq - j0) * 128
                            mm(po, po[:, 0:65], ptb, ptb[:, cs:cs + 128], FV, FV[:, kb, hl, :], kb == 0, kb == 4 * qc + jq)
                        if kb == 4 * qc + 3:
                            for jq in range(4):
                                po = POb[jq]; i = qc * 4 + jq
                                k.op(dve, lambda: V.reciprocal(out=rz[:, jq:jq + 1], in_=po[:, 64:65]), reads=[po], writes=[rz])
                                of = of32s[jq % 2]
                                ts(of, of[:], po, po[:, 0:64], rz[:, jq:jq + 1], ALU.mult, sreads=[rz])
                                k.op(act, lambda: S.activation(out=junk[:, 0:64], in_=of[:], func=AF.Square, accum_out=sstmp[:, 0:1]),
                                     reads=[of], writes=[junk, sstmp])
                                tt(ssf, ssf[:, i:i + 1], ssf, ssf[:, i:i + 1], sstmp, sstmp[:, 0:1], ALU.add)
                                k.op(dve, lambda: V.tensor_copy(out=OJ[:, i, hl * 64:(hl + 1) * 64], in_=of[:]), reads=[of], writes=[OJ])
                            if qc % 2 == 1:
                                precast_step(1)

                    fox_qk(recs[0], 0)
                    for t_ in range(len(recs)):
                        if t_ + 1 < len(recs):
                            fox_qk(recs[t_ + 1], t_ + 1)
                        fox_pv(recs[t_], t_)
                    for i in range(NT):
                        c0 = 1024 + h0 * 64
                        o_store(o_scr[i * 128:(i + 1) * 128, c0:c0 + NH * 64], OJ, OJ[:, i, :])

            if stop_after == "fox":
                k.dma(sp, dbg["d_o"][:], o_scr[:], reads=o_views, writes=[dbg["d_o"]])
                return done([dbg["d_o"]])

            with Scope(k) as eN:
                wst_box[0] = k.sb("wstN", [128, 16 * 144], F32, es=eN)
                QT = k.sb("QT", [128, 4, T], BF16, es=eN)
                KTS = k.sb("KTS", [128, T], BF16, es=eN)
                KTW = k.sb("KTW", [128, T], BF16, es=eN)
                for b_ in (QT, KTS, KTW):
                    k.op(pool, lambda: G.memset(b_[:], 0.0), writes=[b_])
                CK = k.sb("CK", [64, T], BF16, es=eN)
                CV = k.sb("CV", [64, T], BF16, es=eN)
                VS = k.sb("VS", [128, NT, 65], BF16, es=eN)
                VW = k.sb("VW", [128, NT, 65], BF16, es=eN)
                GT = k.sb("GT", [128, NT, 12], F32, es=eN)
                W1K = k.sb("W1K", [64, 32, 128], BF16, es=eN)
                W1V = k.sb("W1V", [64, 32, 128], BF16, es=eN)
                W2K = k.sb("W2K", [128, 64], BF16, es=eN)
                W2V = k.sb("W2V", [128, 64], BF16, es=eN)
                pe_ld = k.sb("pe_ld", [32, 128], F32, es=eN)
                PET = k.sb("PET", [64, 2, 32], BF16, es=eN)
                b1 = k.sb("b1", [128, 2], F32, es=eN)
                HK = k.sb("HK", [128, 128], BF16, es=eN)
                HV = k.sb("HV", [128, 128], BF16, es=eN)
                KC = k.sb("KC", [128, 128], BF16, es=eN)
                VCX = k.sb("VCX", [128, 97], BF16, es=eN)
                B0 = k.sb("B0", [128, 4, 128], BF16, es=eN)
                B1 = k.sb("B1", [128, 4, 128], BF16, es=eN)
                W4X = k.sb("W4X", [128, 4, 128], BF16, es=eN)
                CB = [k.sb(f"CB{i}", [128, 4, 128], BF16, es=eN) for i in range(2)]
                NM4 = [k.sb(f"NM4{i}", [128, 4, 128], BF16, es=eN) for i in range(2)]
                emat = k.sb("emat", [128, T], BF16, es=eN)
                for b_ in NM4 + [emat]:
                    k.op(pool, lambda: G.memset(b_[:], 0.0), writes=[b_])
                selvalid = k.sb("selvalid", [128, 8, 32], F32, es=eN)
                seladd = k.sb("seladd", [128, 8, 32], F32, es=eN)
                OJn = k.sb("OJn", [128, NT, 256], BF16, es=eN)
                OA = k.sb("OA", [128, 4, 64], F32, es=eN)
                wqn = [k.sb(f"wqn{i}", [128, 16, 64], BF16, es=eN) for i in range(2)]
                wtm = k.sb("wtm", [128, 16, 144], BF16, es=eN)
                ptn = [k.sb(f"ptn{i}", [128, 512], BF16, es=eN) for i in range(3)]
                sm = k.sb("sm", [128, 64], F32, es=eN)
                imp = k.sb("imp", [128, 32], F32, es=eN)
                score = k.sb("score", [128, 32], F32, es=eN)
                score2 = k.sb("score2", [128, 32], F32, es=eN)
                mx = k.sb("mx", [128, 16], F32, es=eN)
                negmb = k.sb("negmb", [128, 32], BF16, es=eN)
                gtmp = k.sb("gtmp", [128, 12], F32, es=eN)
                k.dma(sp, emat[0:32, :], CD["emat"][:], reads=[CD["emat"]], writes=[emat])
                for b_, nm in [(selvalid, "selvalid"), (seladd, "seladd")]:
                    k.dma(sp, b_[:], CD[nm][:], reads=[CD[nm]], writes=[b_])
                k.dma(sp, W4X[:, 0, :], CD["w4m"][:], reads=[CD["w4m"]], writes=[W4X])
                for r in range(1, 4):
                    k.op(dve, lambda: V.tensor_copy(out=W4X[:, r, :], in_=W4X[:, 0, :]), reads=[W4X], writes=[W4X])
                wstn = wst_box[0]
                for (Wd, pn) in [(W1K, "cmp_k_w1"), (W1V, "cmp_v_w1")]:
                    for hf in range(2):
                        stv = wstn[0:64, 0:2048].rearrange("p (l h) -> p l h", l=16)
                        k.dma(sp, stv, P[pn].t[0, hf * 1024:(hf + 1) * 1024, :].rearrange("(l d) h -> d l h", d=64), reads=[P[pn]], writes=[wstn])
                        cast(pool, Wd, Wd[:, hf * 16:(hf + 1) * 16, :], wstn, stv)
                for (Wd, pn) in [(W2K, "cmp_k_w2"), (W2V, "cmp_v_w2")]:
                    k.dma(sp, wstn[:, 0:64], P[pn].t[0], reads=[P[pn]], writes=[wstn])
                    cast(pool, Wd, Wd[:], wstn, wstn[:, 0:64])
                k.dma(sp, pe_ld[:, 0:64], P["cmp_pe_k"].t[0], reads=[P["cmp_pe_k"]], writes=[pe_ld])
                k.dma(sp, pe_ld[:, 64:128], P["cmp_pe_v"].t[0], reads=[P["cmp_pe_v"]], writes=[pe_ld])
                for kv in range(2):
                    transpose(PB[0], PB[0][0:64, 0:32], pe_ld, pe_ld[:, kv * 64:(kv + 1) * 64], ident_f, kp=32)
                    evac(dve, PET[:, kv, :], PB[0][0:64, 0:32], [PB[0]], [PET])
                    W1 = W1K if kv == 0 else W1V
                    for l in range(32):
                        mm(PB[1], PB[1][:, 0:1], W1, W1[:, l, :], PET, PET[:, kv, l:l + 1], l == 0, l == 31)
                    evac(dve, b1[:, kv:kv + 1], PB[1][:, 0:1], [PB[1]], [b1])
                k.op(dve, lambda: V.memset(VS[:, :, 64:65], 1.0), writes=[VS])
                k.op(dve, lambda: V.memset(VW[:, :, 64:65], 1.0), writes=[VW])
                k.op(dve, lambda: V.memset(KC[:], 0.0), writes=[KC])
                k.op(dve, lambda: V.memset(VCX[:], 0.0), writes=[VCX])
                k.op(dve, lambda: V.memset(VCX[:, 64:65], 1.0), writes=[VCX])
                k.dma(sp, VCX[:, 65:97], CD["amat"][:], reads=[CD["amat"]], writes=[VCX])
                POb = PB[3:7]

                if stop_after == "nsa0":
                    k.dma(sp, dbg["d_o"][:], o_scr[:], reads=o_views, writes=[dbg["d_o"]])
                    return done([dbg["d_o"]])
                for g in range(4):
                    for r in range(4):
                        wb = wqn[r % 2]
                        load_w(wb, 0, C_NQ + (4 * g + r) * 64, 64)
                        proj_fm(wb, 64, QT, lambda tc: QT[0:64, r, tc * 512:(tc + 1) * 512], 0.125)
                    for ii, (col, dst) in enumerate([(C_KSLC, KTS), (C_KWIN, KTW), (C_KCMP, CK), (C_VCMP, CV)]):
                        wb = wqn[ii % 2]
                        load_w(wb, 0, col + g * 64, 64)
                        proj_fm(wb, 64, dst, lambda tc: dst[0:64, tc * 512:(tc + 1) * 512], None)
                    if os.environ.get("NSA_SKIP") == "qk":
                        k.dma(sp, dbg["d_o"][:], o_scr[:], reads=o_views, writes=[dbg["d_o"]])
                        return done([dbg["d_o"]])
                    wstn_ = wst_box[0]
                    stv_all = wstn_[:, 0:16 * 144].rearrange("p (c n) -> p c n", c=16)
                    for (c0_, col_, n_) in [(0, C_VSLC + g * 64, 64), (64, C_VWIN + g * 64, 64), (128, C_GATE + g * 12, 16)]:
                        k.dma(sp, stv_all[:, :, c0_:c0_ + n_], P["w_in"].t[0, :, col_:col_ + n_].rearrange("(c p) n -> p c n", p=128),
                              reads=[P["w_in"]], writes=[wstn_])
                    cast(pool, wtm, wtm[:], wstn_, stv_all)
                    for i in range(NT):
                        pb = PB[rot[0] % 3]; rot[0] += 1
                        for c in range(16):
                            mm(pb, pb[:, 0:144], xnT, xnT[:, c, i * 128:(i + 1) * 128], wtm, wtm[:, c, :], c == 0, c == 15)
                        evac(dve, VS[:, i, 0:64], pb[:, 0:64], [pb], [VS])
                        evac(dve, VW[:, i, 0:64], pb[:, 64:128], [pb], [VW])
                        evac(dve, gtmp[:], pb[:, 128:140], [pb], [gtmp])
                        k.op(act, lambda: S.activation(out=gtmp[:], in_=gtmp[:], func=AF.Exp, scale=-1.0), reads=[gtmp], writes=[gtmp])
                        ts(gtmp, gtmp[:], gtmp, gtmp[:], 1.0, ALU.add)
                        k.op(dve, lambda: V.reciprocal(out=GT[:, i, :], in_=gtmp[:]), reads=[gtmp], writes=[GT])
                    if os.environ.get("NSA_SKIP") == "proj":
                        k.dma(sp, dbg["d_o"][:], o_scr[:], reads=o_views, writes=[dbg["d_o"]])
                        return done([dbg["d_o"]])
                    for kv, (SRC, W1, H) in enumerate([(CK, W1K, HK), (CV, W1V, HV)]):
                        pb = PB[rot[0] % 3]; rot[0] += 1
                        for l in range(32):
                            mm(pb, pb[:, 0:127], W1, W1[:, l, :], SRC, SRC[:, l:l + 2017:16], l == 0, l == 31)
                        k.op(act, lambda: S.activation(out=H[:, 0:127], in_=pb[:, 0:127], func=AF.Silu, bias=b1[:, kv:kv + 1]),
                             reads=[pb, b1], writes=[H])
                    pb = PB[rot[0] % 3]; rot[0] += 1
                    mm(pb, pb[0:64, 0:127], W2K, W2K[:], HK, HK[:, 0:127], True, True)
                    evac(dve, KC[0:64, 0:127], pb[0:64, 0:127], [pb], [KC])
                    pb = PB[rot[0] % 3]; rot[0] += 1
                    mm(pb, pb[0:127, 0:64], HV, HV[:, 0:127], W2V, W2V[:], True, True)
                    evac(dve, VCX[0:127, 0:64], pb[0:127, 0:64], [pb], [VCX])
                    base = (4 * g) * 128 * VLEN + VOFF
                    if os.environ.get("NSA_SKIP") == "cmp":
                        k.dma(sp, dbg["d_o"][:], o_scr[:], reads=o_views, writes=[dbg["d_o"]])
                        return done([dbg["d_o"]])
                    k.dma(sp, B0[:], bass.AP(brd.t, base, [[VLEN - 1, 128], [128 * VLEN, 4], [1, 128]]), reads=[brd], writes=[B0])
                    k.dma(sp, B1[:], bass.AP(brd.t, base + 128, [[VLEN - 1, 128], [128 * VLEN, 4], [1, 128]]), reads=[brd], writes=[B1])

                    if stop_after == "nsa1":
                        k.dma(sp, dbg["d_o"][:], o_scr[:], reads=o_views, writes=[dbg["d_o"]])
                        return done([dbg["d_o"]])

                    def cb_load(qb):
                        cb = CB[qb % 2]
                        k.dma(sp, cb[:], bass.AP(brd.t, base + 128 * qb - 31, [[VLEN - 16, 128], [128 * VLEN, 4], [1, 128]]), reads=[brd], writes=[cb])

                    def n_qk(rec, slot):
                        kind, qb, kb = rec
                        pb = PB[slot % 3]
                        qap = QT[:, :, qb * 128:(qb + 1) * 128]
                        if kind == "cmp":
                            if qb + 1 < NT:
                                cb_load(qb + 1)
                            cb = CB[qb % 2]
                            mm(pb, pb[:, :], KC, KC[:], QT, qap, True, False)
                            mm(pb, pb[:, :], ident_bf, ident_bf[:], cb, cb[:], False, True)
                            return
                        sel = kind == "slc"
                        KT = KTS if sel else KTW
                        extras = []
                        if kb == qb:
                            extras.append((ident_bf, ident_bf[:], B0, B0[:]))
                        elif kb == qb - 1:
                            extras.append((ident_bf, ident_bf[:], B1, B1[:]))
                        if (not sel) and kb == qb - 4:
                            extras.append((ident_bf, ident_bf[:], W4X, W4X[:]))
                        if sel and qb >= 8:
                            nm4 = NM4[qb % 2]
                            extras.append((emat, emat[:, kb * 128:(kb + 1) * 128], nm4, nm4[:]))
                        mm(pb, pb[:, :], KT, KT[:, kb * 128:(kb + 1) * 128], QT, qap, True, len(extras) == 0)
                        for ei, (lb, la, rb, ra) in enumerate(extras):
                            mm(pb, pb[:, :], lb, la, rb, ra, False, ei == len(extras) - 1)

                    def n_pv(rec, slot):
                        kind, qb, kb = rec
                        pb = PB[slot % 3]; ptb = ptn[slot % 3]
                        k.op(act, lambda: S.activation(out=ptb[:], in_=pb[:], func=AF.Exp), reads=[pb], writes=[ptb])
                        if kind == "cmp":
                            po = PB[(slot + 1) % 3]
                            for r in range(4):
                                mm(po, po[:, r * 97:(r + 1) * 97], ptb, ptb[:, r * 128:(r + 1) * 128], VCX, VCX[:], True, True)
                            ts(sm, sm[:, 0:4], po, po[:, 64:64 + 97 * 3 + 1:97], 1e-30, ALU.max)
                            k.op(dve, lambda: V.reciprocal(out=sm[:, 4:8], in_=sm[:, 0:4]), reads=[sm], writes=[sm])
                            tt(sm, sm[:, 8:12], sm, sm[:, 4:8], GT, GT[:, qb, 0:12:3], ALU.mult)
                            for r in range(4):
                                ts(OA, OA[:, r, :], po, po[:, r * 97:r * 97 + 64], sm[:, 8 + r:9 + r], ALU.mult, sreads=[sm])
                            if qb >= 8:
                                ts(imp, imp[:], po, po[:, 65:97], sm[:, 4:5], ALU.mult, sreads=[sm])
                                for r in range(1, 4):
                                    stt(imp, imp[:], po, po[:, r * 97 + 65:r * 97 + 97], sm[:, 4 + r:5 + r], imp, imp[:], ALU.mult, ALU.add, sreads=[sm])
                                tt(score, score[:], imp, imp[:], selvalid, selvalid[:, qb - 8, :], ALU.mult)
                                tt(score, score[:], score, score[:], seladd, seladd[:, qb - 8, :], ALU.add)
                                k.op(dve, lambda: V.max(out=mx[:, 0:8], in_=score[:]), reads=[score], writes=[mx])
                                k.op(dve, lambda: V.match_replace(out=score2[:], in_to_replace=mx[:, 0:8], in_values=score[:], imm_value=-3.0e38),
                                     reads=[score, mx], writes=[score2])
                                k.op(dve, lambda: V.max(out=mx[:, 8:16], in_=score2[:]), reads=[score2], writes=[mx])
                                ts(negmb, negmb[:], score, score[:], mx[:, 15:16], ALU.is_lt, NEG, ALU.mult, sreads=[mx])
                                transpose(PT, PT[0:32, 0:128], negmb, negmb[:], ident_bf)
                                nm4 = NM4[qb % 2]
                                for r in range(4):
                                    evac(dve if r % 2 else act, nm4[0:32, r, :], PT[0:32, 0:128], [PT], [nm4])
                            return
                        sel = kind == "slc"
                        VT = VS if sel else VW
                        kb_lo = 0 if sel else max(0, qb - 4)
                        for r in range(4):
                            mm(POb[r], POb[r][:, 0:65], ptb, ptb[:, r * 128:(r + 1) * 128], VT, VT[:, kb, :], kb == kb_lo, kb == qb)
                        if kb == qb:
                            gate_off = 1 if sel else 2
                            o0 = 16 if sel else 24
                            for r in range(4):
                                k.op(dve, lambda: V.reciprocal(out=sm[:, o0 + r:o0 + r + 1], in_=POb[r][:, 64:65]), reads=[POb[r]], writes=[sm])
                            tt(sm, sm[:, o0 + 4:o0 + 8], sm, sm[:, o0:o0 + 4], GT, GT[:, qb, gate_off:12:3], ALU.mult)
                            for r in range(4):
                                stt(OA, OA[:, r, :], POb[r], POb[r][:, 0:64], sm[:, o0 + 4 + r:o0 + 5 + r], OA, OA[:, r, :], ALU.mult, ALU.add, sreads=[sm])
                            if sel:
                                k.op(act, lambda: S.activation(out=junk[:, 0:256], in_=OA[:].rearrange("p r d -> p (r d)"), func=AF.Square,
                                                               accum_out=sstmp[:, 1:2]), reads=[OA], writes=[junk, sstmp])
                                tt(ssn, ssn[:, qb:qb + 1], ssn, ssn[:, qb:qb + 1], sstmp, sstmp[:, 1:2], ALU.add)
                                k.op(dve, lambda: V.tensor_copy(out=OJn[:, qb, :], in_=OA[:].rearrange("p r d -> p (r d)")), reads=[OA], writes=[OJn])
                                precast_step(1)

                    recs = []
                    for qb in range(NT):
                        recs.append(("cmp", qb, 0))
                        recs += [("win", qb, kb) for kb in range(max(0, qb - 4), qb + 1)]
                        recs += [("slc", qb, kb) for kb in range(0, qb + 1)]
                    slots = []
                    sl_ = 0
                    for rec in recs:
                        slots.append(sl_)
                        sl_ += 2 if rec[0] == "cmp" else 1
                    cb_load(0)
                    n_qk(recs[0], slots[0])
                    for t_ in range(len(recs)):
                        if t_ + 1 < len(recs):
                            n_qk(recs[t_ + 1], slots[t_ + 1])
                        n_pv(recs[t_], slots[t_])
                    for i in range(NT):
                        o_store(o_scr[i * 128:(i + 1) * 128, g * 256:(g + 1) * 256], OJn, OJn[:, i, :])

        if stop_after == "nsa":
            k.dma(sp, dbg["d_o"][:], o_scr[:], reads=o_views, writes=[dbg["d_o"]])
            return done([dbg["d_o"]])

        OH1a = k.sb("OH1a", [128, NT, 32], F32)
        OH2a = k.sb("OH2a", [128, NT, 32], F32)
        SELb = k.sb("SELb", [128, NT, 32], BF16)
        Wk = k.sb("Wk", [128, NT, 2], F32)
        ROWI = k.sb("ROWI", [128, NT, 2], I32)
        with Scope(k) as eO:
            WO = k.sb("WO", [128, 16, D], BF16, es=eO)
            onw_ld = k.sb("onw_ld", [16, 128], F32, es=eO)
            onw_col = k.sb("onw_col", [128, 16], F32, es=eO)
            rs_n = k.sb("rs_n", [128, NT], F32, es=eO)
            rs_f = k.sb("rs_f", [128, NT], F32, es=eO)
            fnw_bc = k.sb("fnw_bc", [128, D], F32, es=eO)
            WR = k.sb("WR", [128, 16, 36], F32, es=eO)
            RB = k.sb("RB", [128, 36], F32, es=eO)
            ot = [k.sb(f"ot{i}", [128, D], BF16, es=eO) for i in range(2)]
            oT = k.sb("oT", [128, 16, 128], BF16, es=eO)
            xs2 = [k.sb(f"xs2{i}", [128, D], F32, es=eO) for i in range(2)]
            h1t = k.sb("h1t", [128, D], F32, es=eO)
            hn32 = k.sb("hn32", [128, D], F32, es=eO)
            hnb = k.sb("hnb", [128, D], BF16, es=eO)
            hnT = k.sb("hnT", [128, 16, 128], F32, es=eO)
            lg = k.sb("lg", [128, 36], F32, es=eO)
            rt = k.sb("rt", [128, 64], F32, es=eO)
            elg = k.sb("elg", [128, 8], F32, es=eO)
            oh = k.sb("oh", [128, 24], F32, es=eO)
            wso = [k.sb(f"wso{i}", [128, D], F32, es=eO) for i in range(2)]
            for c in range(16):
                k.dma(sp, wso[c % 2][:], P["w_out"].t[0, c * 128:(c + 1) * 128, :], reads=[P["w_out"]], writes=[wso[c % 2]])
                k.op(pool, lambda: G.tensor_copy(out=WO[:, c, :], in_=wso[c % 2][:]), reads=[wso[c % 2]], writes=[WO])
            k.dma(sp, onw_ld[0:8, :], P["nsa_out_norm_w"].t[0].rearrange("(c p) -> c p", p=128), reads=[P["nsa_out_norm_w"]], writes=[onw_ld])
            k.dma(sp, onw_ld[8:16, :], P["fox_out_norm_w"].t[0].rearrange("(c p) -> c p", p=128), reads=[P["fox_out_norm_w"]], writes=[onw_ld])
            transpose(PB[0], PB[0][:, 0:16], onw_ld, onw_ld[:], ident_f, kp=16)
            evac(dve, onw_col[:], PB[0][:, 0:16], [PB[0]], [onw_col])
            k.dma(sp, fnw_bc[:], bass.AP(P["ffn_norm_w"].t, 0, [[0, 128], [1, D]]), reads=[P["ffn_norm_w"]], writes=[fnw_bc])
            with nc.allow_non_contiguous_dma(reason="small router weights"):
                k.dma(sp, WR[:, :, 0:4], P["router_group_w"].t[0].rearrange("(c p) n -> p c n", p=128), reads=[P["router_group_w"]], writes=[WR])
                k.dma(sp, WR[:, :, 4:36], P["router_expert_w"].t[0].rearrange("(c p) n -> p c n", p=128), reads=[P["router_expert_w"]], writes=[WR])
            k.dma(sp, RB[:, 0:4], bass.AP(P["router_group_b"].t, 0, [[0, 128], [1, 4]]), reads=[P["router_group_b"]], writes=[RB])
            k.dma(sp, RB[:, 4:36], bass.AP(P["router_expert_b"].t, 0, [[0, 128], [1, 32]]), reads=[P["router_expert_b"]], writes=[RB])
            for i in range(NT):
                rstd_from_ss(ssn[:, i:i + 1], ssn, rs_n[:, i:i + 1], rs_n, 1024)
                rstd_from_ss(ssf[:, i:i + 1], ssf, rs_f[:, i:i + 1], rs_f, 1024)
            for i in range(NT):
                o_t = ot[i % 2]; xs = xs2[i % 2]
                k.dma(sp, o_t[:], o_scr[i * 128:(i + 1) * 128, :], reads=o_views, writes=[o_t])
                k.dma(sp, xs[:], x_d[i * 128:(i + 1) * 128, :], reads=[x_d], writes=[xs])
                for c4 in range(4):
                    for cc in range(4):
                        c = c4 * 4 + cc
                        transpose(PT, PT[:, cc * 128:(cc + 1) * 128], o_t, o_t[:, c * 128:(c + 1) * 128], ident_bf)
                    for cc in range(4):
                        c = c4 * 4 + cc
                        ts(oT, oT[:, c, :], PT, PT[:, cc * 128:(cc + 1) * 128], onw_col[:, c:c + 1], ALU.mult, sreads=[onw_col])
                for dmb in range(4):
                    pn = PB[(2 * dmb) % 4]; pf = PB[(2 * dmb + 1) % 4]
                    for c in range(8):
                        mm(pn, pn[:], oT, oT[:, c, :], WO, WO[:, c, dmb * 512:(dmb + 1) * 512], c == 0, c == 7)
                    for c in range(8, 16):
                        mm(pf, pf[:], oT, oT[:, c, :], WO, WO[:, c, dmb * 512:(dmb + 1) * 512], c == 8, c == 15)
                    sl = slice(dmb * 512, (dmb + 1) * 512)
                    stt(h1t, h1t[:, sl], pn, pn[:], rs_n[:, i:i + 1], xs, xs[:, sl], ALU.mult, ALU.add, sreads=[rs_n])
                    stt(h1t, h1t[:, sl], pf, pf[:], rs_f[:, i:i + 1], h1t, h1t[:, sl], ALU.mult, ALU.add, sreads=[rs_f])
                k.dma(sp, h1_scr[i * 128:(i + 1) * 128, :], h1t[:], reads=[h1t], writes=[h1_scr])
                k.op(act, lambda: S.activation(out=junk[:], in_=h1t[:], func=AF.Square, accum_out=sstmp[:, 0:1]), reads=[h1t], writes=[junk, sstmp])
                rstd_from_ss(sstmp[:, 0:1], sstmp, rt[:, 0:1], rt, D)
                stt(hn32, hn32[:], h1t, h1t[:], rt[:, 0:1], fnw_bc, fnw_bc[:], ALU.mult, ALU.mult, sreads=[rt])
                k.op(act, lambda: S.copy(out=hnb[:], in_=hn32[:]), reads=[hn32], writes=[hnb])
                k.dma(sp, hn_scr[i * 128:(i + 1) * 128, :], hnb[:], reads=[hnb], writes=[hn_scr])
                for c4 in range(4):
                    pb = PB[4 + c4 % 2]
                    for cc in range(4):
                        c = c4 * 4 + cc
                        transpose(pb, pb[:, cc * 128:(cc + 1) * 128], hn32, hn32[:, c * 128:(c + 1) * 128], ident_f)
                    evac(act if c4 % 2 else dve, hnT[:, c4 * 4:(c4 + 1) * 4, :], pb[:].rearrange("p (c t) -> p c t", c=4), [pb], [hnT])
                pl = PB[6]
                for c in range(16):
                    mm(pl, pl[:, 0:36], hnT, hnT[:, c, :], WR, WR[:, c, :], c == 0, c == 15)
                tt(lg, lg[:], pl, pl[:, 0:36], RB, RB[:], ALU.add)
                k.op(dve, lambda: V.reduce_max(out=rt[:, 1:2], in_=lg[:, 0:4], axis=AX.X), reads=[lg], writes=[rt])
                ts(oh, oh[:, 0:4], lg, lg[:, 0:4], rt[:, 1:2], ALU.is_ge, sreads=[rt])
                ts(rt, rt[:, 2:3], rt, rt[:, 1:2], -1.0, ALU.mult)
                k.op(act, lambda: S.activation(out=rt[:, 8:12], in_=lg[:, 0:4], func=AF.Exp, bias=rt[:, 2:3], accum_out=rt[:, 3:4]),
                     reads=[lg, rt], writes=[rt])
                k.op(dve, lambda: V.reciprocal(out=rt[:, 4:5], in_=rt[:, 3:4]), reads=[rt], writes=[rt])
                ts(elg, elg[:], lg, lg[:, 4:12], oh[:, 0:1], ALU.mult, sreads=[oh])
                for gg in range(1, 4):
                    stt(elg, elg[:], lg, lg[:, 4 + gg * 8:12 + gg * 8], oh[:, gg:gg + 1], elg, elg[:], ALU.mult, ALU.add, sreads=[oh])
                k.op(dve, lambda: V.reduce_max(out=rt[:, 5:6], in_=elg[:], axis=AX.X), reads=[elg], writes=[rt])
                ts(oh, oh[:, 8:16], elg, elg[:], rt[:, 5:6], ALU.is_ge, sreads=[rt])
                stt(elg, elg[:], oh, oh[:, 8:16], -1.0e30, elg, elg[:], ALU.mult, ALU.add)
                k.op(dve, lambda: V.reduce_max(out=rt[:, 6:7], in_=elg[:], axis=AX.X), reads=[elg], writes=[rt])
                ts(oh, oh[:, 16:24], elg, elg[:], rt[:, 6:7], ALU.is_ge, sreads=[rt])
                tt(rt, rt[:, 7:8], rt, rt[:, 6:7], rt, rt[:, 5:6], ALU.subtract)
                k.op(act, lambda: S.activation(out=rt[:, 12:13], in_=rt[:, 7:8], func=AF.Exp), reads=[rt], writes=[rt])
                ts(rt, rt[:, 13:14], rt, rt[:, 12:13], 1.0, ALU.add)
                k.op(dve, lambda: V.reciprocal(out=rt[:, 14:15], in_=rt[:, 13:14]), reads=[rt], writes=[rt])
                tt(Wk, Wk[:, i, 0:1], rt, rt[:, 14:15], rt, rt[:, 4:5], ALU.mult)
                tt(rt, rt[:, 15:16], rt, rt[:, 14:15], rt, rt[:, 12:13], ALU.mult)
                tt(Wk, Wk[:, i, 1:2], rt, rt[:, 15:16], rt, rt[:, 4:5], ALU.mult)
                for gg in range(4):
                    ts(OH1a, OH1a[:, i, gg * 8:(gg + 1) * 8], oh, oh[:, 8:16], oh[:, gg:gg + 1], ALU.mult, sreads=[oh])
                    ts(OH2a, OH2a[:, i, gg * 8:(gg + 1) * 8], oh, oh[:, 16:24], oh[:, gg:gg + 1], ALU.mult, sreads=[oh])
                tt(SELb, SELb[:, i, :], OH1a, OH1a[:, i, :], OH2a, OH2a[:, i, :], ALU.add)

        if stop_after == "oproj":
            k.dma(sp, dbg["d_h1"][:], h1_scr[:], reads=[h1_scr], writes=[dbg["d_h1"]])
            return done([dbg["d_h1"]])

        IDXI = k.sb("IDXI", [128, NSLOT], I32)
        x_views = []
        with Scope(k) as eR:
            onesb = k.sb("onesb", [128, 128], BF16, es=eR)
            stri = k.sb("stri", [128, 128], BF16, es=eR)
            ncnt = k.sb("ncnt", [128, 32], F32, es=eR)
            tl = k.sb("tl", [128, 32], F32, es=eR)
            cA = k.sb("cA", [128, 32], F32, es=eR)
            cBb = k.sb("cBb", [128, 32], F32, es=eR)
            basef = k.sb("basef", [128, 32], F32, es=eR)
            rowf = k.sb("rowf", [128, 32], F32, es=eR)
            tmp32 = k.sb("tmp32", [128, 32], F32, es=eR)
            ROWF = k.sb("ROWF", [128, NT, 2], F32, es=eR)
            esl_f = k.sb("esl_f", [1, NSLOT + 1], F32, es=eR)
            nfl_f = k.sb("nfl_f", [1, NSLOT], F32, es=eR)
            k.op(dve, lambda: V.memset(onesb[:], 1.0), writes=[onesb])
            k.dma(sp, stri[:], CD["stri"][:], reads=[CD["stri"]], writes=[stri])
            pcnt = PB[0]
            for i in range(NT):
                mm(pcnt, pcnt[:, 0:32], onesb, onesb[:], SELb, SELb[:, i, :], i == 0, i == NT - 1)
            evac(dve, ncnt[:], pcnt[:, 0:32], [pcnt], [ncnt])
            k.op(dve, lambda: V.memset(tl[:], 0.0), writes=[tl])
            for j in range(16):
                stt(tl, tl[:], ncnt, ncnt[:], float(128 * j), tl, tl[:], ALU.is_gt, ALU.add)
            k.op(dve, lambda: V.tensor_copy(out=cA[:], in_=tl[:]), reads=[tl], writes=[cA])
            src, dst = cA, cBb
            for sft in [1, 2, 4, 8, 16]:
                k.op(dve, lambda: V.tensor_copy(out=dst[:, 0:sft], in_=src[:, 0:sft]), reads=[src], writes=[dst])
                tt(dst, dst[:, sft:32], src, src[:, sft:32], src, src[:, 0:32 - sft], ALU.add)
                src, dst = dst, src
            cum = src
            tt(basef, basef[:], cum, cum[:], tl, tl[:], ALU.subtract)
            ts(basef, basef[:], basef, basef[:], 128.0, ALU.mult)
            for i in range(NT):
                pp = PB[1 + i % 2]
                for j in range(i):
                    mm(pp, pp[:, 0:32], onesb, onesb[:], SELb, SELb[:, j, :], j == 0, False)
                mm(pp, pp[:, 0:32], stri, stri[:], SELb, SELb[:, i, :], i == 0, True)
                tt(rowf, rowf[:], pp, pp[:, 0:32], basef, basef[:], ALU.add)
                for kk, OH in enumerate([OH1a, OH2a]):
                    tt(tmp32, tmp32[:], rowf, rowf[:], OH, OH[:, i, :], ALU.mult)
                    k.op(dve, lambda: V.reduce_sum(out=ROWF[:, i, kk:kk + 1], in_=tmp32[:], axis=AX.X), reads=[tmp32], writes=[ROWF])
            k.op(dve, lambda: V.tensor_copy(out=ROWI[:], in_=ROWF[:]), reads=[ROWF], writes=[ROWI])
            k.op(dve, lambda: V.memset(esl_f[:], -1.0), writes=[esl_f])
            for s in range(NSLOT):
                k.op(dve, lambda: V.tensor_scalar(out=tmp32[0:1, :], in0=cum[0:1, :], scalar1=float(s), scalar2=None, op0=ALU.is_le,
                                                  op1=ALU.add, accum_out=esl_f[0:1, s + 1:s + 2]), reads=[cum], writes=[tmp32, esl_f])
            ts(esl_f, esl_f[0:1, 1:NSLOT + 1], esl_f, esl_f[0:1, 1:NSLOT + 1], 31.0, ALU.min)
            k.op(dve, lambda: V.memset(nfl_f[:], 1.0), writes=[nfl_f])
            tt(nfl_f, nfl_f[0:1, 2:NSLOT], esl_f, esl_f[0:1, 3:NSLOT + 1], esl_f, esl_f[0:1, 1:NSLOT - 1], ALU.not_equal)
            onesrow = k.sb("onesrow", [1, 128], F32, es=eR)
            iop = k.sb("iop", [128, 1], F32, es=eR)
            idxf = k.sb("idxf", [128, NSLOT], F32, es=eR)
            k.op(dve, lambda: V.memset(onesrow[:], 1.0), writes=[onesrow])
            k.dma(sp, iop[:], CD["iota_p"][:], reads=[CD["iota_p"]], writes=[iop])
            mm(PB[3], PB[3][:, 0:NSLOT], onesrow, onesrow[:], esl_f, esl_f[0:1, 1:NSLOT + 1], True, True)
            mm(PB[4], PB[4][:, 0:NSLOT], onesrow, onesrow[:], nfl_f, nfl_f[:], True, True)
            ts(idxf, idxf[:], PB[3], PB[3][:, 0:NSLOT], 128.0, ALU.mult, iop[:, 0:1], ALU.add, sreads=[iop])
            ts(idxf, idxf[:], idxf, idxf[:], -100000.0, ALU.add)
            tt(idxf, idxf[:], idxf, idxf[:], PB[4], PB[4][:, 0:NSLOT], ALU.mult)
            ts(idxf, idxf[:], idxf, idxf[:], 100000.0, ALU.add)
            k.op(dve, lambda: V.tensor_copy(out=IDXI[:], in_=idxf[:]), reads=[idxf], writes=[IDXI])
            hb = [k.sb(f"hb{i}", [128, D], BF16, es=eR) for i in range(2)]
            for i in range(NT):
                hbt = hb[i % 2]
                k.dma(sp, hbt[:], hn_scr[i * 128:(i + 1) * 128, :], reads=[hn_scr], writes=[hbt])
                for kk in range(2):
                    xv_ = k.view(xslot, "xs_st"); x_views.append(xv_)
                    k.dma(pool, None, None, reads=[hbt, ROWI], writes=[xv_],
                          fn=lambda: G.indirect_dma_start(out=xslot[:], out_offset=bass.IndirectOffsetOnAxis(ap=ROWI[:, i, kk:kk + 1], axis=0),
                                                          in_=hbt[:], in_offset=None))

        with Scope(k) as eM:
            WGs = [k.sb(f"WG{i}", [128, 16, 512], BF16, es=eM) for i in range(2)]
            WUs = [k.sb(f"WU{i}", [128, 16, 512], BF16, es=eM) for i in range(2)]
            WDs = [k.sb(f"WD{i}", [128, 4, D], BF16, es=eM) for i in range(2)]
            xgbs = [k.sb(f"xgb{i}", [128, D], BF16, es=eM) for i in range(2)]
            xgTs = [k.sb(f"xgT{i}", [128, 16, 128], BF16, es=eM) for i in range(2)]
            hTs = [k.sb(f"hT{i}", [128, 4, 128], BF16, es=eM) for i in range(2)]
            sgs = [k.sb(f"sg{i}", [128, 128], F32, es=eM) for i in range(2)]
            ybts = [k.sb(f"ybt{i}", [128, D], F32, es=eM) for i in range(2)]
            PTh = [k.view(PT, "PTa"), k.view(PT, "PTb")]
            precast_step(1000)
            bc_reg = G.to_reg(4095)

            def load_w_slot(s):
                for Wb, src in [(WGs[s % 2], wbf["g"]), (WUs[s % 2], wbf["u"]), (WDs[s % 2], wbf["d"])]:
                    src2d = src.t[:].rearrange("e (p c) f -> (e p) (c f)", p=128)
                    k.dma(pool, None, None, reads=pre_views + [IDXI], writes=[Wb],
                          fn=lambda: G.indirect_dma_start(out=Wb[:].rearrange("p c f -> p (c f)"), out_offset=None, in_=src2d,
                                                          in_offset=bass.IndirectOffsetOnAxis(ap=IDXI[:, s:s + 1], axis=0),
                                                          bounds_check=bc_reg, oob_is_err=False))

            def load_x_dma(s):
                xgb = xgbs[s % 2]
                k.dma(sp, xgb[:], xslot[s * 128:(s + 1) * 128, :], reads=x_views, writes=[xgb])

            def load_x_tr(s):
                xgb = xgbs[s % 2]; xgT = xgTs[s % 2]
                for c4 in range(4):
                    for cc in range(4):
                        c = c4 * 4 + cc
                        transpose(PT, PT[:, cc * 128:(cc + 1) * 128], xgb, xgb[:, c:D:16], ident_bf)
                    evac(act if c4 % 2 else dve, xgT[:, c4 * 4:(c4 + 1) * 4, :], PT[:, 0:512].rearrange("p (c t) -> p c t", c=4), [PT], [xgT])

            load_x_dma(0)
            load_x_dma(1)
            load_w_slot(0)
            load_x_tr(0)
            for s in range(NSLOT):
                xgT = xgTs[s % 2]; ybt = ybts[s % 2]; hT = hTs[s % 2]
                WG, WU, WD = WGs[s % 2], WUs[s % 2], WDs[s % 2]
                if s + 1 < NSLOT:
                    load_x_tr(s + 1)
                    if s + 2 < NSLOT:
                        load_x_dma(s + 2)
                    load_w_slot(s + 1)
                for c2 in range(4):
                    pg = PB[(2 * c2) % 4]; pu = PB[(2 * c2 + 1) % 4]; sg = sgs[c2 % 2]
                    for c in range(16):
                        mm(pg, pg[:, 0:128], WG, WG[:, c, c2:512:4], xgT, xgT[:, c, :], c == 0, c == 15)
                    for c in range(16):
                        mm(pu, pu[:, 0:128], WU, WU[:, c, c2:512:4], xgT, xgT[:, c, :], c == 0, c == 15)
                    k.op(act, lambda: S.activation(out=sg[:], in_=pg[:, 0:128], func=AF.Silu), reads=[pg], writes=[sg])
                    tt(hT, hT[:, c2, :], sg, sg[:], pu, pu[:, 0:128], ALU.mult)
                for dmb in range(4):
                    pd = PB[4 + dmb % 3]
                    for c2 in range(4):
                        mm(pd, pd[:], hT, hT[:, c2, :], WD, WD[:, c2, dmb * 512:(dmb + 1) * 512], c2 == 0, c2 == 3)
                    evac(act if dmb % 2 else dve, ybt[:, dmb * 512:(dmb + 1) * 512], pd[:], [pd], [ybt])
                k.dma(sp, yslot[s * 128:(s + 1) * 128, :], ybt[:], reads=[ybt], writes=[yslot])

        with Scope(k) as eZ:
            fw_bc = k.sb("fw_bc", [128, D], F32, es=eZ)
            k.dma(sp, fw_bc[:], bass.AP(P["final_norm_w"].t, 0, [[0, 128], [1, D]]), reads=[P["final_norm_w"]], writes=[fw_bc])
            h1b = [k.sb(f"h1b{i}", [128, D], F32, es=eZ) for i in range(2)]
            g0 = [k.sb(f"g0{i}", [128, D], F32, es=eZ) for i in range(2)]
            g1 = [k.sb(f"g1{i}", [128, D], F32, es=eZ) for i in range(2)]
            ob = [k.sb(f"ob{i}", [128, D], F32, es=eZ) for i in range(2)]
            rf = k.sb("rf", [128, 4], F32, es=eZ)
            for i in range(NT):
                hh = h1b[i % 2]; a0 = g0[i % 2]; a1 = g1[i % 2]; oo = ob[i % 2]
                k.dma(sp, hh[:], h1_scr[i * 128:(i + 1) * 128, :], reads=[h1_scr], writes=[hh])
                for kk, gb in enumerate([a0, a1]):
                    k.dma(pool, None, None, reads=[yslot, ROWI], writes=[gb],
                          fn=lambda: G.indirect_dma_start(out=gb[:], out_offset=None, in_=yslot[:],
                                                          in_offset=bass.IndirectOffsetOnAxis(ap=ROWI[:, i, kk:kk + 1], axis=0)))
                stt(hh, hh[:], a0, a0[:], Wk[:, i, 0:1], hh, hh[:], ALU.mult, ALU.add, sreads=[Wk])
                stt(hh, hh[:], a1, a1[:], Wk[:, i, 1:2], hh, hh[:], ALU.mult, ALU.add, sreads=[Wk])
                k.op(act, lambda: S.activation(out=junk[:], in_=hh[:], func=AF.Square, accum_out=rf[:, 0:1]), reads=[hh], writes=[junk, rf])
                rstd_from_ss(rf[:, 0:1], rf, rf[:, 1:2], rf, D)
                stt(oo, oo[:], hh, hh[:], rf[:, 1:2], fw_bc, fw_bc[:], ALU.mult, ALU.mult, sreads=[rf])
                k.dma(sp, out_d[i * 128:(i + 1) * 128, :], oo[:], reads=[oo], writes=[out_d])
        return done([out_d])


_NC_CACHE = {}


def _in_maps(inputs, n_cores=8):
    consts = _consts()
    maps = []
    for c in range(n_cores):
        m = {"x": np.ascontiguousarray(inputs["x"][c])}
        for name, shape in PARAM_SPECS:
            m[name] = np.ascontiguousarray(np.asarray(inputs[name], dtype=np.float32).reshape(shape))
        for name, shape, dt in CONST_SPECS:
            m["c_" + name] = consts[name]
        maps.append(m)
    return maps


def kernel(**inputs):
    if "nc" not in _NC_CACHE:
        _NC_CACHE["nc"] = build_nc()
    nc = _NC_CACHE["nc"]
    res = run_bass_kernel_spmd(nc, _in_maps(inputs), core_ids=list(range(8)))
    return np.stack([np.asarray(r["out"], dtype=np.float32) for r in res.results], axis=0)
```

```python
import math
import os
from contextlib import ExitStack

import ml_dtypes
import numpy as np

import concourse.bass as bass
import concourse.mybir as mybir
from concourse.bass_utils import run_bass_kernel_spmd

F32 = mybir.dt.float32
BF16 = mybir.dt.bfloat16
I32 = mybir.dt.int32
AF = mybir.ActivationFunctionType
ALU = mybir.AluOpType
AX = mybir.AxisListType

T = 2048
D = 2048
NT = 16
HD = 64
NEG = -30000.0
IN_COLS = 5696
C_NQ, C_KCMP, C_VCMP, C_KSLC, C_VSLC, C_KWIN, C_VWIN, C_GATE, C_FQ, C_FK, C_FV, C_FF = (
    0, 1024, 1280, 1536, 1792, 2048, 2304, 2560, 2608, 3632, 4656, 5680)
NSLOT = 64
VOFF = 2112
VLEN = 4608


class Eng:
    def __init__(self, name, e, sem, strict_self=True):
        self.name = name; self.e = e; self.sem = sem; self.cnt = 0
        self.waited = {}; self.strict_self = strict_self


class Buf:
    def __init__(self, t, name=""):
        self.t = t; self.name = name; self.w = {}; self.r = {}

    def __getitem__(self, idx):
        return self.t[idx]


class K:
    def __init__(self, nc, es, n_dma_sems=40):
        self.nc = nc; self.es = es

        def mk(name, e, strict=True):
            return Eng(name, e, es.enter_context(nc.semaphore("sem_" + name)), strict)
        self.pe = mk("pe", nc.tensor, strict=False)
        relax = os.environ.get("K_RELAX", "") .split(",")
        self.act = mk("act", nc.scalar, strict="act" not in relax)
        self.dve = mk("dve", nc.vector, strict="dve" not in relax)
        self.pool = mk("pool", nc.gpsimd, strict="pool" not in relax)
        self.sp = mk("sp", nc.sync)
        self.dsems = [[es.enter_context(nc.semaphore(f"dsem{i}")), 0] for i in range(n_dma_sems)]
        self.dnext = 0
        self.nwaits = 0; self.nops = 0; self.ndma = 0

    def sb(self, name, shape, dt, es=None):
        return Buf((es or self.es).enter_context(self.nc.sbuf_tensor(name, list(shape), dt)), name)

    def ps(self, name, shape, dt):
        return Buf(self.es.enter_context(self.nc.psum_tensor(name, list(shape), dt)), name)

    def dram(self, name, shape, dt, kind="Internal"):
        return Buf(self.nc.dram_tensor(name, list(shape), dt, kind=kind), name)

    def view(self, buf, name=""):
        return Buf(buf.t, name or buf.name)

    def _wait(self, E, need, keep_last=False):
        pend = []
        for key, (sem, val) in need.items():
            if sem is E.sem and not E.strict_self:
                continue
            if E.waited.get(key, 0) >= val:
                continue
            pend.append((key, sem, val))
        last = None
        if keep_last and pend:
            last = pend.pop()
        for key, sem, val in pend:
            E.e.wait_ge(sem, val); E.waited[key] = val; self.nwaits += 1
        if last is not None:
            E.waited[last[0]] = last[2]
        return last

    @staticmethod
    def _merge(need, d):
        for kk, (s, v) in d.items():
            if kk not in need or need[kk][1] < v:
                need[kk] = (s, v)

    def _deps(self, reads, writes):
        need = {}
        for b in reads:
            self._merge(need, b.w)
        for b in writes:
            self._merge(need, b.w); self._merge(need, b.r)
        return need

    def op(self, E, fn, reads=(), writes=()):
        last = self._wait(E, self._deps(reads, writes), keep_last=True)
        ins = fn()
        if last is not None:
            ins._wait_ge(last[1], last[2])
        E.cnt += 1; self.nops += 1
        ins.then_inc(E.sem, 1)
        tok = (E.sem, E.cnt); key = id(E.sem)
        for b in reads:
            b.r[key] = tok
        for b in writes:
            b.w = {key: tok}; b.r = {}
        return ins

    def dma(self, E, out_ap, in_ap, reads=(), writes=(), fn=None, sems=None, **kw):
        need = self._deps(reads, writes)
        if sems is not None:
            ds = sems[0][sems[1][0] % len(sems[0])]; sems[1][0] += 1
        else:
            ds = self.dsems[self.dnext]; self.dnext = (self.dnext + 1) % len(self.dsems)
        if ds[1] > 0:
            self._merge(need, {id(ds[0]): (ds[0], ds[1])})
        self._wait(E, need)
        if fn is None:
            ins = E.e.dma_start(out=out_ap, in_=in_ap, **kw)
        else:
            ins = fn()
        ds[1] += 16; self.ndma += 1
        ins.then_inc(ds[0], 16)
        tok = (ds[0], ds[1]); key = id(ds[0])
        for b in reads:
            b.r[key] = tok
        for b in writes:
            b.w = {key: tok}; b.r = {}
        return ins

    def barrier(self):
        engs = [self.pe, self.act, self.dve, self.pool, self.sp]
        for E in engs:
            need = {}
            for F in engs:
                if F is not E and F.cnt > 0:
                    need[id(F.sem)] = (F.sem, F.cnt)
            for ds in self.dsems:
                if ds[1] > 0:
                    need[id(ds[0])] = (ds[0], ds[1])
            self._wait(E, need)

    def finish(self, bufs):
        need = {}
        for b in bufs:
            self._merge(need, b.w)
        self._wait(self.sp, need)


class Scope:
    def __init__(self, k):
        self.k = k; self.es = ExitStack()

    def __enter__(self):
        self.es.__enter__()
        return self.es

    def __exit__(self, *a):
        if a[0] is None:
            self.k.barrier()
        return self.es.__exit__(*a)


def _t5_bucket(n):
    n = np.maximum(n, 0)
    rel = np.log(np.maximum(n, 1).astype(np.float32) / np.float32(16)) / np.float32(math.log(128 / 16))
    large = 16 + (rel * np.float32(16)).astype(np.int32)
    large = np.minimum(large, 31)
    return np.where(n < 16, n, large)


def _consts():
    bf = ml_dtypes.bfloat16
    c = {}
    c["ident_bf"] = np.eye(128, dtype=np.float32).astype(bf)
    c["ident_f"] = np.eye(128, dtype=np.float32)
    i = np.arange(128)[:, None]; j = np.arange(128)[None, :]
    c["caus"] = np.where(i <= j, 0.0, NEG).astype(bf)
    c["w4m"] = np.where(i > j, 0.0, NEG).astype(bf)
    c["stri"] = (i < j).astype(np.float32).astype(bf)
    up = np.zeros((128, 4, 512), np.float32)
    for jo in range(4):
        for to in range(4):
            if jo < to:
                up[:, jo, to * 128:(to + 1) * 128] = 1.0
            elif jo == to:
                up[:, jo, to * 128:(to + 1) * 128] = (i <= j)
    c["upat"] = up
    m = np.arange(VLEN) - VOFF
    oh = np.zeros((33, VLEN), np.float32)
    bk = _t5_bucket(m)
    for idx in range(VLEN):
        if m[idx] >= 0:
            oh[bk[idx], idx] = 1.0
        else:
            oh[32, idx] = 1.0
    c["ohv"] = oh
    sel31 = np.zeros((32, 32), np.float32); sel31[31, :] = 1.0
    c["sel31"] = sel31
    am = np.zeros((128, 32), np.float32)
    for jj in range(32):
        for a in range(4):
            for b in range(2):
                cc = jj * 4 + a - b
                if 0 <= cc < 127:
                    am[cc, jj] += 1.0
    c["amat"] = am.astype(bf)
    em = np.zeros((32, 2048), np.float32)
    for jj in range(32):
        em[jj, jj * 64:(jj + 1) * 64] = 1.0
    c["emat"] = em.astype(bf)
    t = np.arange(1024, 2048)
    blk = np.arange(32)[None, :]
    cur = (t // 64)[:, None]
    valid = (blk * 64 <= t[:, None])
    forced = (blk == 0) | (blk == cur) | (blk == cur - 1)
    add = np.where(valid, np.where(forced, 1e4, 0.0), -1e30).astype(np.float32)
    c["selvalid"] = valid.astype(np.float32).reshape(8, 128, 32).transpose(1, 0, 2).copy()
    c["seladd"] = add.reshape(8, 128, 32).transpose(1, 0, 2).copy()
    c["ones_d"] = np.ones((3, 8, 2048), np.float32).astype(bf)
    c["iota_p"] = np.arange(128, dtype=np.float32).reshape(128, 1)
    return c


CONST_SPECS = [("ident_bf", [128, 128], BF16), ("ident_f", [128, 128], F32), ("caus", [128, 128], BF16),
               ("w4m", [128, 128], BF16), ("stri", [128, 128], BF16), ("upat", [128, 4, 512], F32),
               ("ohv", [33, VLEN], F32), ("sel31", [32, 32], F32), ("amat", [128, 32], BF16),
               ("emat", [32, 2048], BF16), ("selvalid", [128, 8, 32], F32), ("seladd", [128, 8, 32], F32),
               ("ones_d", [3, 8, 2048], BF16), ("iota_p", [128, 1], F32)]

PARAM_SPECS = [("attn_norm_w", [1, 2048]), ("w_in", [1, 2048, IN_COLS]), ("cmp_pe_k", [1, 32, 64]),
               ("cmp_pe_v", [1, 32, 64]), ("cmp_k_w1", [1, 2048, 128]), ("cmp_k_w2", [1, 128, 64]),
               ("cmp_v_w1", [1, 2048, 128]), ("cmp_v_w2", [1, 128, 64]), ("rel_bias_table", [32, 16]),
               ("fox_forget_b", [1, 16]), ("nsa_out_norm_w", [1, 1024]), ("fox_out_norm_w", [1, 1024]),
               ("w_out", [1, 2048, 2048]), ("ffn_norm_w", [1, 2048]), ("router_group_w", [1, 2048, 4]),
               ("router_group_b", [1, 4]), ("router_expert_w", [1, 2048, 32]), ("router_expert_b", [1, 32]),
               ("expert_w_gate", [32, 2048, 512]), ("expert_w_up", [32, 2048, 512]),
               ("expert_w_down", [32, 512, 2048]), ("final_norm_w", [2048])]


def build_nc(stop_after=None, debug=False):
    nc = bass.Bass("TRN2", target_bir_lowering=False)
    P = {}
    x_d = Buf(nc.dram_tensor("x", [T, D], F32, kind="ExternalInput"), "x")
    for name, shape in PARAM_SPECS:
        P[name] = Buf(nc.dram_tensor(name, shape, F32, kind="ExternalInput"), name)
    CD = {}
    for name, shape, dt in CONST_SPECS:
        CD[name] = Buf(nc.dram_tensor("c_" + name, shape, dt, kind="ExternalInput"), name)
    out_d = Buf(nc.dram_tensor("out", [T, D], F32, kind="ExternalOutput"), "out")
    dbg = {}
    V, S, G, TE = nc.vector, nc.scalar, nc.gpsimd, nc.tensor

    with ExitStack() as es:
        k = K(nc, es)
        pe, act, dve, pool, sp = k.pe, k.act, k.dve, k.pool, k.sp

        o_scr = k.dram("o_scr", [T, 2048], BF16)
        vscr = k.dram("vscr", [16, VLEN], BF16)
        brd = k.dram("brd", [16 * 128, VLEN], BF16)
        cscr = k.dram("cscr", [16, 6, T], BF16)
        h1_scr = k.dram("h1_scr", [T, D], F32)
        hn_scr = k.dram("hn_scr", [T, D], BF16)
        xslot = k.dram("xslot", [NSLOT * 128, D], BF16)
        yslot = k.dram("yslot", [NSLOT * 128, D], F32)
        if debug:
            for nm, shp, dt in [("d_o", [T, 2048], BF16), ("d_h1", [T, D], F32)]:
                dbg[nm] = Buf(nc.dram_tensor(nm, shp, dt, kind="ExternalOutput"), nm)

        wbf = {"g": k.dram("wbf_g", [32, 2048, 512], BF16), "u": k.dram("wbf_u", [32, 2048, 512], BF16),
               "d": k.dram("wbf_d", [32, 512, 2048], BF16)}
        pre_sems = ([[es.enter_context(nc.semaphore(f"presem{i}")), 0] for i in range(6)], [0])
        pre_list = []
        pre_views = []
        for e_ in range(32):
            for key_, pn_ in [("g", "expert_w_gate"), ("u", "expert_w_up"), ("d", "expert_w_down")]:
                vw = k.view(wbf[key_], f"wbf_{key_}{e_}")
                pre_views.append(vw)
                pre_list.append((vw, wbf[key_].t[e_], P[pn_], P[pn_].t[e_]))
        pre_pos = [0]

        def precast_step(n):
            for _ in range(n):
                if pre_pos[0] >= len(pre_list):
                    return
                vw, dst_ap, sb_, src_ap = pre_list[pre_pos[0]]; pre_pos[0] += 1
                k.dma(pool, dst_ap, src_ap, reads=[sb_], writes=[vw], sems=pre_sems)

        o_views = []

        def o_store(dst_ap, src_b, src_ap):
            v_ = k.view(o_scr, "o_st")
            k.dma(sp, dst_ap, src_ap, reads=[src_b], writes=[v_])
            o_views.append(v_)

        def done(bufs):
            k.finish(bufs)
            print("ops", k.nops, "waits", k.nwaits, "dmas", k.ndma, flush=True)
            return nc

        PB = [k.ps(f"pb{i}", [128, 512], F32) for i in range(7)]
        PT = k.ps("pt_bf", [128, 1024], BF16)

        ident_bf = k.sb("ident_bf", [128, 128], BF16)
        ident_f = k.sb("ident_f", [128, 128], F32)
        caus = k.sb("caus", [128, 128], BF16)
        for b_, nm in [(ident_bf, "ident_bf"), (ident_f, "ident_f"), (caus, "caus")]:
            k.dma(sp, b_[:], CD[nm][:], reads=[CD[nm]], writes=[b_])
        rstd_tmp = k.sb("rstd_tmp", [128, 4], F32)
        ssn = k.sb("ssn", [128, NT], F32)
        ssf = k.sb("ssf", [128, NT], F32)
        sstmp = k.sb("sstmp", [128, 2], F32)
        eps_t = k.sb("eps_t", [128, 1], F32)
        junk = k.sb("junk", [128, 2048], BF16)
        k.op(dve, lambda: V.memset(ssn[:], 0.0), writes=[ssn])
        k.op(dve, lambda: V.memset(ssf[:], 0.0), writes=[ssf])
        k.op(dve, lambda: V.memset(eps_t[:], 1e-6), writes=[eps_t])

        def evac(E, out_ap, in_ap, reads, writes, scale=None):
            if E is act:
                if scale is None:
                    return k.op(act, lambda: S.copy(out=out_ap, in_=in_ap), reads=reads, writes=writes)
                return k.op(act, lambda: S.activation(out=out_ap, in_=in_ap, func=AF.Copy, scale=scale), reads=reads, writes=writes)
            if scale is None:
                return k.op(dve, lambda: V.tensor_copy(out=out_ap, in_=in_ap), reads=reads, writes=writes)
            return k.op(dve, lambda: V.tensor_scalar(out=out_ap, in0=in_ap, scalar1=scale, scalar2=None, op0=ALU.mult),
                        reads=reads, writes=writes)

        def mm(out_b, out_ap, l_b, l_ap, r_b, r_ap, start, stop, extra_reads=()):
            return k.op(pe, lambda: TE.matmul(out_ap, l_ap, r_ap, start=start, stop=stop),
                        reads=[l_b, r_b] + list(extra_reads), writes=[out_b])

        def transpose(out_b, out_ap, in_b, in_ap, id_b, kp=128):
            return k.op(pe, lambda: TE.transpose(out_ap, in_ap, id_b[0:kp, 0:kp]), reads=[in_b, id_b], writes=[out_b])

        def rstd_from_ss(ss_ap, ss_b, out_ap, out_b, n):
            k.op(act, lambda: S.activation(out=rstd_tmp[:, 0:1], in_=ss_ap, func=AF.Ln, scale=1.0 / n, bias=eps_t[:, 0:1]),
                 reads=[ss_b, eps_t], writes=[rstd_tmp])
            k.op(act, lambda: S.activation(out=out_ap, in_=rstd_tmp[:, 0:1], func=AF.Exp, scale=-0.5),
                 reads=[rstd_tmp], writes=[out_b])

        def tt(out_b, out_ap, a_b, a_ap, b_b, b_ap, op, E=None):
            return k.op(E or dve, lambda: (E or dve).e.tensor_tensor(out=out_ap, in0=a_ap, in1=b_ap, op=op), reads=[a_b, b_b], writes=[out_b])

        def ts(out_b, out_ap, a_b, a_ap, s1, op0, s2=None, op1=None, sreads=()):
            if op1 is None:
                return k.op(dve, lambda: V.tensor_scalar(out=out_ap, in0=a_ap, scalar1=s1, scalar2=None, op0=op0),
                            reads=[a_b] + list(sreads), writes=[out_b])
            return k.op(dve, lambda: V.tensor_scalar(out=out_ap, in0=a_ap, scalar1=s1, scalar2=s2, op0=op0, op1=op1),
                        reads=[a_b] + list(sreads), writes=[out_b])

        def stt(out_b, out_ap, a_b, a_ap, sc, b_b, b_ap, op0, op1, sreads=()):
            return k.op(dve, lambda: V.scalar_tensor_tensor(out=out_ap, in0=a_ap, scalar=sc, in1=b_ap, op0=op0, op1=op1),
                        reads=[a_b, b_b] + list(sreads), writes=[out_b])

        with Scope(k) as esA:
            xnT = k.sb("xnT", [128, 16, T], BF16, es=esA)
            with Scope(k) as e1:
                anw_bc = k.sb("anw_bc", [128, D], F32, es=e1)
                k.dma(sp, anw_bc[:], bass.AP(P["attn_norm_w"].t, 0, [[0, 128], [1, D]]), reads=[P["attn_norm_w"]], writes=[anw_bc])
                xb = [k.sb(f"xb{i}", [128, D], F32, es=e1) for i in range(2)]
                xn_tm = [k.sb(f"xn_tm{i}", [128, D], BF16, es=e1) for i in range(2)]
                ssx = k.sb("ssx", [128, NT], F32, es=e1)
                rsx = k.sb("rsx", [128, NT], F32, es=e1)
                for i in range(NT):
                    xs = xb[i % 2]; xt = xn_tm[i % 2]
                    k.dma(sp, xs[:], x_d[i * 128:(i + 1) * 128, :], reads=[x_d], writes=[xs])
                    k.op(act, lambda: S.activation(out=junk[:], in_=xs[:], func=AF.Square, accum_out=ssx[:, i:i + 1]),
                         reads=[xs], writes=[junk, ssx])
                    rstd_from_ss(ssx[:, i:i + 1], ssx, rsx[:, i:i + 1], rsx, D)
                    stt(xt, xt[:], xs, xs[:], rsx[:, i:i + 1], anw_bc, anw_bc[:], ALU.mult, ALU.mult, sreads=[rsx])
                    for c4 in range(4):
                        for cc in range(4):
                            c = c4 * 4 + cc
                            transpose(PT, PT[:, cc * 128:(cc + 1) * 128], xt, xt[:, c * 128:(c + 1) * 128], ident_bf)
                        evac(act if c4 % 2 else dve, xnT[:, c4 * 4:(c4 + 1) * 4, i * 128:(i + 1) * 128],
                             PT[:, 0:512].rearrange("p (c t) -> p c t", c=4), [PT], [xnT])

            if stop_after == "xn":
                k.dma(sp, dbg["d_o"].t[:].rearrange("(p c) t -> p c t", c=16), xnT[:], reads=[xnT], writes=[dbg["d_o"]])
                return done([dbg["d_o"]])
            with Scope(k) as e2:
                tab = k.sb("tab", [33, 16], F32, es=e2)
                sel31 = k.sb("sel31", [32, 32], F32, es=e2)
                ohv = k.sb("ohv", [33, VLEN], F32, es=e2)
                vsb = k.sb("vsb", [16, VLEN], BF16, es=e2)
                k.dma(sp, tab[0:32, :], P["rel_bias_table"][:], reads=[P["rel_bias_table"]], writes=[tab])
                k.dma(sp, sel31[:], CD["sel31"][:], reads=[CD["sel31"]], writes=[sel31])
                k.dma(sp, ohv[:], CD["ohv"][:], reads=[CD["ohv"]], writes=[ohv])
                mm(PB[0], PB[0][0:32, 0:16], sel31, sel31[:], tab, tab[0:32, :], True, True)
                tt(tab, tab[0:32, :], tab, tab[0:32, :], PB[0], PB[0][0:32, 0:16], ALU.subtract)
                k.op(dve, lambda: V.memset(tab[32:33, :], NEG), writes=[tab])
                for q in range(VLEN // 512):
                    pb = PB[q % 2]
                    mm(pb, pb[0:16, :], tab, tab[:], ohv, ohv[:, q * 512:(q + 1) * 512], True, True)
                    evac(dve, vsb[:, q * 512:(q + 1) * 512], pb[0:16, :], [pb], [vsb])
                k.dma(sp, vscr[:], vsb[:], reads=[vsb], writes=[vscr])
                for h in range(16):
                    k.dma(sp, brd[h * 128:(h + 1) * 128, :], bass.AP(vscr.t, h * VLEN, [[0, 128], [1, VLEN]]), reads=[vscr], writes=[brd])

            wst_box = [None]

            def cast(E, out_b, out_ap, in_b, in_ap):
                return k.op(E, lambda: E.e.tensor_copy(out=out_ap, in_=in_ap), reads=[in_b], writes=[out_b])

            def load_w(buf, c0, col0, ncols):
                wst = wst_box[0]
                stv = wst[:, 0:16 * ncols].rearrange("p (c n) -> p c n", c=16)
                src = P["w_in"].t[0, :, col0:col0 + ncols].rearrange("(c p) n -> p c n", p=128)
                k.dma(sp, stv, src, reads=[P["w_in"]], writes=[wst])
                cast(pool, buf, buf[:, :, c0:c0 + ncols], wst, stv)

            rot = [0]

            def proj_fm(wbuf, ncols, dst_b, dst_fn, scale):
                for tc in range(4):
                    pb = PB[rot[0] % 3]; rot[0] += 1
                    for c in range(16):
                        mm(pb, pb[0:ncols, :], wbuf, wbuf[:, c, 0:ncols], xnT, xnT[:, c, tc * 512:(tc + 1) * 512], c == 0, c == 15)
                    evac(act if rot[0] % 2 else dve, dst_fn(tc), pb[0:ncols, :], [pb], [dst_b], scale)

            with Scope(k) as eC:
                wst_box[0] = k.sb("wstC", [128, 16 * 16], F32, es=eC)
                wf = k.sb("wf", [128, 16, 16], BF16, es=eC)
                fb_bc = k.sb("fb_bc", [128, 16], F32, es=eC)
                logf = k.sb("logf", [128, NT, 16], F32, es=eC)
                upat = k.sb("upat", [128, 4, 512], F32, es=eC)
                onesf = k.sb("onesf", [128, 512], F32, es=eC)
                cT = k.sb("cT", [16, T], F32, es=eC)
                r1 = k.sb("r1", [16, T], F32, es=eC)
                parts = k.sb("parts", [16, 6, T], BF16, es=eC)
                ztmp = k.sb("ztmp", [128, 16], F32, es=eC)
                load_w(wf, 0, C_FF, 16)
                k.dma(sp, fb_bc[:], bass.AP(P["fox_forget_b"].t, 0, [[0, 128], [1, 16]]), reads=[P["fox_forget_b"]], writes=[fb_bc])
                k.dma(sp, upat[:], CD["upat"][:], reads=[CD["upat"]], writes=[upat])
                k.op(dve, lambda: V.memset(onesf[:], 1.0), writes=[onesf])
                if stop_after == "wf":
                    k.dma(sp, dbg["d_o"].t[0:128, 0:256], wf[:].rearrange("p a b -> p (a b)"), reads=[wf], writes=[dbg["d_o"]])
                    k.dma(pool, dbg["d_o"].t[128:256, 0:16], fb_bc[:], reads=[fb_bc], writes=[dbg["d_o"]])
                    return done([dbg["d_o"]])
                for i in range(NT):
                    pb = PB[i % 2]
                    for c in range(16):
                        mm(pb, pb[:, 0:16], xnT, xnT[:, c, i * 128:(i + 1) * 128], wf, wf[:, c, :], c == 0, c == 15)
                    tt(ztmp, ztmp[:], pb, pb[:, 0:16], fb_bc, fb_bc[:], ALU.add)
                    k.op(act, lambda: S.activation(out=ztmp[:], in_=ztmp[:], func=AF.Exp, scale=-1.0), reads=[ztmp], writes=[ztmp])
                    k.op(act, lambda: S.activation(out=ztmp[:], in_=ztmp[:], func=AF.Ln, scale=1.0, bias=1.0), reads=[ztmp], writes=[ztmp])
                    ts(logf, logf[:, i, :], ztmp, ztmp[:], -1.0, ALU.mult)
                for q in range(4):
                    pb = PB[q % 2]
                    n = 4 * q + 4
                    for j in range(n):
                        jo = j - 4 * q
                        if jo >= 0:
                            mm(pb, pb[0:16, :], logf, logf[:, j, :], upat, upat[:, jo, :], j == 0, j == n - 1)
                        else:
                            mm(pb, pb[0:16, :], logf, logf[:, j, :], onesf, onesf[:], j == 0, False)
                    evac(dve, cT[:, q * 512:(q + 1) * 512], pb[0:16, :], [pb], [cT])
                if stop_after == "lf":
                    k.dma(sp, dbg["d_h1"].t[0:128, 0:256], logf[:].rearrange("p a b -> p (a b)"), reads=[logf], writes=[dbg["d_h1"]])
                    k.dma(sp, dbg["d_h1"].t[128:144, :], cT[:], reads=[cT], writes=[dbg["d_h1"]])
                    return done([dbg["d_h1"]])
                k.op(dve, lambda: V.tensor_copy(out=parts[:, 0, :], in_=cT[:]), reads=[cT], writes=[parts])
                tt(r1, r1[:], cT, cT[:], parts, parts[:, 0, :], ALU.subtract)
                k.op(dve, lambda: V.tensor_copy(out=parts[:, 1, :], in_=r1[:]), reads=[r1], writes=[parts])
                tt(r1, r1[:], r1, r1[:], parts, parts[:, 1, :], ALU.subtract)
                k.op(dve, lambda: V.tensor_copy(out=parts[:, 2, :], in_=r1[:]), reads=[r1], writes=[parts])
                ts(parts, parts[:, 3:6, :], parts, parts[:, 0:3, :], -1.0, ALU.mult)
                k.dma(sp, cscr[:], parts[:], reads=[parts], writes=[cscr])

            if stop_after == "cs":
                k.dma(sp, dbg["d_o"].t[0:96, :].rearrange("(h k) t -> h k t", k=6), cscr[:], reads=[cscr], writes=[dbg["d_o"]])
                return done([dbg["d_o"]])
            with Scope(k) as eF:
                NH = 4
                wst_box[0] = k.sb("wstF", [128, 16 * 256], F32, es=eF)
                FQ = k.sb("FQ", [128, NH, T], BF16, es=eF)
                FK = k.sb("FK", [128, NH, T], BF16, es=eF)
                k.op(pool, lambda: G.memset(FQ[:], 0.0), writes=[FQ])
                k.op(pool, lambda: G.memset(FK[:], 0.0), writes=[FK])
                FV = k.sb("FV", [128, NT, NH, 65], BF16, es=eF)
                OJ = k.sb("OJ", [128, NT, NH * 64], BF16, es=eF)
                wq = [k.sb(f"wq{i}", [128, 16, 128], BF16, es=eF) for i in range(2)]
                wv = k.sb("wv", [128, 16, NH * 64], BF16, es=eF)
                pt_sb = [k.sb(f"pt_sb{i}", [128, 512], BF16, es=eF) for i in range(3)]
                rz = k.sb("rz", [128, 8], F32, es=eF)
                of32s = [k.sb(f"of32_{i}", [128, 64], F32, es=eF) for i in range(2)]
                FQh = [k.view(FQ, f"FQ{h}") for h in range(NH)]
                FKh = [k.view(FK, f"FK{h}") for h in range(NH)]
                FQc = k.view(FQ, "FQc"); FKc = k.view(FK, "FKc")
                for v_ in FQh + [FQc]:
                    v_.w = dict(FQ.w)
                for v_ in FKh + [FKc]:
                    v_.w = dict(FK.w)
                k.op(dve, lambda: V.memset(FV[:, :, :, 64:65], 1.0), writes=[FV])
                POb = PB[3:7]
                for fp in range(16 // NH):
                    h0 = fp * NH
                    for pj in range(NH // 2):
                        h = h0 + 2 * pj
                        for wb, col, DST, DSTh, sc in [(wq[0], C_FQ, FQ, FQh, 0.125), (wq[1], C_FK, FK, FKh, None)]:
                            load_w(wb, 0, col + h * 64, 128)
                            for tc in range(4):
                                pb = PB[rot[0] % 3]; rot[0] += 1
                                for c in range(16):
                                    mm(pb, pb[:, :], wb, wb[:, c, :], xnT, xnT[:, c, tc * 512:(tc + 1) * 512], c == 0, c == 15)
                                evac(act, DST[0:64, 2 * pj, tc * 512:(tc + 1) * 512], pb[0:64, :], [pb], [DSTh[2 * pj]], sc)
                                evac(dve, DST[64:128, 2 * pj + 1, tc * 512:(tc + 1) * 512], pb[64:128, :], [pb], [DSTh[2 * pj + 1]], sc)
                    load_w(wv, 0, C_FV + h0 * 64, NH * 64)
                    for i in range(NT):
                        pb = PB[rot[0] % 3]; rot[0] += 1
                        for c in range(16):
                            mm(pb, pb[:, 0:NH * 64], xnT, xnT[:, c, i * 128:(i + 1) * 128], wv, wv[:, c, :], c == 0, c == 15)
                        evac(act if i % 2 else dve, FV[:, i, :, 0:64], pb[:, 0:NH * 64].rearrange("p (h d) -> p h d", h=NH), [pb], [FV])
                    for par, r0 in [(0, 64), (1, 0)]:
                        hs = slice(h0 + par, h0 + NH, 2); ls = slice(par, NH, 2); nh2 = NH // 2
                        k.dma(sp, FQ[r0:r0 + 3, ls, :], cscr.t[hs, 0:3, :].rearrange("h k t -> k h t"), reads=[cscr], writes=[FQc])
                        k.dma(sp, FQ[r0 + 3:r0 + 6, ls, :], CD["ones_d"].t[:, 0:nh2, :], reads=[CD["ones_d"]], writes=[FQc])
                        k.dma(sp, FK[r0:r0 + 3, ls, :], CD["ones_d"].t[:, 0:nh2, :], reads=[CD["ones_d"]], writes=[FKc])
                        k.dma(sp, FK[r0 + 3:r0 + 6, ls, :], cscr.t[hs, 3:6, :].rearrange("h k t -> k h t"), reads=[cscr], writes=[FKc])
                    recs = [(hl, qc, kb) for hl in range(NH) for qc in range(4) for kb in range(4 * qc + 4)]

                    def geom(rec):
                        hl, qc, kb = rec
                        dg = kb - 4 * qc
                        q0 = qc * 512 + (dg * 128 if dg > 0 else 0)
                        return hl, qc, kb, dg, q0, (qc + 1) * 512 - q0

                    def fox_qk(rec, slot):
                        hl, qc, kb, dg, q0, n = geom(rec)
                        pb = PB[slot % 3]
                        mm(pb, pb[:, 0:n], FKh[hl], FK[:, hl, kb * 128:(kb + 1) * 128], FQh[hl], FQ[:, hl, q0:q0 + n],
                           True, dg < 0, extra_reads=[FQc, FKc])
                        if dg >= 0:
                            mm(pb, pb[:, 0:128], ident_bf, ident_bf[:], caus, caus[:], False, True)

                    def fox_pv(rec, slot):
                        hl, qc, kb, dg, q0, n = geom(rec)
                        pb = PB[slot % 3]; ptb = pt_sb[slot % 3]
                        k.op(act, lambda: S.activation(out=ptb[:, 0:n], in_=pb[:, 0:n], func=AF.Exp), reads=[pb], writes=[ptb])
                        j0 = max(dg, 0)
                        for jq in range(j0, 4):
                            po = POb[jq]
                            cs = (jq - j0) * 128
                            mm(po, po[:, 0:65], ptb, ptb[:, cs:cs + 128], FV, FV[:, kb, hl, :], kb == 0, kb == 4 * qc + jq)
                        if kb == 4 * qc + 3:
                            for jq in range(4):
                                po = POb[jq]; i = qc * 4 + jq
                                k.op(dve, lambda: V.reciprocal(out=rz[:, jq:jq + 1], in_=po[:, 64:65]), reads=[po], writes=[rz])
                                of = of32s[jq % 2]
                                ts(of, of[:], po, po[:, 0:64], rz[:, jq:jq + 1], ALU.mult, sreads=[rz])
                                k.op(act, lambda: S.activation(out=junk[:, 0:64], in_=of[:], func=AF.Square, accum_out=sstmp[:, 0:1]),
                                     reads=[of], writes=[junk, sstmp])
                                tt(ssf, ssf[:, i:i + 1], ssf, ssf[:, i:i + 1], sstmp, sstmp[:, 0:1], ALU.add)
                                k.op(dve, lambda: V.tensor_copy(out=OJ[:, i, hl * 64:(hl + 1) * 64], in_=of[:]), reads=[of], writes=[OJ])
                            if qc % 2 == 1:
                                precast_step(1)

                    fox_qk(recs[0], 0)
                    for t_ in range(len(recs)):
                        if t_ + 1 < len(recs):
                            fox_qk(recs[t_ + 1], t_ + 1)
                        fox_pv(recs[t_], t_)
                    for i in range(NT):
                        c0 = 1024 + h0 * 64
                        o_store(o_scr[i * 128:(i + 1) * 128, c0:c0 + NH * 64], OJ, OJ[:, i, :])

            if stop_after == "fox":
                k.dma(sp, dbg["d_o"][:], o_scr[:], reads=o_views, writes=[dbg["d_o"]])
                return done([dbg["d_o"]])

            with Scope(k) as eN:
                wst_box[0] = k.sb("wstN", [128, 16 * 144], F32, es=eN)
                QT = k.sb("QT", [128, 4, T], BF16, es=eN)
                KTS = k.sb("KTS", [128, T], BF16, es=eN)
                KTW = k.sb("KTW", [128, T], BF16, es=eN)
                k.op(dve, lambda: V.memset(QT[:], 0.0), writes=[QT])
                for b_ in (KTS, KTW):
                    k.op(pool, lambda: G.memset(b_[:], 0.0), writes=[b_])
                CK = k.sb("CK", [64, T], BF16, es=eN)
                CV = k.sb("CV", [64, T], BF16, es=eN)
                VS = k.sb("VS", [128, NT, 65], BF16, es=eN)
                VW = k.sb("VW", [128, NT, 65], BF16, es=eN)
                GT = k.sb("GT", [128, NT, 12], F32, es=eN)
                W1K = k.sb("W1K", [64, 32, 128], BF16, es=eN)
                W1V = k.sb("W1V", [64, 32, 128], BF16, es=eN)
                W2K = k.sb("W2K", [128, 64], BF16, es=eN)
                W2V = k.sb("W2V", [128, 64], BF16, es=eN)
                pe_ld = k.sb("pe_ld", [32, 128], F32, es=eN)
                PET = k.sb("PET", [64, 2, 32], BF16, es=eN)
                b1 = k.sb("b1", [128, 2], F32, es=eN)
                HK = k.sb("HK", [128, 128], BF16, es=eN)
                HV = k.sb("HV", [128, 128], BF16, es=eN)
                KC = k.sb("KC", [128, 128], BF16, es=eN)
                VCX = k.sb("VCX", [128, 97], BF16, es=eN)
                B0 = k.sb("B0", [128, 4, 128], BF16, es=eN)
                B1 = k.sb("B1", [128, 4, 128], BF16, es=eN)
                W4X = k.sb("W4X", [128, 4, 128], BF16, es=eN)
                CB = [k.sb(f"CB{i}", [128, 4, 128], BF16, es=eN) for i in range(2)]
                NM4 = [k.sb(f"NM4{i}", [128, 4, 128], BF16, es=eN) for i in range(2)]
                emat = k.sb("emat", [128, T], BF16, es=eN)
                for b_ in NM4 + [emat]:
                    k.op(pool, lambda: G.memset(b_[:], 0.0), writes=[b_])
                selvalid = k.sb("selvalid", [128, 8, 32], F32, es=eN)
                seladd = k.sb("seladd", [128, 8, 32], F32, es=eN)
                OJn = k.sb("OJn", [128, NT, 256], BF16, es=eN)
                OA = k.sb("OA", [128, 4, 64], F32, es=eN)
                wqn = [k.sb(f"wqn{i}", [128, 16, 64], BF16, es=eN) for i in range(2)]
                wtm = k.sb("wtm", [128, 16, 144], BF16, es=eN)
                ptn = [k.sb(f"ptn{i}", [128, 512], BF16, es=eN) for i in range(3)]
                sm = k.sb("sm", [128, 64], F32, es=eN)
                imp = k.sb("imp", [128, 32], F32, es=eN)
                score = k.sb("score", [128, 32], F32, es=eN)
                score2 = k.sb("score2", [128, 32], F32, es=eN)
                mx = k.sb("mx", [128, 16], F32, es=eN)
                negmb = k.sb("negmb", [128, 32], BF16, es=eN)
                gtmp = k.sb("gtmp", [128, 12], F32, es=eN)
                k.dma(sp, emat[0:32, :], CD["emat"][:], reads=[CD["emat"]], writes=[emat])
                for b_, nm in [(selvalid, "selvalid"), (seladd, "seladd")]:
                    k.dma(sp, b_[:], CD[nm][:], reads=[CD[nm]], writes=[b_])
                k.dma(sp, W4X[:, 0, :], CD["w4m"][:], reads=[CD["w4m"]], writes=[W4X])
                for r in range(1, 4):
                    k.op(dve, lambda: V.tensor_copy(out=W4X[:, r, :], in_=W4X[:, 0, :]), reads=[W4X], writes=[W4X])
                wstn = wst_box[0]
                for (Wd, pn) in [(W1K, "cmp_k_w1"), (W1V, "cmp_v_w1")]:
                    for hf in range(2):
                        stv = wstn[0:64, 0:2048].rearrange("p (l h) -> p l h", l=16)
                        k.dma(sp, stv, P[pn].t[0, hf * 1024:(hf + 1) * 1024, :].rearrange("(l d) h -> d l h", d=64), reads=[P[pn]], writes=[wstn])
                        if hf == 0:
                            k.op(dve, lambda: V.tensor_copy(out=Wd[:, hf * 16:(hf + 1) * 16, :], in_=stv), reads=[wstn], writes=[Wd])
                        else:
                            k.op(act, lambda: S.copy(out=Wd[:, hf * 16:(hf + 1) * 16, :], in_=stv), reads=[wstn], writes=[Wd])
                for (Wd, pn) in [(W2K, "cmp_k_w2"), (W2V, "cmp_v_w2")]:
                    k.dma(sp, wstn[:, 0:64], P[pn].t[0], reads=[P[pn]], writes=[wstn])
                    cast(pool, Wd, Wd[:], wstn, wstn[:, 0:64])
                k.dma(sp, pe_ld[:, 0:64], P["cmp_pe_k"].t[0], reads=[P["cmp_pe_k"]], writes=[pe_ld])
                k.dma(sp, pe_ld[:, 64:128], P["cmp_pe_v"].t[0], reads=[P["cmp_pe_v"]], writes=[pe_ld])
                for kv in range(2):
                    transpose(PB[0], PB[0][0:64, 0:32], pe_ld, pe_ld[:, kv * 64:(kv + 1) * 64], ident_f, kp=32)
                    evac(dve, PET[:, kv, :], PB[0][0:64, 0:32], [PB[0]], [PET])
                    W1 = W1K if kv == 0 else W1V
                    for l in range(32):
                        mm(PB[1], PB[1][:, 0:1], W1, W1[:, l, :], PET, PET[:, kv, l:l + 1], l == 0, l == 31)
                    evac(dve, b1[:, kv:kv + 1], PB[1][:, 0:1], [PB[1]], [b1])
                k.op(dve, lambda: V.memset(VS[:, :, 64:65], 1.0), writes=[VS])
                k.op(dve, lambda: V.memset(VW[:, :, 64:65], 1.0), writes=[VW])
                k.op(dve, lambda: V.memset(KC[:], 0.0), writes=[KC])
                k.op(dve, lambda: V.memset(VCX[:], 0.0), writes=[VCX])
                k.op(dve, lambda: V.memset(VCX[:, 64:65], 1.0), writes=[VCX])
                k.dma(sp, VCX[:, 65:97], CD["amat"][:], reads=[CD["amat"]], writes=[VCX])
                POb = PB[3:7]

                if stop_after == "nsa0":
                    k.dma(sp, dbg["d_o"][:], o_scr[:], reads=o_views, writes=[dbg["d_o"]])
                    return done([dbg["d_o"]])
                for g in range(4):
                    for r in range(4):
                        wb = wqn[r % 2]
                        load_w(wb, 0, C_NQ + (4 * g + r) * 64, 64)
                        proj_fm(wb, 64, QT, lambda tc: QT[0:64, r, tc * 512:(tc + 1) * 512], 0.125)
                    for ii, (col, dst) in enumerate([(C_KSLC, KTS), (C_KWIN, KTW), (C_KCMP, CK), (C_VCMP, CV)]):
                        wb = wqn[ii % 2]
                        load_w(wb, 0, col + g * 64, 64)
                        proj_fm(wb, 64, dst, lambda tc: dst[0:64, tc * 512:(tc + 1) * 512], None)
                    if os.environ.get("NSA_SKIP") == "qk":
                        k.dma(sp, dbg["d_o"][:], o_scr[:], reads=o_views, writes=[dbg["d_o"]])
                        return done([dbg["d_o"]])
                    wstn_ = wst_box[0]
                    stv_all = wstn_[:, 0:16 * 144].rearrange("p (c n) -> p c n", c=16)
                    for (c0_, col_, n_) in [(0, C_VSLC + g * 64, 64), (64, C_VWIN + g * 64, 64), (128, C_GATE + g * 12, 16)]:
                        k.dma(sp, stv_all[:, :, c0_:c0_ + n_], P["w_in"].t[0, :, col_:col_ + n_].rearrange("(c p) n -> p c n", p=128),
                              reads=[P["w_in"]], writes=[wstn_])
                    cast(pool, wtm, wtm[:], wstn_, stv_all)
                    for i in range(NT):
                        pb = PB[rot[0] % 3]; rot[0] += 1
                        for c in range(16):
                            mm(pb, pb[:, 0:144], xnT, xnT[:, c, i * 128:(i + 1) * 128], wtm, wtm[:, c, :], c == 0, c == 15)
                        evac(dve, VS[:, i, 0:64], pb[:, 0:64], [pb], [VS])
                        evac(dve, VW[:, i, 0:64], pb[:, 64:128], [pb], [VW])
                        evac(dve, gtmp[:], pb[:, 128:140], [pb], [gtmp])
                        k.op(act, lambda: S.activation(out=gtmp[:], in_=gtmp[:], func=AF.Exp, scale=-1.0), reads=[gtmp], writes=[gtmp])
                        ts(gtmp, gtmp[:], gtmp, gtmp[:], 1.0, ALU.add)
                        k.op(dve, lambda: V.reciprocal(out=GT[:, i, :], in_=gtmp[:]), reads=[gtmp], writes=[GT])
                    if os.environ.get("NSA_SKIP") == "proj":
                        k.dma(sp, dbg["d_o"][:], o_scr[:], reads=o_views, writes=[dbg["d_o"]])
                        return done([dbg["d_o"]])
                    for kv, (SRC, W1, H) in enumerate([(CK, W1K, HK), (CV, W1V, HV)]):
                        pb = PB[rot[0] % 3]; rot[0] += 1
                        for l in range(32):
                            mm(pb, pb[:, 0:127], W1, W1[:, l, :], SRC, SRC[:, l:l + 2017:16], l == 0, l == 31)
                        k.op(act, lambda: S.activation(out=H[:, 0:127], in_=pb[:, 0:127], func=AF.Silu, bias=b1[:, kv:kv + 1]),
                             reads=[pb, b1], writes=[H])
                    pb = PB[rot[0] % 3]; rot[0] += 1
                    mm(pb, pb[0:64, 0:127], W2K, W2K[:], HK, HK[:, 0:127], True, True)
                    evac(dve, KC[0:64, 0:127], pb[0:64, 0:127], [pb], [KC])
                    pb = PB[rot[0] % 3]; rot[0] += 1
                    mm(pb, pb[0:127, 0:64], HV, HV[:, 0:127], W2V, W2V[:], True, True)
                    evac(dve, VCX[0:127, 0:64], pb[0:127, 0:64], [pb], [VCX])
                    base = (4 * g) * 128 * VLEN + VOFF
                    if os.environ.get("NSA_SKIP") == "cmp":
                        k.dma(sp, dbg["d_o"][:], o_scr[:], reads=o_views, writes=[dbg["d_o"]])
                        return done([dbg["d_o"]])
                    k.dma(sp, B0[:], bass.AP(brd.t, base, [[VLEN - 1, 128], [128 * VLEN, 4], [1, 128]]), reads=[brd], writes=[B0])
                    k.dma(sp, B1[:], bass.AP(brd.t, base + 128, [[VLEN - 1, 128], [128 * VLEN, 4], [1, 128]]), reads=[brd], writes=[B1])

                    if stop_after == "nsa1":
                        k.dma(sp, dbg["d_o"][:], o_scr[:], reads=o_views, writes=[dbg["d_o"]])
                        return done([dbg["d_o"]])

                    def cb_load(qb):
                        cb = CB[qb % 2]
                        k.dma(sp, cb[:], bass.AP(brd.t, base + 128 * qb - 31, [[VLEN - 16, 128], [128 * VLEN, 4], [1, 128]]), reads=[brd], writes=[cb])

                    def n_qk(rec, slot):
                        kind, qb, kb = rec
                        pb = PB[slot % 3]
                        qap = QT[:, :, qb * 128:(qb + 1) * 128]
                        if kind == "cmp":
                            if qb + 1 < NT:
                                cb_load(qb + 1)
                            cb = CB[qb % 2]
                            mm(pb, pb[:, :], KC, KC[:], QT, qap, True, False)
                            mm(pb, pb[:, :], ident_bf, ident_bf[:], cb, cb[:], False, True)
                            return
                        sel = kind == "slc"
                        KT = KTS if sel else KTW
                        extras = []
                        if kb == qb:
                            extras.append((ident_bf, ident_bf[:], B0, B0[:]))
                        elif kb == qb - 1:
                            extras.append((ident_bf, ident_bf[:], B1, B1[:]))
                        if (not sel) and kb == qb - 4:
                            extras.append((ident_bf, ident_bf[:], W4X, W4X[:]))
                        if sel and qb >= 8:
                            nm4 = NM4[qb % 2]
                            extras.append((emat, emat[:, kb * 128:(kb + 1) * 128], nm4, nm4[:]))
                        mm(pb, pb[:, :], KT, KT[:, kb * 128:(kb + 1) * 128], QT, qap, True, len(extras) == 0)
                        for ei, (lb, la, rb, ra) in enumerate(extras):
                            mm(pb, pb[:, :], lb, la, rb, ra, False, ei == len(extras) - 1)

                    def n_pv(rec, slot):
                        kind, qb, kb = rec
                        pb = PB[slot % 3]; ptb = ptn[slot % 3]
                        k.op(act, lambda: S.activation(out=ptb[:], in_=pb[:], func=AF.Exp), reads=[pb], writes=[ptb])
                        if kind == "cmp":
                            po = PB[(slot + 1) % 3]
                            for r in range(4):
                                mm(po, po[:, r * 97:(r + 1) * 97], ptb, ptb[:, r * 128:(r + 1) * 128], VCX, VCX[:], True, True)
                            ts(sm, sm[:, 0:4], po, po[:, 64:64 + 97 * 3 + 1:97], 1e-30, ALU.max)
                            k.op(dve, lambda: V.reciprocal(out=sm[:, 4:8], in_=sm[:, 0:4]), reads=[sm], writes=[sm])
                            tt(sm, sm[:, 8:12], sm, sm[:, 4:8], GT, GT[:, qb, 0:12:3], ALU.mult)
                            for r in range(4):
                                ts(OA, OA[:, r, :], po, po[:, r * 97:r * 97 + 64], sm[:, 8 + r:9 + r], ALU.mult, sreads=[sm])
                            if qb >= 8:
                                ts(imp, imp[:], po, po[:, 65:97], sm[:, 4:5], ALU.mult, sreads=[sm])
                                for r in range(1, 4):
                                    stt(imp, imp[:], po, po[:, r * 97 + 65:r * 97 + 97], sm[:, 4 + r:5 + r], imp, imp[:], ALU.mult, ALU.add, sreads=[sm])
                                tt(score, score[:], imp, imp[:], selvalid, selvalid[:, qb - 8, :], ALU.mult)
                                tt(score, score[:], score, score[:], seladd, seladd[:, qb - 8, :], ALU.add)
                                k.op(dve, lambda: V.max(out=mx[:, 0:8], in_=score[:]), reads=[score], writes=[mx])
                                k.op(dve, lambda: V.match_replace(out=score2[:], in_to_replace=mx[:, 0:8], in_values=score[:], imm_value=-3.0e38),
                                     reads=[score, mx], writes=[score2])
                                k.op(dve, lambda: V.max(out=mx[:, 8:16], in_=score2[:]), reads=[score2], writes=[mx])
                                ts(negmb, negmb[:], score, score[:], mx[:, 15:16], ALU.is_lt, NEG, ALU.mult, sreads=[mx])
                                transpose(PT, PT[0:32, 0:128], negmb, negmb[:], ident_bf)
                                nm4 = NM4[qb % 2]
                                for r in range(4):
                                    evac(dve if r % 2 else act, nm4[0:32, r, :], PT[0:32, 0:128], [PT], [nm4])
                            return
                        sel = kind == "slc"
                        VT = VS if sel else VW
                        kb_lo = 0 if sel else max(0, qb - 4)
                        for r in range(4):
                            mm(POb[r], POb[r][:, 0:65], ptb, ptb[:, r * 128:(r + 1) * 128], VT, VT[:, kb, :], kb == kb_lo, kb == qb)
                        if kb == qb:
                            gate_off = 1 if sel else 2
                            o0 = 16 if sel else 24
                            for r in range(4):
                                k.op(dve, lambda: V.reciprocal(out=sm[:, o0 + r:o0 + r + 1], in_=POb[r][:, 64:65]), reads=[POb[r]], writes=[sm])
                            tt(sm, sm[:, o0 + 4:o0 + 8], sm, sm[:, o0:o0 + 4], GT, GT[:, qb, gate_off:12:3], ALU.mult)
                            for r in range(4):
                                stt(OA, OA[:, r, :], POb[r], POb[r][:, 0:64], sm[:, o0 + 4 + r:o0 + 5 + r], OA, OA[:, r, :], ALU.mult, ALU.add, sreads=[sm])
                            if sel:
                                k.op(act, lambda: S.activation(out=junk[:, 0:256], in_=OA[:].rearrange("p r d -> p (r d)"), func=AF.Square,
                                                               accum_out=sstmp[:, 1:2]), reads=[OA], writes=[junk, sstmp])
                                tt(ssn, ssn[:, qb:qb + 1], ssn, ssn[:, qb:qb + 1], sstmp, sstmp[:, 1:2], ALU.add)
                                k.op(dve, lambda: V.tensor_copy(out=OJn[:, qb, :], in_=OA[:].rearrange("p r d -> p (r d)")), reads=[OA], writes=[OJn])
                                precast_step(1)

                    recs = []
                    for qb in range(NT):
                        recs.append(("cmp", qb, 0))
                        recs += [("win", qb, kb) for kb in range(max(0, qb - 4), qb + 1)]
                        recs += [("slc", qb, kb) for kb in range(0, qb + 1)]
                    slots = []
                    sl_ = 0
                    for rec in recs:
                        slots.append(sl_)
                        sl_ += 2 if rec[0] == "cmp" else 1
                    cb_load(0)
                    n_qk(recs[0], slots[0])
                    for t_ in range(len(recs)):
                        if t_ + 1 < len(recs):
                            n_qk(recs[t_ + 1], slots[t_ + 1])
                        n_pv(recs[t_], slots[t_])
                    for i in range(NT):
                        o_store(o_scr[i * 128:(i + 1) * 128, g * 256:(g + 1) * 256], OJn, OJn[:, i, :])

        if stop_after == "nsa":
            k.dma(sp, dbg["d_o"][:], o_scr[:], reads=o_views, writes=[dbg["d_o"]])
            return done([dbg["d_o"]])

        OH1a = k.sb("OH1a", [128, NT, 32], F32)
        OH2a = k.sb("OH2a", [128, NT, 32], F32)
        SELb = k.sb("SELb", [128, NT, 32], BF16)
        Wk = k.sb("Wk", [128, NT, 2], F32)
        ROWI = k.sb("ROWI", [128, NT, 2], I32)
        with Scope(k) as eO:
            WO = k.sb("WO", [128, 16, D], BF16, es=eO)
            onw_ld = k.sb("onw_ld", [16, 128], F32, es=eO)
            onw_col = k.sb("onw_col", [128, 16], F32, es=eO)
            rs_n = k.sb("rs_n", [128, NT], F32, es=eO)
            rs_f = k.sb("rs_f", [128, NT], F32, es=eO)
            fnw_bc = k.sb("fnw_bc", [128, D], F32, es=eO)
            WR = k.sb("WR", [128, 16, 36], F32, es=eO)
            RB = k.sb("RB", [128, 36], F32, es=eO)
            ot = [k.sb(f"ot{i}", [128, D], BF16, es=eO) for i in range(2)]
            oT = k.sb("oT", [128, 16, 128], BF16, es=eO)
            xs2 = [k.sb(f"xs2{i}", [128, D], F32, es=eO) for i in range(2)]
            h1t = k.sb("h1t", [128, D], F32, es=eO)
            hn32 = k.sb("hn32", [128, D], F32, es=eO)
            hnb = k.sb("hnb", [128, D], BF16, es=eO)
            hnT = k.sb("hnT", [128, 16, 128], F32, es=eO)
            lg = k.sb("lg", [128, 36], F32, es=eO)
            rt = k.sb("rt", [128, 64], F32, es=eO)
            elg = k.sb("elg", [128, 8], F32, es=eO)
            oh = k.sb("oh", [128, 24], F32, es=eO)
            wso = [k.sb(f"wso{i}", [128, D], F32, es=eO) for i in range(2)]
            for c in range(16):
                k.dma(sp, wso[c % 2][:], P["w_out"].t[0, c * 128:(c + 1) * 128, :], reads=[P["w_out"]], writes=[wso[c % 2]])
                k.op(pool, lambda: G.tensor_copy(out=WO[:, c, :], in_=wso[c % 2][:]), reads=[wso[c % 2]], writes=[WO])
            k.dma(sp, onw_ld[0:8, :], P["nsa_out_norm_w"].t[0].rearrange("(c p) -> c p", p=128), reads=[P["nsa_out_norm_w"]], writes=[onw_ld])
            k.dma(sp, onw_ld[8:16, :], P["fox_out_norm_w"].t[0].rearrange("(c p) -> c p", p=128), reads=[P["fox_out_norm_w"]], writes=[onw_ld])
            transpose(PB[0], PB[0][:, 0:16], onw_ld, onw_ld[:], ident_f, kp=16)
            evac(dve, onw_col[:], PB[0][:, 0:16], [PB[0]], [onw_col])
            k.dma(sp, fnw_bc[:], bass.AP(P["ffn_norm_w"].t, 0, [[0, 128], [1, D]]), reads=[P["ffn_norm_w"]], writes=[fnw_bc])
            with nc.allow_non_contiguous_dma(reason="small router weights"):
                k.dma(sp, WR[:, :, 0:4], P["router_group_w"].t[0].rearrange("(c p) n -> p c n", p=128), reads=[P["router_group_w"]], writes=[WR])
                k.dma(sp, WR[:, :, 4:36], P["router_expert_w"].t[0].rearrange("(c p) n -> p c n", p=128), reads=[P["router_expert_w"]], writes=[WR])
            k.dma(sp, RB[:, 0:4], bass.AP(P["router_group_b"].t, 0, [[0, 128], [1, 4]]), reads=[P["router_group_b"]], writes=[RB])
            k.dma(sp, RB[:, 4:36], bass.AP(P["router_expert_b"].t, 0, [[0, 128], [1, 32]]), reads=[P["router_expert_b"]], writes=[RB])
            for i in range(NT):
                rstd_from_ss(ssn[:, i:i + 1], ssn, rs_n[:, i:i + 1], rs_n, 1024)
                rstd_from_ss(ssf[:, i:i + 1], ssf, rs_f[:, i:i + 1], rs_f, 1024)
            for i in range(NT):
                o_t = ot[i % 2]; xs = xs2[i % 2]
                k.dma(sp, o_t[:], o_scr[i * 128:(i + 1) * 128, :], reads=o_views, writes=[o_t])
                k.dma(sp, xs[:], x_d[i * 128:(i + 1) * 128, :], reads=[x_d], writes=[xs])
                for c4 in range(4):
                    for cc in range(4):
                        c = c4 * 4 + cc
                        transpose(PT, PT[:, cc * 128:(cc + 1) * 128], o_t, o_t[:, c * 128:(c + 1) * 128], ident_bf)
                    for cc in range(4):
                        c = c4 * 4 + cc
                        ts(oT, oT[:, c, :], PT, PT[:, cc * 128:(cc + 1) * 128], onw_col[:, c:c + 1], ALU.mult, sreads=[onw_col])
                for dmb in range(4):
                    pn = PB[(2 * dmb) % 4]; pf = PB[(2 * dmb + 1) % 4]
                    for c in range(8):
                        mm(pn, pn[:], oT, oT[:, c, :], WO, WO[:, c, dmb * 512:(dmb + 1) * 512], c == 0, c == 7)
                    for c in range(8, 16):
                        mm(pf, pf[:], oT, oT[:, c, :], WO, WO[:, c, dmb * 512:(dmb + 1) * 512], c == 8, c == 15)
                    sl = slice(dmb * 512, (dmb + 1) * 512)
                    stt(h1t, h1t[:, sl], pn, pn[:], rs_n[:, i:i + 1], xs, xs[:, sl], ALU.mult, ALU.add, sreads=[rs_n])
                    stt(h1t, h1t[:, sl], pf, pf[:], rs_f[:, i:i + 1], h1t, h1t[:, sl], ALU.mult, ALU.add, sreads=[rs_f])
                k.dma(sp, h1_scr[i * 128:(i + 1) * 128, :], h1t[:], reads=[h1t], writes=[h1_scr])
                k.op(act, lambda: S.activation(out=junk[:], in_=h1t[:], func=AF.Square, accum_out=sstmp[:, 0:1]), reads=[h1t], writes=[junk, sstmp])
                rstd_from_ss(sstmp[:, 0:1], sstmp, rt[:, 0:1], rt, D)
                stt(hn32, hn32[:], h1t, h1t[:], rt[:, 0:1], fnw_bc, fnw_bc[:], ALU.mult, ALU.mult, sreads=[rt])
                k.op(act, lambda: S.copy(out=hnb[:], in_=hn32[:]), reads=[hn32], writes=[hnb])
                k.dma(sp, hn_scr[i * 128:(i + 1) * 128, :], hnb[:], reads=[hnb], writes=[hn_scr])
                for c4 in range(4):
                    pb = PB[4 + c4 % 2]
                    for cc in range(4):
                        c = c4 * 4 + cc
                        transpose(pb, pb[:, cc * 128:(cc + 1) * 128], hn32, hn32[:, c * 128:(c + 1) * 128], ident_f)
                    evac(act if c4 % 2 else dve, hnT[:, c4 * 4:(c4 + 1) * 4, :], pb[:].rearrange("p (c t) -> p c t", c=4), [pb], [hnT])
                pl = PB[6]
                for c in range(16):
                    mm(pl, pl[:, 0:36], hnT, hnT[:, c, :], WR, WR[:, c, :], c == 0, c == 15)
                tt(lg, lg[:], pl, pl[:, 0:36], RB, RB[:], ALU.add)
                k.op(dve, lambda: V.reduce_max(out=rt[:, 1:2], in_=lg[:, 0:4], axis=AX.X), reads=[lg], writes=[rt])
                ts(oh, oh[:, 0:4], lg, lg[:, 0:4], rt[:, 1:2], ALU.is_ge, sreads=[rt])
                ts(rt, rt[:, 2:3], rt, rt[:, 1:2], -1.0, ALU.mult)
                k.op(act, lambda: S.activation(out=rt[:, 8:12], in_=lg[:, 0:4], func=AF.Exp, bias=rt[:, 2:3], accum_out=rt[:, 3:4]),
                     reads=[lg, rt], writes=[rt])
                k.op(dve, lambda: V.reciprocal(out=rt[:, 4:5], in_=rt[:, 3:4]), reads=[rt], writes=[rt])
                ts(elg, elg[:], lg, lg[:, 4:12], oh[:, 0:1], ALU.mult, sreads=[oh])
                for gg in range(1, 4):
                    stt(elg, elg[:], lg, lg[:, 4 + gg * 8:12 + gg * 8], oh[:, gg:gg + 1], elg, elg[:], ALU.mult, ALU.add, sreads=[oh])
                k.op(dve, lambda: V.reduce_max(out=rt[:, 5:6], in_=elg[:], axis=AX.X), reads=[elg], writes=[rt])
                ts(oh, oh[:, 8:16], elg, elg[:], rt[:, 5:6], ALU.is_ge, sreads=[rt])
                stt(elg, elg[:], oh, oh[:, 8:16], -1.0e30, elg, elg[:], ALU.mult, ALU.add)
                k.op(dve, lambda: V.reduce_max(out=rt[:, 6:7], in_=elg[:], axis=AX.X), reads=[elg], writes=[rt])
                ts(oh, oh[:, 16:24], elg, elg[:], rt[:, 6:7], ALU.is_ge, sreads=[rt])
                tt(rt, rt[:, 7:8], rt, rt[:, 6:7], rt, rt[:, 5:6], ALU.subtract)
                k.op(act, lambda: S.activation(out=rt[:, 12:13], in_=rt[:, 7:8], func=AF.Exp), reads=[rt], writes=[rt])
                ts(rt, rt[:, 13:14], rt, rt[:, 12:13], 1.0, ALU.add)
                k.op(dve, lambda: V.reciprocal(out=rt[:, 14:15], in_=rt[:, 13:14]), reads=[rt], writes=[rt])
                tt(Wk, Wk[:, i, 0:1], rt, rt[:, 14:15], rt, rt[:, 4:5], ALU.mult)
                tt(rt, rt[:, 15:16], rt, rt[:, 14:15], rt, rt[:, 12:13], ALU.mult)
                tt(Wk, Wk[:, i, 1:2], rt, rt[:, 15:16], rt, rt[:, 4:5], ALU.mult)
                for gg in range(4):
                    ts(OH1a, OH1a[:, i, gg * 8:(gg + 1) * 8], oh, oh[:, 8:16], oh[:, gg:gg + 1], ALU.mult, sreads=[oh])
                    ts(OH2a, OH2a[:, i, gg * 8:(gg + 1) * 8], oh, oh[:, 16:24], oh[:, gg:gg + 1], ALU.mult, sreads=[oh])
                tt(SELb, SELb[:, i, :], OH1a, OH1a[:, i, :], OH2a, OH2a[:, i, :], ALU.add)

        if stop_after == "oproj":
            k.dma(sp, dbg["d_h1"][:], h1_scr[:], reads=[h1_scr], writes=[dbg["d_h1"]])
            return done([dbg["d_h1"]])

        IDXI = k.sb("IDXI", [128, NSLOT], I32)
        x_views = []
        with Scope(k) as eR:
            onesb = k.sb("onesb", [128, 128], BF16, es=eR)
            stri = k.sb("stri", [128, 128], BF16, es=eR)
            ncnt = k.sb("ncnt", [128, 32], F32, es=eR)
            tl = k.sb("tl", [128, 32], F32, es=eR)
            cA = k.sb("cA", [128, 32], F32, es=eR)
            cBb = k.sb("cBb", [128, 32], F32, es=eR)
            basef = k.sb("basef", [128, 32], F32, es=eR)
            rowf = k.sb("rowf", [128, 32], F32, es=eR)
            tmp32 = k.sb("tmp32", [128, 32], F32, es=eR)
            ROWF = k.sb("ROWF", [128, NT, 2], F32, es=eR)
            esl_f = k.sb("esl_f", [1, NSLOT + 1], F32, es=eR)
            nfl_f = k.sb("nfl_f", [1, NSLOT], F32, es=eR)
            k.op(dve, lambda: V.memset(onesb[:], 1.0), writes=[onesb])
            k.dma(sp, stri[:], CD["stri"][:], reads=[CD["stri"]], writes=[stri])
            pcnt = PB[0]
            for i in range(NT):
                mm(pcnt, pcnt[:, 0:32], onesb, onesb[:], SELb, SELb[:, i, :], i == 0, i == NT - 1)
            evac(dve, ncnt[:], pcnt[:, 0:32], [pcnt], [ncnt])
            k.op(dve, lambda: V.memset(tl[:], 0.0), writes=[tl])
            for j in range(16):
                stt(tl, tl[:], ncnt, ncnt[:], float(128 * j), tl, tl[:], ALU.is_gt, ALU.add)
            k.op(dve, lambda: V.tensor_copy(out=cA[:], in_=tl[:]), reads=[tl], writes=[cA])
            src, dst = cA, cBb
            for sft in [1, 2, 4, 8, 16]:
                k.op(dve, lambda: V.tensor_copy(out=dst[:, 0:sft], in_=src[:, 0:sft]), reads=[src], writes=[dst])
                tt(dst, dst[:, sft:32], src, src[:, sft:32], src, src[:, 0:32 - sft], ALU.add)
                src, dst = dst, src
            cum = src
            tt(basef, basef[:], cum, cum[:], tl, tl[:], ALU.subtract)
            ts(basef, basef[:], basef, basef[:], 128.0, ALU.mult)
            for i in range(NT):
                pp = PB[1 + i % 2]
                for j in range(i):
                    mm(pp, pp[:, 0:32], onesb, onesb[:], SELb, SELb[:, j, :], j == 0, False)
                mm(pp, pp[:, 0:32], stri, stri[:], SELb, SELb[:, i, :], i == 0, True)
                tt(rowf, rowf[:], pp, pp[:, 0:32], basef, basef[:], ALU.add)
                for kk, OH in enumerate([OH1a, OH2a]):
                    tt(tmp32, tmp32[:], rowf, rowf[:], OH, OH[:, i, :], ALU.mult)
                    k.op(dve, lambda: V.reduce_sum(out=ROWF[:, i, kk:kk + 1], in_=tmp32[:], axis=AX.X), reads=[tmp32], writes=[ROWF])
            k.op(dve, lambda: V.tensor_copy(out=ROWI[:], in_=ROWF[:]), reads=[ROWF], writes=[ROWI])
            k.op(dve, lambda: V.memset(esl_f[:], -1.0), writes=[esl_f])
            for s in range(NSLOT):
                k.op(dve, lambda: V.tensor_scalar(out=tmp32[0:1, :], in0=cum[0:1, :], scalar1=float(s), scalar2=None, op0=ALU.is_le,
                                                  op1=ALU.add, accum_out=esl_f[0:1, s + 1:s + 2]), reads=[cum], writes=[tmp32, esl_f])
            ts(esl_f, esl_f[0:1, 1:NSLOT + 1], esl_f, esl_f[0:1, 1:NSLOT + 1], 31.0, ALU.min)
            k.op(dve, lambda: V.memset(nfl_f[:], 1.0), writes=[nfl_f])
            tt(nfl_f, nfl_f[0:1, 2:NSLOT], esl_f, esl_f[0:1, 3:NSLOT + 1], esl_f, esl_f[0:1, 1:NSLOT - 1], ALU.not_equal)
            onesrow = k.sb("onesrow", [1, 128], F32, es=eR)
            iop = k.sb("iop", [128, 1], F32, es=eR)
            idxf = k.sb("idxf", [128, NSLOT], F32, es=eR)
            k.op(dve, lambda: V.memset(onesrow[:], 1.0), writes=[onesrow])
            k.dma(sp, iop[:], CD["iota_p"][:], reads=[CD["iota_p"]], writes=[iop])
            mm(PB[3], PB[3][:, 0:NSLOT], onesrow, onesrow[:], esl_f, esl_f[0:1, 1:NSLOT + 1], True, True)
            mm(PB[4], PB[4][:, 0:NSLOT], onesrow, onesrow[:], nfl_f, nfl_f[:], True, True)
            ts(idxf, idxf[:], PB[3], PB[3][:, 0:NSLOT], 128.0, ALU.mult, iop[:, 0:1], ALU.add, sreads=[iop])
            ts(idxf, idxf[:], idxf, idxf[:], -100000.0, ALU.add)
            tt(idxf, idxf[:], idxf, idxf[:], PB[4], PB[4][:, 0:NSLOT], ALU.mult)
            ts(idxf, idxf[:], idxf, idxf[:], 100000.0, ALU.add)
            k.op(dve, lambda: V.tensor_copy(out=IDXI[:], in_=idxf[:]), reads=[idxf], writes=[IDXI])
            hb = [k.sb(f"hb{i}", [128, D], BF16, es=eR) for i in range(2)]
            for i in range(NT):
                hbt = hb[i % 2]
                k.dma(sp, hbt[:], hn_scr[i * 128:(i + 1) * 128, :], reads=[hn_scr], writes=[hbt])
                for kk in range(2):
                    xv_ = k.view(xslot, "xs_st"); x_views.append(xv_)
                    k.dma(pool, None, None, reads=[hbt, ROWI], writes=[xv_],
                          fn=lambda: G.indirect_dma_start(out=xslot[:], out_offset=bass.IndirectOffsetOnAxis(ap=ROWI[:, i, kk:kk + 1], axis=0),
                                                          in_=hbt[:], in_offset=None))

        with Scope(k) as eM:
            WGs = [k.sb(f"WG{i}", [128, 16, 512], BF16, es=eM) for i in range(2)]
            WUs = [k.sb(f"WU{i}", [128, 16, 512], BF16, es=eM) for i in range(2)]
            WDs = [k.sb(f"WD{i}", [128, 4, D], BF16, es=eM) for i in range(2)]
            xgbs = [k.sb(f"xgb{i}", [128, D], BF16, es=eM) for i in range(2)]
            xgTs = [k.sb(f"xgT{i}", [128, 16, 128], BF16, es=eM) for i in range(2)]
            hTs = [k.sb(f"hT{i}", [128, 4, 128], BF16, es=eM) for i in range(2)]
            sgs = [k.sb(f"sg{i}", [128, 128], F32, es=eM) for i in range(2)]
            ybts = [k.sb(f"ybt{i}", [128, D], F32, es=eM) for i in range(2)]
            PTh = [k.view(PT, "PTa"), k.view(PT, "PTb")]
            precast_step(1000)
            bc_reg = G.to_reg(4095)

            def load_w_slot(s):
                for Wb, src in [(WGs[s % 2], wbf["g"]), (WUs[s % 2], wbf["u"]), (WDs[s % 2], wbf["d"])]:
                    src2d = src.t[:].rearrange("e (p c) f -> (e p) (c f)", p=128)
                    k.dma(pool, None, None, reads=pre_views + [IDXI], writes=[Wb],
                          fn=lambda: G.indirect_dma_start(out=Wb[:].rearrange("p c f -> p (c f)"), out_offset=None, in_=src2d,
                                                          in_offset=bass.IndirectOffsetOnAxis(ap=IDXI[:, s:s + 1], axis=0),
                                                          bounds_check=bc_reg, oob_is_err=False))

            def load_x_dma(s):
                xgb = xgbs[s % 2]
                k.dma(sp, xgb[:], xslot[s * 128:(s + 1) * 128, :], reads=x_views, writes=[xgb])

            def load_x_tr(s):
                xgb = xgbs[s % 2]; xgT = xgTs[s % 2]
                for c4 in range(4):
                    for cc in range(4):
                        c = c4 * 4 + cc
                        transpose(PT, PT[:, cc * 128:(cc + 1) * 128], xgb, xgb[:, c:D:16], ident_bf)
                    evac(act if c4 % 2 else dve, xgT[:, c4 * 4:(c4 + 1) * 4, :], PT[:, 0:512].rearrange("p (c t) -> p c t", c=4), [PT], [xgT])

            load_x_dma(0)
            load_x_dma(1)
            load_w_slot(0)
            load_x_tr(0)
            for s in range(NSLOT):
                xgT = xgTs[s % 2]; ybt = ybts[s % 2]; hT = hTs[s % 2]
                WG, WU, WD = WGs[s % 2], WUs[s % 2], WDs[s % 2]
                if s + 1 < NSLOT:
                    load_x_tr(s + 1)
                    if s + 2 < NSLOT:
                        load_x_dma(s + 2)
                    load_w_slot(s + 1)
                for c2 in range(4):
                    pg = PB[(2 * c2) % 4]; pu = PB[(2 * c2 + 1) % 4]; sg = sgs[c2 % 2]
                    for c in range(16):
                        mm(pg, pg[:, 0:128], WG, WG[:, c, c2:512:4], xgT, xgT[:, c, :], c == 0, c == 15)
                    for c in range(16):
                        mm(pu, pu[:, 0:128], WU, WU[:, c, c2:512:4], xgT, xgT[:, c, :], c == 0, c == 15)
                    k.op(act, lambda: S.activation(out=sg[:], in_=pg[:, 0:128], func=AF.Silu), reads=[pg], writes=[sg])
                    tt(hT, hT[:, c2, :], sg, sg[:], pu, pu[:, 0:128], ALU.mult)
                for dmb in range(4):
                    pd = PB[4 + dmb % 3]
                    for c2 in range(4):
                        mm(pd, pd[:], hT, hT[:, c2, :], WD, WD[:, c2, dmb * 512:(dmb + 1) * 512], c2 == 0, c2 == 3)
                    evac(act if dmb % 2 else dve, ybt[:, dmb * 512:(dmb + 1) * 512], pd[:], [pd], [ybt])
                k.dma(sp, yslot[s * 128:(s + 1) * 128, :], ybt[:], reads=[ybt], writes=[yslot])

        with Scope(k) as eZ:
            fw_bc = k.sb("fw_bc", [128, D], F32, es=eZ)
            k.dma(sp, fw_bc[:], bass.AP(P["final_norm_w"].t, 0, [[0, 128], [1, D]]), reads=[P["final_norm_w"]], writes=[fw_bc])
            h1b = [k.sb(f"h1b{i}", [128, D], F32, es=eZ) for i in range(2)]
            g0 = [k.sb(f"g0{i}", [128, D], F32, es=eZ) for i in range(2)]
            g1 = [k.sb(f"g1{i}", [128, D], F32, es=eZ) for i in range(2)]
            ob = [k.sb(f"ob{i}", [128, D], F32, es=eZ) for i in range(2)]
            rf = k.sb("rf", [128, 4], F32, es=eZ)
            for i in range(NT):
                hh = h1b[i % 2]; a0 = g0[i % 2]; a1 = g1[i % 2]; oo = ob[i % 2]
                k.dma(sp, hh[:], h1_scr[i * 128:(i + 1) * 128, :], reads=[h1_scr], writes=[hh])
                for kk, gb in enumerate([a0, a1]):
                    k.dma(pool, None, None, reads=[yslot, ROWI], writes=[gb],
                          fn=lambda: G.indirect_dma_start(out=gb[:], out_offset=None, in_=yslot[:],
                                                          in_offset=bass.IndirectOffsetOnAxis(ap=ROWI[:, i, kk:kk + 1], axis=0)))
                stt(hh, hh[:], a0, a0[:], Wk[:, i, 0:1], hh, hh[:], ALU.mult, ALU.add, sreads=[Wk])
                stt(hh, hh[:], a1, a1[:], Wk[:, i, 1:2], hh, hh[:], ALU.mult, ALU.add, sreads=[Wk])
                k.op(act, lambda: S.activation(out=junk[:], in_=hh[:], func=AF.Square, accum_out=rf[:, 0:1]), reads=[hh], writes=[junk, rf])
                rstd_from_ss(rf[:, 0:1], rf, rf[:, 1:2], rf, D)
                stt(oo, oo[:], hh, hh[:], rf[:, 1:2], fw_bc, fw_bc[:], ALU.mult, ALU.mult, sreads=[rf])
                k.dma(sp, out_d[i * 128:(i + 1) * 128, :], oo[:], reads=[oo], writes=[out_d])
        return done([out_d])


_NC_CACHE = {}


def _in_maps(inputs, n_cores=8):
    consts = _consts()
    maps = []
    for c in range(n_cores):
        m = {"x": np.ascontiguousarray(inputs["x"][c])}
        for name, shape in PARAM_SPECS:
            m[name] = np.ascontiguousarray(np.asarray(inputs[name], dtype=np.float32).reshape(shape))
        for name, shape, dt in CONST_SPECS:
            m["c_" + name] = consts[name]
        maps.append(m)
    return maps


def kernel(**inputs):
    if "nc" not in _NC_CACHE:
        _NC_CACHE["nc"] = build_nc()
    nc = _NC_CACHE["nc"]
    res = run_bass_kernel_spmd(nc, _in_maps(inputs), core_ids=list(range(8)))
    return np.stack([np.asarray(r["out"], dtype=np.float32) for r in res.results], axis=0)
```

```python
import math
import os
from contextlib import ExitStack

import ml_dtypes
import numpy as np

import concourse.bass as bass
import concourse.mybir as mybir
from concourse.bass_utils import run_bass_kernel_spmd

F32 = mybir.dt.float32
BF16 = mybir.dt.bfloat16
I32 = mybir.dt.int32
AF = mybir.ActivationFunctionType
ALU = mybir.AluOpType
AX = mybir.AxisListType

T = 2048
D = 2048
NT = 16
HD = 64
NEG = -30000.0
IN_COLS = 5696
C_NQ, C_KCMP, C_VCMP, C_KSLC, C_VSLC, C_KWIN, C_VWIN, C_GATE, C_FQ, C_FK, C_FV, C_FF = (
    0, 1024, 1280, 1536, 1792, 2048, 2304, 2560, 2608, 3632, 4656, 5680)
NSLOT = 64
VOFF = 2112
VLEN = 4608


class Eng:
    def __init__(self, name, e, sem, strict_self=True):
        self.name = name; self.e = e; self.sem = sem; self.cnt = 0
        self.waited = {}; self.strict_self = strict_self


class Buf:
    def __init__(self, t, name=""):
        self.t = t; self.name = name; self.w = {}; self.r = {}

    def __getitem__(self, idx):
        return self.t[idx]


class K:
    def __init__(self, nc, es, n_dma_sems=40):
        self.nc = nc; self.es = es

        def mk(name, e, strict=True):
            return Eng(name, e, es.enter_context(nc.semaphore("sem_" + name)), strict)
        self.pe = mk("pe", nc.tensor, strict=False)
        relax = os.environ.get("K_RELAX", "") .split(",")
        self.act = mk("act", nc.scalar, strict="act" not in relax)
        self.dve = mk("dve", nc.vector, strict="dve" not in relax)
        self.pool = mk("pool", nc.gpsimd, strict="pool" not in relax)
        self.sp = mk("sp", nc.sync)
        self.dsems = [[es.enter_context(nc.semaphore(f"dsem{i}")), 0] for i in range(n_dma_sems)]
        self.dnext = 0
        self.nwaits = 0; self.nops = 0; self.ndma = 0

    def sb(self, name, shape, dt, es=None):
        return Buf((es or self.es).enter_context(self.nc.sbuf_tensor(name, list(shape), dt)), name)

    def ps(self, name, shape, dt):
        return Buf(self.es.enter_context(self.nc.psum_tensor(name, list(shape), dt)), name)

    def dram(self, name, shape, dt, kind="Internal"):
        return Buf(self.nc.dram_tensor(name, list(shape), dt, kind=kind), name)

    def view(self, buf, name=""):
        return Buf(buf.t, name or buf.name)

    def _wait(self, E, need, keep_last=False):
        pend = []
        for key, (sem, val) in need.items():
            if sem is E.sem and not E.strict_self:
                continue
            if E.waited.get(key, 0) >= val:
                continue
            pend.append((key, sem, val))
        last = None
        if keep_last and pend:
            last = pend.pop()
        for key, sem, val in pend:
            E.e.wait_ge(sem, val); E.waited[key] = val; self.nwaits += 1
        if last is not None:
            E.waited[last[0]] = last[2]
        return last

    @staticmethod
    def _merge(need, d):
        for kk, (s, v) in d.items():
            if kk not in need or need[kk][1] < v:
                need[kk] = (s, v)

    def _deps(self, reads, writes):
        need = {}
        for b in reads:
            self._merge(need, b.w)
        for b in writes:
            self._merge(need, b.w); self._merge(need, b.r)
        return need

    def op(self, E, fn, reads=(), writes=()):
        last = self._wait(E, self._deps(reads, writes), keep_last=True)
        ins = fn()
        if last is not None:
            ins._wait_ge(last[1], last[2])
        E.cnt += 1; self.nops += 1
        ins.then_inc(E.sem, 1)
        tok = (E.sem, E.cnt); key = id(E.sem)
        for b in reads:
            b.r[key] = tok
        for b in writes:
            b.w = {key: tok}; b.r = {}
        return ins

    def dma(self, E, out_ap, in_ap, reads=(), writes=(), fn=None, sems=None, **kw):
        need = self._deps(reads, writes)
        if sems is not None:
            ds = sems[0][sems[1][0] % len(sems[0])]; sems[1][0] += 1
        else:
            ds = self.dsems[self.dnext]; self.dnext = (self.dnext + 1) % len(self.dsems)
        if ds[1] > 0:
            self._merge(need, {id(ds[0]): (ds[0], ds[1])})
        self._wait(E, need)
        if fn is None:
            ins = E.e.dma_start(out=out_ap, in_=in_ap, **kw)
        else:
            ins = fn()
        ds[1] += 16; self.ndma += 1
        ins.then_inc(ds[0], 16)
        tok = (ds[0], ds[1]); key = id(ds[0])
        for b in reads:
            b.r[key] = tok
        for b in writes:
            b.w = {key: tok}; b.r = {}
        return ins

    def barrier(self):
        engs = [self.pe, self.act, self.dve, self.pool, self.sp]
        for E in engs:
            need = {}
            for F in engs:
                if F is not E and F.cnt > 0:
                    need[id(F.sem)] = (F.sem, F.cnt)
            for ds in self.dsems:
                if ds[1] > 0:
                    need[id(ds[0])] = (ds[0], ds[1])
            self._wait(E, need)

    def finish(self, bufs):
        need = {}
        for b in bufs:
            self._merge(need, b.w)
        self._wait(self.sp, need)


class Scope:
    def __init__(self, k):
        self.k = k; self.es = ExitStack()

    def __enter__(self):
        self.es.__enter__()
        return self.es

    def __exit__(self, *a):
        if a[0] is None:
            self.k.barrier()
        return self.es.__exit__(*a)


def _t5_bucket(n):
    n = np.maximum(n, 0)
    rel = np.log(np.maximum(n, 1).astype(np.float32) / np.float32(16)) / np.float32(math.log(128 / 16))
    large = 16 + (rel * np.float32(16)).astype(np.int32)
    large = np.minimum(large, 31)
    return np.where(n < 16, n, large)


def _consts():
    bf = ml_dtypes.bfloat16
    c = {}
    c["ident_bf"] = np.eye(128, dtype=np.float32).astype(bf)
    c["ident_f"] = np.eye(128, dtype=np.float32)
    i = np.arange(128)[:, None]; j = np.arange(128)[None, :]
    c["caus"] = np.where(i <= j, 0.0, NEG).astype(bf)
    c["w4m"] = np.where(i > j, 0.0, NEG).astype(bf)
    c["stri"] = (i < j).astype(np.float32).astype(bf)
    up = np.zeros((128, 4, 512), np.float32)
    for jo in range(4):
        for to in range(4):
            if jo < to:
                up[:, jo, to * 128:(to + 1) * 128] = 1.0
            elif jo == to:
                up[:, jo, to * 128:(to + 1) * 128] = (i <= j)
    c["upat"] = up
    m = np.arange(VLEN) - VOFF
    oh = np.zeros((33, VLEN), np.float32)
    bk = _t5_bucket(m)
    for idx in range(VLEN):
        if m[idx] >= 0:
            oh[bk[idx], idx] = 1.0
        else:
            oh[32, idx] = 1.0
    c["ohv"] = oh
    sel31 = np.zeros((32, 32), np.float32); sel31[31, :] = 1.0
    c["sel31"] = sel31
    am = np.zeros((128, 32), np.float32)
    for jj in range(32):
        for a in range(4):
            for b in range(2):
                cc = jj * 4 + a - b
                if 0 <= cc < 127:
                    am[cc, jj] += 1.0
    c["amat"] = am.astype(bf)
    em = np.zeros((32, 2048), np.float32)
    for jj in range(32):
        em[jj, jj * 64:(jj + 1) * 64] = 1.0
    c["emat"] = em.astype(bf)
    t = np.arange(1024, 2048)
    blk = np.arange(32)[None, :]
    cur = (t // 64)[:, None]
    valid = (blk * 64 <= t[:, None])
    forced = (blk == 0) | (blk == cur) | (blk == cur - 1)
    add = np.where(valid, np.where(forced, 1e4, 0.0), -1e30).astype(np.float32)
    c["selvalid"] = valid.astype(np.float32).reshape(8, 128, 32).transpose(1, 0, 2).copy()
    c["seladd"] = add.reshape(8, 128, 32).transpose(1, 0, 2).copy()
    c["ones_d"] = np.ones((3, 8, 2048), np.float32).astype(bf)
    c["iota_p"] = np.arange(128, dtype=np.float32).reshape(128, 1)
    return c


CONST_SPECS = [("ident_bf", [128, 128], BF16), ("ident_f", [128, 128], F32), ("caus", [128, 128], BF16),
               ("w4m", [128, 128], BF16), ("stri", [128, 128], BF16), ("upat", [128, 4, 512], F32),
               ("ohv", [33, VLEN], F32), ("sel31", [32, 32], F32), ("amat", [128, 32], BF16),
               ("emat", [32, 2048], BF16), ("selvalid", [128, 8, 32], F32), ("seladd", [128, 8, 32], F32),
               ("ones_d", [3, 8, 2048], BF16), ("iota_p", [128, 1], F32)]

PARAM_SPECS = [("attn_norm_w", [1, 2048]), ("w_in", [1, 2048, IN_COLS]), ("cmp_pe_k", [1, 32, 64]),
               ("cmp_pe_v", [1, 32, 64]), ("cmp_k_w1", [1, 2048, 128]), ("cmp_k_w2", [1, 128, 64]),
               ("cmp_v_w1", [1, 2048, 128]), ("cmp_v_w2", [1, 128, 64]), ("rel_bias_table", [32, 16]),
               ("fox_forget_b", [1, 16]), ("nsa_out_norm_w", [1, 1024]), ("fox_out_norm_w", [1, 1024]),
               ("w_out", [1, 2048, 2048]), ("ffn_norm_w", [1, 2048]), ("router_group_w", [1, 2048, 4]),
               ("router_group_b", [1, 4]), ("router_expert_w", [1, 2048, 32]), ("router_expert_b", [1, 32]),
               ("expert_w_gate", [32, 2048, 512]), ("expert_w_up", [32, 2048, 512]),
               ("expert_w_down", [32, 512, 2048]), ("final_norm_w", [2048])]


def build_nc(stop_after=None, debug=False):
    nc = bass.Bass("TRN2", target_bir_lowering=False)
    P = {}
    x_d = Buf(nc.dram_tensor("x", [T, D], F32, kind="ExternalInput"), "x")
    for name, shape in PARAM_SPECS:
        P[name] = Buf(nc.dram_tensor(name, shape, F32, kind="ExternalInput"), name)
    CD = {}
    for name, shape, dt in CONST_SPECS:
        CD[name] = Buf(nc.dram_tensor("c_" + name, shape, dt, kind="ExternalInput"), name)
    out_d = Buf(nc.dram_tensor("out", [T, D], F32, kind="ExternalOutput"), "out")
    dbg = {}
    V, S, G, TE = nc.vector, nc.scalar, nc.gpsimd, nc.tensor

    with ExitStack() as es:
        k = K(nc, es)
        pe, act, dve, pool, sp = k.pe, k.act, k.dve, k.pool, k.sp

        o_scr = k.dram("o_scr", [T, 2048], BF16)
        vscr = k.dram("vscr", [16, VLEN], BF16)
        brd = k.dram("brd", [16 * 128, VLEN], BF16)
        cscr = k.dram("cscr", [16, 6, T], BF16)
        h1_scr = k.dram("h1_scr", [T, D], F32)
        hn_scr = k.dram("hn_scr", [T, D], BF16)
        xslot = k.dram("xslot", [NSLOT * 128, D], BF16)
        yslot = k.dram("yslot", [NSLOT * 128, D], F32)
        if debug:
            for nm, shp, dt in [("d_o", [T, 2048], BF16), ("d_h1", [T, D], F32)]:
                dbg[nm] = Buf(nc.dram_tensor(nm, shp, dt, kind="ExternalOutput"), nm)

        wbf = {"g": k.dram("wbf_g", [32, 2048, 512], BF16), "u": k.dram("wbf_u", [32, 2048, 512], BF16),
               "d": k.dram("wbf_d", [32, 512, 2048], BF16)}
        pre_sems = ([[es.enter_context(nc.semaphore(f"presem{i}")), 0] for i in range(6)], [0])
        pre_list = []
        pre_views = []
        for e_ in range(32):
            for key_, pn_ in [("g", "expert_w_gate"), ("u", "expert_w_up"), ("d", "expert_w_down")]:
                vw = k.view(wbf[key_], f"wbf_{key_}{e_}")
                pre_views.append(vw)
                pre_list.append((vw, wbf[key_].t[e_], P[pn_], P[pn_].t[e_]))
        pre_pos = [0]

        def precast_step(n):
            for _ in range(n):
                if pre_pos[0] >= len(pre_list):
                    return
                vw, dst_ap, sb_, src_ap = pre_list[pre_pos[0]]; pre_pos[0] += 1
                k.dma(pool, dst_ap, src_ap, reads=[sb_], writes=[vw], sems=pre_sems)

        o_views = []

        def o_store(dst_ap, src_b, src_ap):
            v_ = k.view(o_scr, "o_st")
            k.dma(sp, dst_ap, src_ap, reads=[src_b], writes=[v_])
            o_views.append(v_)

        def done(bufs):
            k.finish(bufs)
            print("ops", k.nops, "waits", k.nwaits, "dmas", k.ndma, flush=True)
            return nc

        PB = [k.ps(f"pb{i}", [128, 512], F32) for i in range(7)]
        PT = k.ps("pt_bf", [128, 1024], BF16)

        ident_bf = k.sb("ident_bf", [128, 128], BF16)
        ident_f = k.sb("ident_f", [128, 128], F32)
        caus = k.sb("caus", [128, 128], BF16)
        for b_, nm in [(ident_bf, "ident_bf"), (ident_f, "ident_f"), (caus, "caus")]:
            k.dma(sp, b_[:], CD[nm][:], reads=[CD[nm]], writes=[b_])
        rstd_tmp = k.sb("rstd_tmp", [128, 4], F32)
        ssn = k.sb("ssn", [128, NT], F32)
        ssf = k.sb("ssf", [128, NT], F32)
        sstmp = k.sb("sstmp", [128, 2], F32)
        eps_t = k.sb("eps_t", [128, 1], F32)
        junk = k.sb("junk", [128, 2048], BF16)
        k.op(dve, lambda: V.memset(ssn[:], 0.0), writes=[ssn])
        k.op(dve, lambda: V.memset(ssf[:], 0.0), writes=[ssf])
        k.op(dve, lambda: V.memset(eps_t[:], 1e-6), writes=[eps_t])

        def evac(E, out_ap, in_ap, reads, writes, scale=None):
            if E is act:
                if scale is None:
                    return k.op(act, lambda: S.copy(out=out_ap, in_=in_ap), reads=reads, writes=writes)
                return k.op(act, lambda: S.activation(out=out_ap, in_=in_ap, func=AF.Copy, scale=scale), reads=reads, writes=writes)
            if scale is None:
                return k.op(dve, lambda: V.tensor_copy(out=out_ap, in_=in_ap), reads=reads, writes=writes)
            return k.op(dve, lambda: V.tensor_scalar(out=out_ap, in0=in_ap, scalar1=scale, scalar2=None, op0=ALU.mult),
                        reads=reads, writes=writes)

        def mm(out_b, out_ap, l_b, l_ap, r_b, r_ap, start, stop, extra_reads=()):
            return k.op(pe, lambda: TE.matmul(out_ap, l_ap, r_ap, start=start, stop=stop),
                        reads=[l_b, r_b] + list(extra_reads), writes=[out_b])

        def transpose(out_b, out_ap, in_b, in_ap, id_b, kp=128):
            return k.op(pe, lambda: TE.transpose(out_ap, in_ap, id_b[0:kp, 0:kp]), reads=[in_b, id_b], writes=[out_b])

        def rstd_from_ss(ss_ap, ss_b, out_ap, out_b, n):
            k.op(act, lambda: S.activation(out=rstd_tmp[:, 0:1], in_=ss_ap, func=AF.Ln, scale=1.0 / n, bias=eps_t[:, 0:1]),
                 reads=[ss_b, eps_t], writes=[rstd_tmp])
            k.op(act, lambda: S.activation(out=out_ap, in_=rstd_tmp[:, 0:1], func=AF.Exp, scale=-0.5),
                 reads=[rstd_tmp], writes=[out_b])

        def tt(out_b, out_ap, a_b, a_ap, b_b, b_ap, op, E=None):
            return k.op(E or dve, lambda: (E or dve).e.tensor_tensor(out=out_ap, in0=a_ap, in1=b_ap, op=op), reads=[a_b, b_b], writes=[out_b])

        def ts(out_b, out_ap, a_b, a_ap, s1, op0, s2=None, op1=None, sreads=()):
            if op1 is None:
                return k.op(dve, lambda: V.tensor_scalar(out=out_ap, in0=a_ap, scalar1=s1, scalar2=None, op0=op0),
                            reads=[a_b] + list(sreads), writes=[out_b])
            return k.op(dve, lambda: V.tensor_scalar(out=out_ap, in0=a_ap, scalar1=s1, scalar2=s2, op0=op0, op1=op1),
                        reads=[a_b] + list(sreads), writes=[out_b])

        def stt(out_b, out_ap, a_b, a_ap, sc, b_b, b_ap, op0, op1, sreads=()):
            return k.op(dve, lambda: V.scalar_tensor_tensor(out=out_ap, in0=a_ap, scalar=sc, in1=b_ap, op0=op0, op1=op1),
                        reads=[a_b, b_b] + list(sreads), writes=[out_b])

        with Scope(k) as esA:
            xnT = k.sb("xnT", [128, 16, T], BF16, es=esA)
            with Scope(k) as e1:
                anw_bc = k.sb("anw_bc", [128, D], F32, es=e1)
                k.dma(sp, anw_bc[:], bass.AP(P["attn_norm_w"].t, 0, [[0, 128], [1, D]]), reads=[P["attn_norm_w"]], writes=[anw_bc])
                xb = [k.sb(f"xb{i}", [128, D], F32, es=e1) for i in range(2)]
                xn_tm = [k.sb(f"xn_tm{i}", [128, D], BF16, es=e1) for i in range(2)]
                ssx = k.sb("ssx", [128, NT], F32, es=e1)
                rsx = k.sb("rsx", [128, NT], F32, es=e1)
                for i in range(NT):
                    xs = xb[i % 2]; xt = xn_tm[i % 2]
                    k.dma(sp, xs[:], x_d[i * 128:(i + 1) * 128, :], reads=[x_d], writes=[xs])
                    k.op(act, lambda: S.activation(out=junk[:], in_=xs[:], func=AF.Square, accum_out=ssx[:, i:i + 1]),
                         reads=[xs], writes=[junk, ssx])
                    rstd_from_ss(ssx[:, i:i + 1], ssx, rsx[:, i:i + 1], rsx, D)
                    stt(xt, xt[:], xs, xs[:], rsx[:, i:i + 1], anw_bc, anw_bc[:], ALU.mult, ALU.mult, sreads=[rsx])
                    for c4 in range(4):
                        for cc in range(4):
                            c = c4 * 4 + cc
                            transpose(PT, PT[:, cc * 128:(cc + 1) * 128], xt, xt[:, c * 128:(c + 1) * 128], ident_bf)
                        evac(act if c4 % 2 else dve, xnT[:, c4 * 4:(c4 + 1) * 4, i * 128:(i + 1) * 128],
                             PT[:, 0:512].rearrange("p (c t) -> p c t", c=4), [PT], [xnT])

            if stop_after == "xn":
                k.dma(sp, dbg["d_o"].t[:].rearrange("(p c) t -> p c t", c=16), xnT[:], reads=[xnT], writes=[dbg["d_o"]])
                return done([dbg["d_o"]])
            with Scope(k) as e2:
                tab = k.sb("tab", [33, 16], F32, es=e2)
                sel31 = k.sb("sel31", [32, 32], F32, es=e2)
                ohv = k.sb("ohv", [33, VLEN], F32, es=e2)
                vsb = k.sb("vsb", [16, VLEN], BF16, es=e2)
                k.dma(sp, tab[0:32, :], P["rel_bias_table"][:], reads=[P["rel_bias_table"]], writes=[tab])
                k.dma(sp, sel31[:], CD["sel31"][:], reads=[CD["sel31"]], writes=[sel31])
                k.dma(sp, ohv[:], CD["ohv"][:], reads=[CD["ohv"]], writes=[ohv])
                mm(PB[0], PB[0][0:32, 0:16], sel31, sel31[:], tab, tab[0:32, :], True, True)
                tt(tab, tab[0:32, :], tab, tab[0:32, :], PB[0], PB[0][0:32, 0:16], ALU.subtract)
                k.op(dve, lambda: V.memset(tab[32:33, :], NEG), writes=[tab])
                for q in range(VLEN // 512):
                    pb = PB[q % 2]
                    mm(pb, pb[0:16, :], tab, tab[:], ohv, ohv[:, q * 512:(q + 1) * 512], True, True)
                    evac(dve, vsb[:, q * 512:(q + 1) * 512], pb[0:16, :], [pb], [vsb])
                k.dma(sp, vscr[:], vsb[:], reads=[vsb], writes=[vscr])
                for h in range(16):
                    k.dma(sp, brd[h * 128:(h + 1) * 128, :], bass.AP(vscr.t, h * VLEN, [[0, 128], [1, VLEN]]), reads=[vscr], writes=[brd])

            wst_box = [None]

            def cast(E, out_b, out_ap, in_b, in_ap):
                return k.op(E, lambda: E.e.tensor_copy(out=out_ap, in_=in_ap), reads=[in_b], writes=[out_b])

            def load_w(buf, c0, col0, ncols):
                wst = wst_box[0]
                stv = wst[:, 0:16 * ncols].rearrange("p (c n) -> p c n", c=16)
                src = P["w_in"].t[0, :, col0:col0 + ncols].rearrange("(c p) n -> p c n", p=128)
                k.dma(sp, stv, src, reads=[P["w_in"]], writes=[wst])
                cast(pool, buf, buf[:, :, c0:c0 + ncols], wst, stv)

            rot = [0]

            def proj_fm(wbuf, ncols, dst_b, dst_fn, scale):
                for tc in range(4):
                    pb = PB[rot[0] % 3]; rot[0] += 1
                    for c in range(16):
                        mm(pb, pb[0:ncols, :], wbuf, wbuf[:, c, 0:ncols], xnT, xnT[:, c, tc * 512:(tc + 1) * 512], c == 0, c == 15)
                    evac(act if rot[0] % 2 else dve, dst_fn(tc), pb[0:ncols, :], [pb], [dst_b], scale)

            with Scope(k) as eC:
                wst_box[0] = k.sb("wstC", [128, 16 * 16], F32, es=eC)
                wf = k.sb("wf", [128, 16, 16], BF16, es=eC)
                fb_bc = k.sb("fb_bc", [128, 16], F32, es=eC)
                logf = k.sb("logf", [128, NT, 16], F32, es=eC)
                upat = k.sb("upat", [128, 4, 512], F32, es=eC)
                onesf = k.sb("onesf", [128, 512], F32, es=eC)
                cT = k.sb("cT", [16, T], F32, es=eC)
                r1 = k.sb("r1", [16, T], F32, es=eC)
                parts = k.sb("parts", [16, 6, T], BF16, es=eC)
                ztmp = k.sb("ztmp", [128, 16], F32, es=eC)
                load_w(wf, 0, C_FF, 16)
                k.dma(sp, fb_bc[:], bass.AP(P["fox_forget_b"].t, 0, [[0, 128], [1, 16]]), reads=[P["fox_forget_b"]], writes=[fb_bc])
                k.dma(sp, upat[:], CD["upat"][:], reads=[CD["upat"]], writes=[upat])
                k.op(dve, lambda: V.memset(onesf[:], 1.0), writes=[onesf])
                if stop_after == "wf":
                    k.dma(sp, dbg["d_o"].t[0:128, 0:256], wf[:].rearrange("p a b -> p (a b)"), reads=[wf], writes=[dbg["d_o"]])
                    k.dma(pool, dbg["d_o"].t[128:256, 0:16], fb_bc[:], reads=[fb_bc], writes=[dbg["d_o"]])
                    return done([dbg["d_o"]])
                for i in range(NT):
                    pb = PB[i % 2]
                    for c in range(16):
                        mm(pb, pb[:, 0:16], xnT, xnT[:, c, i * 128:(i + 1) * 128], wf, wf[:, c, :], c == 0, c == 15)
                    tt(ztmp, ztmp[:], pb, pb[:, 0:16], fb_bc, fb_bc[:], ALU.add)
                    k.op(act, lambda: S.activation(out=ztmp[:], in_=ztmp[:], func=AF.Exp, scale=-1.0), reads=[ztmp], writes=[ztmp])
                    k.op(act, lambda: S.activation(out=ztmp[:], in_=ztmp[:], func=AF.Ln, scale=1.0, bias=1.0), reads=[ztmp], writes=[ztmp])
                    ts(logf, logf[:, i, :], ztmp, ztmp[:], -1.0, ALU.mult)
                for q in range(4):
                    pb = PB[q % 2]
                    n = 4 * q + 4
                    for j in range(n):
                        jo = j - 4 * q
                        if jo >= 0:
                            mm(pb, pb[0:16, :], logf, logf[:, j, :], upat, upat[:, jo, :], j == 0, j == n - 1)
                        else:
                            mm(pb, pb[0:16, :], logf, logf[:, j, :], onesf, onesf[:], j == 0, False)
                    evac(dve, cT[:, q * 512:(q + 1) * 512], pb[0:16, :], [pb], [cT])
                if stop_after == "lf":
                    k.dma(sp, dbg["d_h1"].t[0:128, 0:256], logf[:].rearrange("p a b -> p (a b)"), reads=[logf], writes=[dbg["d_h1"]])
                    k.dma(sp, dbg["d_h1"].t[128:144, :], cT[:], reads=[cT], writes=[dbg["d_h1"]])
                    return done([dbg["d_h1"]])
                k.op(dve, lambda: V.tensor_copy(out=parts[:, 0, :], in_=cT[:]), reads=[cT], writes=[parts])
                tt(r1, r1[:], cT, cT[:], parts, parts[:, 0, :], ALU.subtract)
                k.op(dve, lambda: V.tensor_copy(out=parts[:, 1, :], in_=r1[:]), reads=[r1], writes=[parts])
                tt(r1, r1[:], r1, r1[:], parts, parts[:, 1, :], ALU.subtract)
                k.op(dve, lambda: V.tensor_copy(out=parts[:, 2, :], in_=r1[:]), reads=[r1], writes=[parts])
                ts(parts, parts[:, 3:6, :], parts, parts[:, 0:3, :], -1.0, ALU.mult)
                k.dma(sp, cscr[:], parts[:], reads=[parts], writes=[cscr])

            if stop_after == "cs":
                k.dma(sp, dbg["d_o"].t[0:96, :].rearrange("(h k) t -> h k t", k=6), cscr[:], reads=[cscr], writes=[dbg["d_o"]])
                return done([dbg["d_o"]])
            with Scope(k) as eF:
                NH = 4
                wst_box[0] = k.sb("wstF", [128, 16 * 256], F32, es=eF)
                FQ = k.sb("FQ", [128, NH, T], BF16, es=eF)
                FK = k.sb("FK", [128, NH, T], BF16, es=eF)
                k.op(pool, lambda: G.memset(FQ[:], 0.0), writes=[FQ])
                k.op(pool, lambda: G.memset(FK[:], 0.0), writes=[FK])
                FV = k.sb("FV", [128, NT, NH, 65], BF16, es=eF)
                OJ = k.sb("OJ", [128, NT, NH * 64], BF16, es=eF)
                wq = [k.sb(f"wq{i}", [128, 16, 128], BF16, es=eF) for i in range(2)]
                wv = k.sb("wv", [128, 16, NH * 64], BF16, es=eF)
                pt_sb = [k.sb(f"pt_sb{i}", [128, 512], BF16, es=eF) for i in range(3)]
                rz = k.sb("rz", [128, 8], F32, es=eF)
                of32s = [k.sb(f"of32_{i}", [128, 64], F32, es=eF) for i in range(2)]
                FQh = [k.view(FQ, f"FQ{h}") for h in range(NH)]
                FKh = [k.view(FK, f"FK{h}") for h in range(NH)]
                FQc = k.view(FQ, "FQc"); FKc = k.view(FK, "FKc")
                for v_ in FQh + [FQc]:
                    v_.w = dict(FQ.w)
                for v_ in FKh + [FKc]:
                    v_.w = dict(FK.w)
                k.op(dve, lambda: V.memset(FV[:, :, :, 64:65], 1.0), writes=[FV])
                POb = PB[3:7]
                for fp in range(16 // NH):
                    h0 = fp * NH
                    for pj in range(NH // 2):
                        h = h0 + 2 * pj
                        for wb, col, DST, DSTh, sc in [(wq[0], C_FQ, FQ, FQh, 0.125), (wq[1], C_FK, FK, FKh, None)]:
                            load_w(wb, 0, col + h * 64, 128)
                            for tc in range(4):
                                pb = PB[rot[0] % 3]; rot[0] += 1
                                for c in range(16):
                                    mm(pb, pb[:, :], wb, wb[:, c, :], xnT, xnT[:, c, tc * 512:(tc + 1) * 512], c == 0, c == 15)
                                evac(act, DST[0:64, 2 * pj, tc * 512:(tc + 1) * 512], pb[0:64, :], [pb], [DSTh[2 * pj]], sc)
                                evac(dve, DST[64:128, 2 * pj + 1, tc * 512:(tc + 1) * 512], pb[64:128, :], [pb], [DSTh[2 * pj + 1]], sc)
                    load_w(wv, 0, C_FV + h0 * 64, NH * 64)
                    for i in range(NT):
                        pb = PB[rot[0] % 3]; rot[0] += 1
                        for c in range(16):
                            mm(pb, pb[:, 0:NH * 64], xnT, xnT[:, c, i * 128:(i + 1) * 128], wv, wv[:, c, :], c == 0, c == 15)
                        evac(act if i % 2 else dve, FV[:, i, :, 0:64], pb[:, 0:NH * 64].rearrange("p (h d) -> p h d", h=NH), [pb], [FV])
                    for par, r0 in [(0, 64), (1, 0)]:
                        hs = slice(h0 + par, h0 + NH, 2); ls = slice(par, NH, 2); nh2 = NH // 2
                        k.dma(sp, FQ[r0:r0 + 3, ls, :], cscr.t[hs, 0:3, :].rearrange("h k t -> k h t"), reads=[cscr], writes=[FQc])
                        k.dma(sp, FQ[r0 + 3:r0 + 6, ls, :], CD["ones_d"].t[:, 0:nh2, :], reads=[CD["ones_d"]], writes=[FQc])
                        k.dma(sp, FK[r0:r0 + 3, ls, :], CD["ones_d"].t[:, 0:nh2, :], reads=[CD["ones_d"]], writes=[FKc])
                        k.dma(sp, FK[r0 + 3:r0 + 6, ls, :], cscr.t[hs, 3:6, :].rearrange("h k t -> k h t"), reads=[cscr], writes=[FKc])
                    recs = [(hl, qc, kb) for hl in range(NH) for qc in range(4) for kb in range(4 * qc + 4)]

                    def geom(rec):
                        hl, qc, kb = rec
                        dg = kb - 4 * qc
                        q0 = qc * 512 + (dg * 128 if dg > 0 else 0)
                        return hl, qc, kb, dg, q0, (qc + 1) * 512 - q0

                    def fox_qk(rec, slot):
                        hl, qc, kb, dg, q0, n = geom(rec)
                        pb = PB[slot % 3]
                        mm(pb, pb[:, 0:n], FKh[hl], FK[:, hl, kb * 128:(kb + 1) * 128], FQh[hl], FQ[:, hl, q0:q0 + n],
                           True, dg < 0, extra_reads=[FQc, FKc])
                        if dg >= 0:
                            mm(pb, pb[:, 0:128], ident_bf, ident_bf[:], caus, caus[:], False, True)

                    def fox_pv(rec, slot):
                        hl, qc, kb, dg, q0, n = geom(rec)
                        pb = PB[slot % 3]; ptb = pt_sb[slot % 3]
                        k.op(act, lambda: S.activation(out=ptb[:, 0:n], in_=pb[:, 0:n], func=AF.Exp), reads=[pb], writes=[ptb])
                        j0 = max(dg, 0)
                        for jq in range(j0, 4):
                            po = POb[jq]
                            cs = (jq - j0) * 128
                            mm(po, po[:, 0:65], ptb, ptb[:, cs:cs + 128], FV, FV[:, kb, hl, :], kb == 0, kb == 4 * qc + jq)
                        if kb == 4 * qc + 3:
                            for jq in range(4):
                                po = POb[jq]; i = qc * 4 + jq
                                k.op(dve, lambda: V.reciprocal(out=rz[:, jq:jq + 1], in_=po[:, 64:65]), reads=[po], writes=[rz])
                                of = of32s[jq % 2]
                                ts(of, of[:], po, po[:, 0:64], rz[:, jq:jq + 1], ALU.mult, sreads=[rz])
                                k.op(act, lambda: S.activation(out=junk[:, 0:64], in_=of[:], func=AF.Square, accum_out=sstmp[:, 0:1]),
                                     reads=[of], writes=[junk, sstmp])
                                tt(ssf, ssf[:, i:i + 1], ssf, ssf[:, i:i + 1], sstmp, sstmp[:, 0:1], ALU.add)
                                k.op(dve, lambda: V.tensor_copy(out=OJ[:, i, hl * 64:(hl + 1) * 64], in_=of[:]), reads=[of], writes=[OJ])
                            if qc % 2 == 1:
                                precast_step(1)

                    fox_qk(recs[0], 0)
                    for t_ in range(len(recs)):
                        if t_ + 1 < len(recs):
                            fox_qk(recs[t_ + 1], t_ + 1)
                        fox_pv(recs[t_], t_)
                    for i in range(NT):
                        c0 = 1024 + h0 * 64
                        o_store(o_scr[i * 128:(i + 1) * 128, c0:c0 + NH * 64], OJ, OJ[:, i, :])

            if stop_after == "fox":
                k.dma(sp, dbg["d_o"][:], o_scr[:], reads=o_views, writes=[dbg["d_o"]])
                return done([dbg["d_o"]])

            with Scope(k) as eN:
                wst_box[0] = k.sb("wstN", [128, 16 * 144], F32, es=eN)
                QT = k.sb("QT", [128, 4, T], BF16, es=eN)
                KTS = k.sb("KTS", [128, T], BF16, es=eN)
                KTW = k.sb("KTW", [128, T], BF16, es=eN)
                k.op(dve, lambda: V.memset(QT[:], 0.0), writes=[QT])
                for b_ in (KTS, KTW):
                    k.op(pool, lambda: G.memset(b_[:], 0.0), writes=[b_])
                CK = k.sb("CK", [64, T], BF16, es=eN)
                CV = k.sb("CV", [64, T], BF16, es=eN)
                VS = k.sb("VS", [128, NT, 65], BF16, es=eN)
                VW = k.sb("VW", [128, NT, 65], BF16, es=eN)
                GT = k.sb("GT", [128, NT, 12], F32, es=eN)
                W1K = k.sb("W1K", [64, 32, 128], BF16, es=eN)
                W1V = k.sb("W1V", [64, 32, 128], BF16, es=eN)
                W2K = k.sb("W2K", [128, 64], BF16, es=eN)
                W2V = k.sb("W2V", [128, 64], BF16, es=eN)
                pe_ld = k.sb("pe_ld", [32, 128], F32, es=eN)
                PET = k.sb("PET", [64, 2, 32], BF16, es=eN)
                b1 = k.sb("b1", [128, 2], F32, es=eN)
                HK = k.sb("HK", [128, 128], BF16, es=eN)
                HV = k.sb("HV", [128, 128], BF16, es=eN)
                KC = k.sb("KC", [128, 128], BF16, es=eN)
                VCX = k.sb("VCX", [128, 97], BF16, es=eN)
                B0 = k.sb("B0", [128, 4, 128], BF16, es=eN)
                B1 = k.sb("B1", [128, 4, 128], BF16, es=eN)
                W4X = k.sb("W4X", [128, 4, 128], BF16, es=eN)
                CB = [k.sb(f"CB{i}", [128, 4, 128], BF16, es=eN) for i in range(2)]
                NM4 = [k.sb(f"NM4{i}", [128, 4, 128], BF16, es=eN) for i in range(2)]
                emat = k.sb("emat", [128, T], BF16, es=eN)
                for b_ in NM4 + [emat]:
                    k.op(pool, lambda: G.memset(b_[:], 0.0), writes=[b_])
                selvalid = k.sb("selvalid", [128, 8, 32], F32, es=eN)
                seladd = k.sb("seladd", [128, 8, 32], F32, es=eN)
                OJn = k.sb("OJn", [128, NT, 256], BF16, es=eN)
                OA = k.sb("OA", [128, 4, 64], F32, es=eN)
                wqn = [k.sb(f"wqn{i}", [128, 16, 64], BF16, es=eN) for i in range(2)]
                wtm = k.sb("wtm", [128, 16, 144], BF16, es=eN)
                ptn = [k.sb(f"ptn{i}", [128, 512], BF16, es=eN) for i in range(3)]
                sm = k.sb("sm", [128, 64], F32, es=eN)
                imp = k.sb("imp", [128, 32], F32, es=eN)
                score = k.sb("score", [128, 32], F32, es=eN)
                score2 = k.sb("score2", [128, 32], F32, es=eN)
                mx = k.sb("mx", [128, 16], F32, es=eN)
                negmb = k.sb("negmb", [128, 32], BF16, es=eN)
                gtmp = k.sb("gtmp", [128, 12], F32, es=eN)
                k.dma(sp, emat[0:32, :], CD["emat"][:], reads=[CD["emat"]], writes=[emat])
                for b_, nm in [(selvalid, "selvalid"), (seladd, "seladd")]:
                    k.dma(sp, b_[:], CD[nm][:], reads=[CD[nm]], writes=[b_])
                k.dma(sp, W4X[:, 0, :], CD["w4m"][:], reads=[CD["w4m"]], writes=[W4X])
                for r in range(1, 4):
                    k.op(dve, lambda: V.tensor_copy(out=W4X[:, r, :], in_=W4X[:, 0, :]), reads=[W4X], writes=[W4X])
                wstn = wst_box[0]
                for (Wd, pn) in [(W1K, "cmp_k_w1"), (W1V, "cmp_v_w1")]:
                    for hf in range(2):
                        stv = wstn[0:64, 0:2048].rearrange("p (l h) -> p l h", l=16)
                        k.dma(sp, stv, P[pn].t[0, hf * 1024:(hf + 1) * 1024, :].rearrange("(l d) h -> d l h", d=64), reads=[P[pn]], writes=[wstn])
                        if hf == 0:
                            k.op(dve, lambda: V.tensor_copy(out=Wd[:, hf * 16:(hf + 1) * 16, :], in_=stv), reads=[wstn], writes=[Wd])
                        else:
                            k.op(act, lambda: S.copy(out=Wd[:, hf * 16:(hf + 1) * 16, :], in_=stv), reads=[wstn], writes=[Wd])
                for (Wd, pn) in [(W2K, "cmp_k_w2"), (W2V, "cmp_v_w2")]:
                    k.dma(sp, wstn[:, 0:64], P[pn].t[0], reads=[P[pn]], writes=[wstn])
                    cast(pool, Wd, Wd[:], wstn, wstn[:, 0:64])
                k.dma(sp, pe_ld[:, 0:64], P["cmp_pe_k"].t[0], reads=[P["cmp_pe_k"]], writes=[pe_ld])
                k.dma(sp, pe_ld[:, 64:128], P["cmp_pe_v"].t[0], reads=[P["cmp_pe_v"]], writes=[pe_ld])
                for kv in range(2):
                    transpose(PB[0], PB[0][0:64, 0:32], pe_ld, pe_ld[:, kv * 64:(kv + 1) * 64], ident_f, kp=32)
                    evac(dve, PET[:, kv, :], PB[0][0:64, 0:32], [PB[0]], [PET])
                    W1 = W1K if kv == 0 else W1V
                    for l in range(32):
                        mm(PB[1], PB[1][:, 0:1], W1, W1[:, l, :], PET, PET[:, kv, l:l + 1], l == 0, l == 31)
                    evac(dve, b1[:, kv:kv + 1], PB[1][:, 0:1], [PB[1]], [b1])
                k.op(dve, lambda: V.memset(VS[:, :, 64:65], 1.0), writes=[VS])
                k.op(dve, lambda: V.memset(VW[:, :, 64:65], 1.0), writes=[VW])
                k.op(dve, lambda: V.memset(KC[:], 0.0), writes=[KC])
                k.op(dve, lambda: V.memset(VCX[:], 0.0), writes=[VCX])
                k.op(dve, lambda: V.memset(VCX[:, 64:65], 1.0), writes=[VCX])
                k.dma(sp, VCX[:, 65:97], CD["amat"][:], reads=[CD["amat"]], writes=[VCX])
                POb = PB[3:7]

                if stop_after == "nsa0":
                    k.dma(sp, dbg["d_o"][:], o_scr[:], reads=o_views, writes=[dbg["d_o"]])
                    return done([dbg["d_o"]])
                for g in range(4):
                    for r in range(4):
                        wb = wqn[r % 2]
                        load_w(wb, 0, C_NQ + (4 * g + r) * 64, 64)
                        proj_fm(wb, 64, QT, lambda tc: QT[0:64, r, tc * 512:(tc + 1) * 512], 0.125)
                    for ii, (col, dst) in enumerate([(C_KSLC, KTS), (C_KWIN, KTW), (C_KCMP, CK), (C_VCMP, CV)]):
                        wb = wqn[ii % 2]
                        load_w(wb, 0, col + g * 64, 64)
                        proj_fm(wb, 64, dst, lambda tc: dst[0:64, tc * 512:(tc + 1) * 512], None)
                    if os.environ.get("NSA_SKIP") == "qk":
                        k.dma(sp, dbg["d_o"][:], o_scr[:], reads=o_views, writes=[dbg["d_o"]])
                        return done([dbg["d_o"]])
                    wstn_ = wst_box[0]
                    stv_all = wstn_[:, 0:16 * 144].rearrange("p (c n) -> p c n", c=16)
                    for (c0_, col_, n_) in [(0, C_VSLC + g * 64, 64), (64, C_VWIN + g * 64, 64), (128, C_GATE + g * 12, 16)]:
                        k.dma(sp, stv_all[:, :, c0_:c0_ + n_], P["w_in"].t[0, :, col_:col_ + n_].rearrange("(c p) n -> p c n", p=128),
                              reads=[P["w_in"]], writes=[wstn_])
                    cast(pool, wtm, wtm[:], wstn_, stv_all)
                    for i in range(NT):
                        pb = PB[rot[0] % 3]; rot[0] += 1
                        for c in range(16):
                            mm(pb, pb[:, 0:144], xnT, xnT[:, c, i * 128:(i + 1) * 128], wtm, wtm[:, c, :], c == 0, c == 15)
                        evac(dve, VS[:, i, 0:64], pb[:, 0:64], [pb], [VS])
                        evac(dve, VW[:, i, 0:64], pb[:, 64:128], [pb], [VW])
                        evac(dve, gtmp[:], pb[:, 128:140], [pb], [gtmp])
                        k.op(act, lambda: S.activation(out=gtmp[:], in_=gtmp[:], func=AF.Exp, scale=-1.0), reads=[gtmp], writes=[gtmp])
                        ts(gtmp, gtmp[:], gtmp, gtmp[:], 1.0, ALU.add)
                        k.op(dve, lambda: V.reciprocal(out=GT[:, i, :], in_=gtmp[:]), reads=[gtmp], writes=[GT])
                    if os.environ.get("NSA_SKIP") == "proj":
                        k.dma(sp, dbg["d_o"][:], o_scr[:], reads=o_views, writes=[dbg["d_o"]])
                        return done([dbg["d_o"]])
                    for kv, (SRC, W1, H) in enumerate([(CK, W1K, HK), (CV, W1V, HV)]):
                        pb = PB[rot[0] % 3]; rot[0] += 1
                        for l in range(32):
                            mm(pb, pb[:, 0:127], W1, W1[:, l, :], SRC, SRC[:, l:l + 2017:16], l == 0, l == 31)
                        k.op(act, lambda: S.activation(out=H[:, 0:127], in_=pb[:, 0:127], func=AF.Silu, bias=b1[:, kv:kv + 1]),
                             reads=[pb, b1], writes=[H])
                    pb = PB[rot[0] % 3]; rot[0] += 1
                    mm(pb, pb[0:64, 0:127], W2K, W2K[:], HK, HK[:, 0:127], True, True)
                    evac(dve, KC[0:64, 0:127], pb[0:64, 0:127], [pb], [KC])
                    pb = PB[rot[0] % 3]; rot[0] += 1
                    mm(pb, pb[0:127, 0:64], HV, HV[:, 0:127], W2V, W2V[:], True, True)
                    evac(dve, VCX[0:127, 0:64], pb[0:127, 0:64], [pb], [VCX])
                    base = (4 * g) * 128 * VLEN + VOFF
                    if os.environ.get("NSA_SKIP") == "cmp":
                        k.dma(sp, dbg["d_o"][:], o_scr[:], reads=o_views, writes=[dbg["d_o"]])
                        return done([dbg["d_o"]])
                    k.dma(sp, B0[:], bass.AP(brd.t, base, [[VLEN - 1, 128], [128 * VLEN, 4], [1, 128]]), reads=[brd], writes=[B0])
                    k.dma(sp, B1[:], bass.AP(brd.t, base + 128, [[VLEN - 1, 128], [128 * VLEN, 4], [1, 128]]), reads=[brd], writes=[B1])

                    if stop_after == "nsa1":
                        k.dma(sp, dbg["d_o"][:], o_scr[:], reads=o_views, writes=[dbg["d_o"]])
                        return done([dbg["d_o"]])

                    def cb_load(qb):
                        cb = CB[qb % 2]
                        k.dma(sp, cb[:], bass.AP(brd.t, base + 128 * qb - 31, [[VLEN - 16, 128], [128 * VLEN, 4], [1, 128]]), reads=[brd], writes=[cb])

                    def n_qk(rec, slot):
                        kind, qb, kb = rec
                        pb = PB[slot % 3]
                        qap = QT[:, :, qb * 128:(qb + 1) * 128]
                        if kind == "cmp":
                            if qb + 1 < NT:
                                cb_load(qb + 1)
                            cb = CB[qb % 2]
                            mm(pb, pb[:, :], KC, KC[:], QT, qap, True, False)
                            mm(pb, pb[:, :], ident_bf, ident_bf[:], cb, cb[:], False, True)
                            return
                        sel = kind == "slc"
                        KT = KTS if sel else KTW
                        extras = []
                        if kb == qb:
                            extras.append((ident_bf, ident_bf[:], B0, B0[:]))
                        elif kb == qb - 1:
                            extras.append((ident_bf, ident_bf[:], B1, B1[:]))
                        if (not sel) and kb == qb - 4:
                            extras.append((ident_bf, ident_bf[:], W4X, W4X[:]))
                        if sel and qb >= 8:
                            nm4 = NM4[qb % 2]
                            extras.append((emat, emat[:, kb * 128:(kb + 1) * 128], nm4, nm4[:]))
                        mm(pb, pb[:, :], KT, KT[:, kb * 128:(kb + 1) * 128], QT, qap, True, len(extras) == 0)
                        for ei, (lb, la, rb, ra) in enumerate(extras):
                            mm(pb, pb[:, :], lb, la, rb, ra, False, ei == len(extras) - 1)

                    def n_pv(rec, slot):
                        kind, qb, kb = rec
                        pb = PB[slot % 3]; ptb = ptn[slot % 3]
                        k.op(act, lambda: S.activation(out=ptb[:], in_=pb[:], func=AF.Exp), reads=[pb], writes=[ptb])
                        if kind == "cmp":
                            po = PB[(slot + 1) % 3]
                            for r in range(4):
                                mm(po, po[:, r * 97:(r + 1) * 97], ptb, ptb[:, r * 128:(r + 1) * 128], VCX, VCX[:], True, True)
                            ts(sm, sm[:, 0:4], po, po[:, 64:64 + 97 * 3 + 1:97], 1e-30, ALU.max)
                            k.op(dve, lambda: V.reciprocal(out=sm[:, 4:8], in_=sm[:, 0:4]), reads=[sm], writes=[sm])
                            tt(sm, sm[:, 8:12], sm, sm[:, 4:8], GT, GT[:, qb, 0:12:3], ALU.mult)
                            for r in range(4):
                                ts(OA, OA[:, r, :], po, po[:, r * 97:r * 97 + 64], sm[:, 8 + r:9 + r], ALU.mult, sreads=[sm])
                            if qb >= 8:
                                ts(imp, imp[:], po, po[:, 65:97], sm[:, 4:5], ALU.mult, sreads=[sm])
                                for r in range(1, 4):
                                    stt(imp, imp[:], po, po[:, r * 97 + 65:r * 97 + 97], sm[:, 4 + r:5 + r], imp, imp[:], ALU.mult, ALU.add, sreads=[sm])
                                tt(score, score[:], imp, imp[:], selvalid, selvalid[:, qb - 8, :], ALU.mult)
                                tt(score, score[:], score, score[:], seladd, seladd[:, qb - 8, :], ALU.add)
                                k.op(dve, lambda: V.max(out=mx[:, 0:8], in_=score[:]), reads=[score], writes=[mx])
                                k.op(dve, lambda: V.match_replace(out=score2[:], in_to_replace=mx[:, 0:8], in_values=score[:], imm_value=-3.0e38),
                                     reads=[score, mx], writes=[score2])
                                k.op(dve, lambda: V.max(out=mx[:, 8:16], in_=score2[:]), reads=[score2], writes=[mx])
                                ts(negmb, negmb[:], score, score[:], mx[:, 15:16], ALU.is_lt, NEG, ALU.mult, sreads=[mx])
                                transpose(PT, PT[0:32, 0:128], negmb, negmb[:], ident_bf)
                                nm4 = NM4[qb % 2]
                                for r in range(4):
                                    evac(dve if r % 2 else act, nm4[0:32, r, :], PT[0:32, 0:128], [PT], [nm4])
                            return
                        sel = kind == "slc"
                        VT = VS if sel else VW
                        kb_lo = 0 if sel else max(0, qb - 4)
                        for r in range(4):
                            mm(POb[r], POb[r][:, 0:65], ptb, ptb[:, r * 128:(r + 1) * 128], VT, VT[:, kb, :], kb == kb_lo, kb == qb)
                        if kb == qb:
                            gate_off = 1 if sel else 2
                            o0 = 16 if sel else 24
                            for r in range(4):
                                k.op(dve, lambda: V.reciprocal(out=sm[:, o0 + r:o0 + r + 1], in_=POb[r][:, 64:65]), reads=[POb[r]], writes=[sm])
                            tt(sm, sm[:, o0 + 4:o0 + 8], sm, sm[:, o0:o0 + 4], GT, GT[:, qb, gate_off:12:3], ALU.mult)
                            for r in range(4):
                                stt(OA, OA[:, r, :], POb[r], POb[r][:, 0:64], sm[:, o0 + 4 + r:o0 + 5 + r], OA, OA[:, r, :], ALU.mult, ALU.add, sreads=[sm])
                            if sel:
                                k.op(act, lambda: S.activation(out=junk[:, 0:256], in_=OA[:].rearrange("p r d -> p (r d)"), func=AF.Square,
                                                               accum_out=sstmp[:, 1:2]), reads=[OA], writes=[junk, sstmp])
                                tt(ssn, ssn[:, qb:qb + 1], ssn, ssn[:, qb:qb + 1], sstmp, sstmp[:, 1:2], ALU.add)
                                k.op(dve, lambda: V.tensor_copy(out=OJn[:, qb, :], in_=OA[:].rearrange("p r d -> p (r d)")), reads=[OA], writes=[OJn])
                                precast_step(1)

                    recs = []
                    for qb in range(NT):
                        recs.append(("cmp", qb, 0))
                        recs += [("win", qb, kb) for kb in range(max(0, qb - 4), qb + 1)]
                        recs += [("slc", qb, kb) for kb in range(0, qb + 1)]
                    slots = []
                    sl_ = 0
                    for rec in recs:
                        slots.append(sl_)
                        sl_ += 2 if rec[0] == "cmp" else 1
                    cb_load(0)
                    n_qk(recs[0], slots[0])
                    for t_ in range(len(recs)):
                        if t_ + 1 < len(recs):
                            n_qk(recs[t_ + 1], slots[t_ + 1])
                        n_pv(recs[t_], slots[t_])
                    for i in range(NT):
                        o_store(o_scr[i * 128:(i + 1) * 128, g * 256:(g + 1) * 256], OJn, OJn[:, i, :])

        if stop_after == "nsa":
            k.dma(sp, dbg["d_o"][:], o_scr[:], reads=o_views, writes=[dbg["d_o"]])
            return done([dbg["d_o"]])

        OH1a = k.sb("OH1a", [128, NT, 32], F32)
        OH2a = k.sb("OH2a", [128, NT, 32], F32)
        SELb = k.sb("SELb", [128, NT, 32], BF16)
        Wk = k.sb("Wk", [128, NT, 2], F32)
        ROWI = k.sb("ROWI", [128, NT, 2], I32)
        with Scope(k) as eO:
            WO = k.sb("WO", [128, 16, D], BF16, es=eO)
            onw_ld = k.sb("onw_ld", [16, 128], F32, es=eO)
            onw_col = k.sb("onw_col", [128, 16], F32, es=eO)
            rs_n = k.sb("rs_n", [128, NT], F32, es=eO)
            rs_f = k.sb("rs_f", [128, NT], F32, es=eO)
            fnw_bc = k.sb("fnw_bc", [128, D], F32, es=eO)
            WR = k.sb("WR", [128, 16, 36], F32, es=eO)
            RB = k.sb("RB", [128, 36], F32, es=eO)
            ot = [k.sb(f"ot{i}", [128, D], BF16, es=eO) for i in range(2)]
            oT = k.sb("oT", [128, 16, 128], BF16, es=eO)
            xs2 = [k.sb(f"xs2{i}", [128, D], F32, es=eO) for i in range(2)]
            h1t = k.sb("h1t", [128, D], F32, es=eO)
            hn32 = k.sb("hn32", [128, D], F32, es=eO)
            hnb = k.sb("hnb", [128, D], BF16, es=eO)
            hnT = k.sb("hnT", [128, 16, 128], F32, es=eO)
            lg = k.sb("lg", [128, 36], F32, es=eO)
            rt = k.sb("rt", [128, 64], F32, es=eO)
            elg = k.sb("elg", [128, 8], F32, es=eO)
            oh = k.sb("oh", [128, 24], F32, es=eO)
            wso = [k.sb(f"wso{i}", [128, D], F32, es=eO) for i in range(2)]
            for c in range(16):
                k.dma(sp, wso[c % 2][:], P["w_out"].t[0, c * 128:(c + 1) * 128, :], reads=[P["w_out"]], writes=[wso[c % 2]])
                k.op(pool, lambda: G.tensor_copy(out=WO[:, c, :], in_=wso[c % 2][:]), reads=[wso[c % 2]], writes=[WO])
            k.dma(sp, onw_ld[0:8, :], P["nsa_out_norm_w"].t[0].rearrange("(c p) -> c p", p=128), reads=[P["nsa_out_norm_w"]], writes=[onw_ld])
            k.dma(sp, onw_ld[8:16, :], P["fox_out_norm_w"].t[0].rearrange("(c p) -> c p", p=128), reads=[P["fox_out_norm_w"]], writes=[onw_ld])
            transpose(PB[0], PB[0][:, 0:16], onw_ld, onw_ld[:], ident_f, kp=16)
            evac(dve, onw_col[:], PB[0][:, 0:16], [PB[0]], [onw_col])
            k.dma(sp, fnw_bc[:], bass.AP(P["ffn_norm_w"].t, 0, [[0, 128], [1, D]]), reads=[P["ffn_norm_w"]], writes=[fnw_bc])
            with nc.allow_non_contiguous_dma(reason="small router weights"):
                k.dma(sp, WR[:, :, 0:4], P["router_group_w"].t[0].rearrange("(c p) n -> p c n", p=128), reads=[P["router_group_w"]], writes=[WR])
                k.dma(sp, WR[:, :, 4:36], P["router_expert_w"].t[0].rearrange("(c p) n -> p c n", p=128), reads=[P["router_expert_w"]], writes=[WR])
            k.dma(sp, RB[:, 0:4], bass.AP(P["router_group_b"].t, 0, [[0, 128], [1, 4]]), reads=[P["router_group_b"]], writes=[RB])
            k.dma(sp, RB[:, 4:36], bass.AP(P["router_expert_b"].t, 0, [[0, 128], [1, 32]]), reads=[P["router_expert_b"]], writes=[RB])
            for i in range(NT):
                rstd_from_ss(ssn[:, i:i + 1], ssn, rs_n[:, i:i + 1], rs_n, 1024)
                rstd_from_ss(ssf[:, i:i + 1], ssf, rs_f[:, i:i + 1], rs_f, 1024)
            def oproj_load(i):
                k.dma(sp, ot[i % 2][:], o_scr[i * 128:(i + 1) * 128, :], reads=o_views, writes=[ot[i % 2]])
                k.dma(sp, xs2[i % 2][:], x_d[i * 128:(i + 1) * 128, :], reads=[x_d], writes=[xs2[i % 2]])

            oproj_load(0)
            for i in range(NT):
                o_t = ot[i % 2]; xs = xs2[i % 2]
                if i + 1 < NT:
                    oproj_load(i + 1)
                for c4 in range(4):
                    for cc in range(4):
                        c = c4 * 4 + cc
                        transpose(PT, PT[:, cc * 128:(cc + 1) * 128], o_t, o_t[:, c * 128:(c + 1) * 128], ident_bf)
                    for cc in range(4):
                        c = c4 * 4 + cc
                        ts(oT, oT[:, c, :], PT, PT[:, cc * 128:(cc + 1) * 128], onw_col[:, c:c + 1], ALU.mult, sreads=[onw_col])
                for dmb in range(4):
                    pn = PB[(2 * dmb) % 4]; pf = PB[(2 * dmb + 1) % 4]
                    for c in range(8):
                        mm(pn, pn[:], oT, oT[:, c, :], WO, WO[:, c, dmb * 512:(dmb + 1) * 512], c == 0, c == 7)
                    for c in range(8, 16):
                        mm(pf, pf[:], oT, oT[:, c, :], WO, WO[:, c, dmb * 512:(dmb + 1) * 512], c == 8, c == 15)
                    sl = slice(dmb * 512, (dmb + 1) * 512)
                    stt(h1t, h1t[:, sl], pn, pn[:], rs_n[:, i:i + 1], xs, xs[:, sl], ALU.mult, ALU.add, sreads=[rs_n])
                    stt(h1t, h1t[:, sl], pf, pf[:], rs_f[:, i:i + 1], h1t, h1t[:, sl], ALU.mult, ALU.add, sreads=[rs_f])
                k.dma(sp, h1_scr[i * 128:(i + 1) * 128, :], h1t[:], reads=[h1t], writes=[h1_scr])
                k.op(act, lambda: S.activation(out=junk[:], in_=h1t[:], func=AF.Square, accum_out=sstmp[:, 0:1]), reads=[h1t], writes=[junk, sstmp])
                rstd_from_ss(sstmp[:, 0:1], sstmp, rt[:, 0:1], rt, D)
                stt(hn32, hn32[:], h1t, h1t[:], rt[:, 0:1], fnw_bc, fnw_bc[:], ALU.mult, ALU.mult, sreads=[rt])
                k.op(act, lambda: S.copy(out=hnb[:], in_=hn32[:]), reads=[hn32], writes=[hnb])
                k.dma(sp, hn_scr[i * 128:(i + 1) * 128, :], hnb[:], reads=[hnb], writes=[hn_scr])
                for c4 in range(4):
                    pb = PB[4 + c4 % 2]
                    for cc in range(4):
                        c = c4 * 4 + cc
                        transpose(pb, pb[:, cc * 128:(cc + 1) * 128], hn32, hn32[:, c * 128:(c + 1) * 128], ident_f)
                    evac(act if c4 % 2 else dve, hnT[:, c4 * 4:(c4 + 1) * 4, :], pb[:].rearrange("p (c t) -> p c t", c=4), [pb], [hnT])
                pl = PB[6]
                for c in range(16):
                    mm(pl, pl[:, 0:36], hnT, hnT[:, c, :], WR, WR[:, c, :], c == 0, c == 15)
                tt(lg, lg[:], pl, pl[:, 0:36], RB, RB[:], ALU.add)
                k.op(dve, lambda: V.reduce_max(out=rt[:, 1:2], in_=lg[:, 0:4], axis=AX.X), reads=[lg], writes=[rt])
                ts(oh, oh[:, 0:4], lg, lg[:, 0:4], rt[:, 1:2], ALU.is_ge, sreads=[rt])
                ts(rt, rt[:, 2:3], rt, rt[:, 1:2], -1.0, ALU.mult)
                k.op(act, lambda: S.activation(out=rt[:, 8:12], in_=lg[:, 0:4], func=AF.Exp, bias=rt[:, 2:3], accum_out=rt[:, 3:4]),
                     reads=[lg, rt], writes=[rt])
                k.op(dve, lambda: V.reciprocal(out=rt[:, 4:5], in_=rt[:, 3:4]), reads=[rt], writes=[rt])
                ts(elg, elg[:], lg, lg[:, 4:12], oh[:, 0:1], ALU.mult, sreads=[oh])
                for gg in range(1, 4):
                    stt(elg, elg[:], lg, lg[:, 4 + gg * 8:12 + gg * 8], oh[:, gg:gg + 1], elg, elg[:], ALU.mult, ALU.add, sreads=[oh])
                k.op(dve, lambda: V.reduce_max(out=rt[:, 5:6], in_=elg[:], axis=AX.X), reads=[elg], writes=[rt])
                ts(oh, oh[:, 8:16], elg, elg[:], rt[:, 5:6], ALU.is_ge, sreads=[rt])
                stt(elg, elg[:], oh, oh[:, 8:16], -1.0e30, elg, elg[:], ALU.mult, ALU.add)
                k.op(dve, lambda: V.reduce_max(out=rt[:, 6:7], in_=elg[:], axis=AX.X), reads=[elg], writes=[rt])
                ts(oh, oh[:, 16:24], elg, elg[:], rt[:, 6:7], ALU.is_ge, sreads=[rt])
                tt(rt, rt[:, 7:8], rt, rt[:, 6:7], rt, rt[:, 5:6], ALU.subtract)
                k.op(act, lambda: S.activation(out=rt[:, 12:13], in_=rt[:, 7:8], func=AF.Exp), reads=[rt], writes=[rt])
                ts(rt, rt[:, 13:14], rt, rt[:, 12:13], 1.0, ALU.add)
                k.op(dve, lambda: V.reciprocal(out=rt[:, 14:15], in_=rt[:, 13:14]), reads=[rt], writes=[rt])
                tt(Wk, Wk[:, i, 0:1], rt, rt[:, 14:15], rt, rt[:, 4:5], ALU.mult)
                tt(rt, rt[:, 15:16], rt, rt[:, 14:15], rt, rt[:, 12:13], ALU.mult)
                tt(Wk, Wk[:, i, 1:2], rt, rt[:, 15:16], rt, rt[:, 4:5], ALU.mult)
                for gg in range(4):
                    ts(OH1a, OH1a[:, i, gg * 8:(gg + 1) * 8], oh, oh[:, 8:16], oh[:, gg:gg + 1], ALU.mult, sreads=[oh])
                    ts(OH2a, OH2a[:, i, gg * 8:(gg + 1) * 8], oh, oh[:, 16:24], oh[:, gg:gg + 1], ALU.mult, sreads=[oh])
                tt(SELb, SELb[:, i, :], OH1a, OH1a[:, i, :], OH2a, OH2a[:, i, :], ALU.add)

        if stop_after == "oproj":
            k.dma(sp, dbg["d_h1"][:], h1_scr[:], reads=[h1_scr], writes=[dbg["d_h1"]])
            return done([dbg["d_h1"]])

        IDXI = k.sb("IDXI", [128, NSLOT], I32)
        x_views = []
        with Scope(k) as eR:
            onesb = k.sb("onesb", [128, 128], BF16, es=eR)
            stri = k.sb("stri", [128, 128], BF16, es=eR)
            ncnt = k.sb("ncnt", [128, 32], F32, es=eR)
            tl = k.sb("tl", [128, 32], F32, es=eR)
            cA = k.sb("cA", [128, 32], F32, es=eR)
            cBb = k.sb("cBb", [128, 32], F32, es=eR)
            basef = k.sb("basef", [128, 32], F32, es=eR)
            rowf = k.sb("rowf", [128, 32], F32, es=eR)
            tmp32 = k.sb("tmp32", [128, 32], F32, es=eR)
            ROWF = k.sb("ROWF", [128, NT, 2], F32, es=eR)
            esl_f = k.sb("esl_f", [1, NSLOT + 1], F32, es=eR)
            nfl_f = k.sb("nfl_f", [1, NSLOT], F32, es=eR)
            k.op(dve, lambda: V.memset(onesb[:], 1.0), writes=[onesb])
            k.dma(sp, stri[:], CD["stri"][:], reads=[CD["stri"]], writes=[stri])
            pcnt = PB[0]
            for i in range(NT):
                mm(pcnt, pcnt[:, 0:32], onesb, onesb[:], SELb, SELb[:, i, :], i == 0, i == NT - 1)
            evac(dve, ncnt[:], pcnt[:, 0:32], [pcnt], [ncnt])
            k.op(dve, lambda: V.memset(tl[:], 0.0), writes=[tl])
            for j in range(16):
                stt(tl, tl[:], ncnt, ncnt[:], float(128 * j), tl, tl[:], ALU.is_gt, ALU.add)
            k.op(dve, lambda: V.tensor_copy(out=cA[:], in_=tl[:]), reads=[tl], writes=[cA])
            src, dst = cA, cBb
            for sft in [1, 2, 4, 8, 16]:
                k.op(dve, lambda: V.tensor_copy(out=dst[:, 0:sft], in_=src[:, 0:sft]), reads=[src], writes=[dst])
                tt(dst, dst[:, sft:32], src, src[:, sft:32], src, src[:, 0:32 - sft], ALU.add)
                src, dst = dst, src
            cum = src
            tt(basef, basef[:], cum, cum[:], tl, tl[:], ALU.subtract)
            ts(basef, basef[:], basef, basef[:], 128.0, ALU.mult)
            for i in range(NT):
                pp = PB[1 + i % 2]
                for j in range(i):
                    mm(pp, pp[:, 0:32], onesb, onesb[:], SELb, SELb[:, j, :], j == 0, False)
                mm(pp, pp[:, 0:32], stri, stri[:], SELb, SELb[:, i, :], i == 0, True)
                tt(rowf, rowf[:], pp, pp[:, 0:32], basef, basef[:], ALU.add)
                for kk, OH in enumerate([OH1a, OH2a]):
                    tt(tmp32, tmp32[:], rowf, rowf[:], OH, OH[:, i, :], ALU.mult)
                    k.op(dve, lambda: V.reduce_sum(out=ROWF[:, i, kk:kk + 1], in_=tmp32[:], axis=AX.X), reads=[tmp32], writes=[ROWF])
            k.op(dve, lambda: V.tensor_copy(out=ROWI[:], in_=ROWF[:]), reads=[ROWF], writes=[ROWI])
            k.op(dve, lambda: V.memset(esl_f[:], -1.0), writes=[esl_f])
            for s in range(NSLOT):
                k.op(dve, lambda: V.tensor_scalar(out=tmp32[0:1, :], in0=cum[0:1, :], scalar1=float(s), scalar2=None, op0=ALU.is_le,
                                                  op1=ALU.add, accum_out=esl_f[0:1, s + 1:s + 2]), reads=[cum], writes=[tmp32, esl_f])
            ts(esl_f, esl_f[0:1, 1:NSLOT + 1], esl_f, esl_f[0:1, 1:NSLOT + 1], 31.0, ALU.min)
            k.op(dve, lambda: V.memset(nfl_f[:], 1.0), writes=[nfl_f])
            tt(nfl_f, nfl_f[0:1, 2:NSLOT], esl_f, esl_f[0:1, 3:NSLOT + 1], esl_f, esl_f[0:1, 1:NSLOT - 1], ALU.not_equal)
            onesrow = k.sb("onesrow", [1, 128], F32, es=eR)
            iop = k.sb("iop", [128, 1], F32, es=eR)
            idxf = k.sb("idxf", [128, NSLOT], F32, es=eR)
            k.op(dve, lambda: V.memset(onesrow[:], 1.0), writes=[onesrow])
            k.dma(sp, iop[:], CD["iota_p"][:], reads=[CD["iota_p"]], writes=[iop])
            mm(PB[3], PB[3][:, 0:NSLOT], onesrow, onesrow[:], esl_f, esl_f[0:1, 1:NSLOT + 1], True, True)
            mm(PB[4], PB[4][:, 0:NSLOT], onesrow, onesrow[:], nfl_f, nfl_f[:], True, True)
            ts(idxf, idxf[:], PB[3], PB[3][:, 0:NSLOT], 128.0, ALU.mult, iop[:, 0:1], ALU.add, sreads=[iop])
            ts(idxf, idxf[:], idxf, idxf[:], -100000.0, ALU.add)
            tt(idxf, idxf[:], idxf, idxf[:], PB[4], PB[4][:, 0:NSLOT], ALU.mult)
            ts(idxf, idxf[:], idxf, idxf[:], 100000.0, ALU.add)
            k.op(dve, lambda: V.tensor_copy(out=IDXI[:], in_=idxf[:]), reads=[idxf], writes=[IDXI])
            hb = [k.sb(f"hb{i}", [128, D], BF16, es=eR) for i in range(2)]
            for i in range(NT):
                hbt = hb[i % 2]
                k.dma(sp, hbt[:], hn_scr[i * 128:(i + 1) * 128, :], reads=[hn_scr], writes=[hbt])
                for kk in range(2):
                    xv_ = k.view(xslot, "xs_st"); x_views.append(xv_)
                    k.dma(pool, None, None, reads=[hbt, ROWI], writes=[xv_],
                          fn=lambda: G.indirect_dma_start(out=xslot[:], out_offset=bass.IndirectOffsetOnAxis(ap=ROWI[:, i, kk:kk + 1], axis=0),
                                                          in_=hbt[:], in_offset=None))

        with Scope(k) as eM:
            WGs = [k.sb(f"WG{i}", [128, 16, 512], BF16, es=eM) for i in range(2)]
            WUs = [k.sb(f"WU{i}", [128, 16, 512], BF16, es=eM) for i in range(2)]
            WDs = [k.sb(f"WD{i}", [128, 4, D], BF16, es=eM) for i in range(2)]
            xgbs = [k.sb(f"xgb{i}", [128, D], BF16, es=eM) for i in range(2)]
            xgTs = [k.sb(f"xgT{i}", [128, 16, 128], BF16, es=eM) for i in range(2)]
            hTs = [k.sb(f"hT{i}", [128, 4, 128], BF16, es=eM) for i in range(2)]
            sgs = [k.sb(f"sg{i}", [128, 128], F32, es=eM) for i in range(2)]
            ybts = [k.sb(f"ybt{i}", [128, D], F32, es=eM) for i in range(2)]
            PTh = [k.view(PT, "PTa"), k.view(PT, "PTb")]
            precast_step(1000)
            bc_reg = G.to_reg(4095)

            def load_w_slot(s):
                for Wb, src in [(WGs[s % 2], wbf["g"]), (WUs[s % 2], wbf["u"]), (WDs[s % 2], wbf["d"])]:
                    src2d = src.t[:].rearrange("e (p c) f -> (e p) (c f)", p=128)
                    k.dma(pool, None, None, reads=pre_views + [IDXI], writes=[Wb],
                          fn=lambda: G.indirect_dma_start(out=Wb[:].rearrange("p c f -> p (c f)"), out_offset=None, in_=src2d,
                                                          in_offset=bass.IndirectOffsetOnAxis(ap=IDXI[:, s:s + 1], axis=0),
                                                          bounds_check=bc_reg, oob_is_err=False))

            def load_x_dma(s):
                xgb = xgbs[s % 2]
                k.dma(sp, xgb[:], xslot[s * 128:(s + 1) * 128, :], reads=x_views, writes=[xgb])

            def load_x_tr(s):
                xgb = xgbs[s % 2]; xgT = xgTs[s % 2]
                for c4 in range(4):
                    for cc in range(4):
                        c = c4 * 4 + cc
                        transpose(PT, PT[:, cc * 128:(cc + 1) * 128], xgb, xgb[:, c:D:16], ident_bf)
                    evac(act if c4 % 2 else dve, xgT[:, c4 * 4:(c4 + 1) * 4, :], PT[:, 0:512].rearrange("p (c t) -> p c t", c=4), [PT], [xgT])

            load_x_dma(0)
            load_x_dma(1)
            load_w_slot(0)
            load_x_tr(0)
            for s in range(NSLOT):
                xgT = xgTs[s % 2]; ybt = ybts[s % 2]; hT = hTs[s % 2]
                WG, WU, WD = WGs[s % 2], WUs[s % 2], WDs[s % 2]
                if s + 1 < NSLOT:
                    load_x_tr(s + 1)
                    if s + 2 < NSLOT:
                        load_x_dma(s + 2)
                    load_w_slot(s + 1)
                for c2 in range(4):
                    pg = PB[(2 * c2) % 4]; pu = PB[(2 * c2 + 1) % 4]; sg = sgs[c2 % 2]
                    for c in range(16):
                        mm(pg, pg[:, 0:128], WG, WG[:, c, c2:512:4], xgT, xgT[:, c, :], c == 0, c == 15)
                    for c in range(16):
                        mm(pu, pu[:, 0:128], WU, WU[:, c, c2:512:4], xgT, xgT[:, c, :], c == 0, c == 15)
                    k.op(act, lambda: S.activation(out=sg[:], in_=pg[:, 0:128], func=AF.Silu), reads=[pg], writes=[sg])
                    tt(hT, hT[:, c2, :], sg, sg[:], pu, pu[:, 0:128], ALU.mult)
                for dmb in range(4):
                    pd = PB[4 + dmb % 3]
                    for c2 in range(4):
                        mm(pd, pd[:], hT, hT[:, c2, :], WD, WD[:, c2, dmb * 512:(dmb + 1) * 512], c2 == 0, c2 == 3)
                    evac(act if dmb % 2 else dve, ybt[:, dmb * 512:(dmb + 1) * 512], pd[:], [pd], [ybt])
                k.dma(sp, yslot[s * 128:(s + 1) * 128, :], ybt[:], reads=[ybt], writes=[yslot])

        with Scope(k) as eZ:
            fw_bc = k.sb("fw_bc", [128, D], F32, es=eZ)
            k.dma(sp, fw_bc[:], bass.AP(P["final_norm_w"].t, 0, [[0, 128], [1, D]]), reads=[P["final_norm_w"]], writes=[fw_bc])
            h1b = [k.sb(f"h1b{i}", [128, D], F32, es=eZ) for i in range(2)]
            g0 = [k.sb(f"g0{i}", [128, D], F32, es=eZ) for i in range(2)]
            g1 = [k.sb(f"g1{i}", [128, D], F32, es=eZ) for i in range(2)]
            ob = [k.sb(f"ob{i}", [128, D], F32, es=eZ) for i in range(2)]
            rf = k.sb("rf", [128, 4], F32, es=eZ)
            def final_load(i):
                k.dma(sp, h1b[i % 2][:], h1_scr[i * 128:(i + 1) * 128, :], reads=[h1_scr], writes=[h1b[i % 2]])
                for kk, gb in enumerate([g0[i % 2], g1[i % 2]]):
                    k.dma(pool, None, None, reads=[yslot, ROWI], writes=[gb],
                          fn=lambda: G.indirect_dma_start(out=gb[:], out_offset=None, in_=yslot[:],
                                                          in_offset=bass.IndirectOffsetOnAxis(ap=ROWI[:, i, kk:kk + 1], axis=0)))

            final_load(0)
            for i in range(NT):
                hh = h1b[i % 2]; a0 = g0[i % 2]; a1 = g1[i % 2]; oo = ob[i % 2]
                if i + 1 < NT:
                    final_load(i + 1)
                stt(hh, hh[:], a0, a0[:], Wk[:, i, 0:1], hh, hh[:], ALU.mult, ALU.add, sreads=[Wk])
                stt(hh, hh[:], a1, a1[:], Wk[:, i, 1:2], hh, hh[:], ALU.mult, ALU.add, sreads=[Wk])
                k.op(act, lambda: S.activation(out=junk[:], in_=hh[:], func=AF.Square, accum_out=rf[:, 0:1]), reads=[hh], writes=[junk, rf])
                rstd_from_ss(rf[:, 0:1], rf, rf[:, 1:2], rf, D)
                stt(oo, oo[:], hh, hh[:], rf[:, 1:2], fw_bc, fw_bc[:], ALU.mult, ALU.mult, sreads=[rf])
                k.dma(sp, out_d[i * 128:(i + 1) * 128, :], oo[:], reads=[oo], writes=[out_d])
        return done([out_d])


_NC_CACHE = {}


def _in_maps(inputs, n_cores=8):
    consts = _consts()
    maps = []
    for c in range(n_cores):
        m = {"x": np.ascontiguousarray(inputs["x"][c])}
        for name, shape in PARAM_SPECS:
            m[name] = np.ascontiguousarray(np.asarray(inputs[name], dtype=np.float32).reshape(shape))
        for name, shape, dt in CONST_SPECS:
            m["c_" + name] = consts[name]
        maps.append(m)
    return maps


def kernel(**inputs):
    if "nc" not in _NC_CACHE:
        _NC_CACHE["nc"] = build_nc()
    nc = _NC_CACHE["nc"]
    res = run_bass_kernel_spmd(nc, _in_maps(inputs), core_ids=list(range(8)))
    return np.stack([np.asarray(r["out"], dtype=np.float32) for r in res.results], axis=0)
```

```python
import math
import os
from contextlib import ExitStack

import ml_dtypes
import numpy as np

import concourse.bass as bass
import concourse.mybir as mybir
from concourse.bass_utils import run_bass_kernel_spmd

F32 = mybir.dt.float32
BF16 = mybir.dt.bfloat16
I32 = mybir.dt.int32
AF = mybir.ActivationFunctionType
ALU = mybir.AluOpType
AX = mybir.AxisListType

T = 2048
D = 2048
NT = 16
HD = 64
NEG = -30000.0
IN_COLS = 5696
C_NQ, C_KCMP, C_VCMP, C_KSLC, C_VSLC, C_KWIN, C_VWIN, C_GATE, C_FQ, C_FK, C_FV, C_FF = (
    0, 1024, 1280, 1536, 1792, 2048, 2304, 2560, 2608, 3632, 4656, 5680)
NSLOT = 64
VOFF = 2112
VLEN = 4608


class Eng:
    def __init__(self, name, e, sem, strict_self=True):
        self.name = name; self.e = e; self.sem = sem; self.cnt = 0
        self.waited = {}; self.strict_self = strict_self


class Buf:
    def __init__(self, t, name=""):
        self.t = t; self.name = name; self.w = {}; self.r = {}

    def __getitem__(self, idx):
        return self.t[idx]


class K:
    def __init__(self, nc, es, n_dma_sems=40):
        self.nc = nc; self.es = es

        def mk(name, e, strict=True):
            return Eng(name, e, es.enter_context(nc.semaphore("sem_" + name)), strict)
        self.pe = mk("pe", nc.tensor, strict=False)
        relax = os.environ.get("K_RELAX", "") .split(",")
        self.act = mk("act", nc.scalar, strict="act" not in relax)
        self.dve = mk("dve", nc.vector, strict="dve" not in relax)
        self.pool = mk("pool", nc.gpsimd, strict="pool" not in relax)
        self.sp = mk("sp", nc.sync)
        self.dsems = [[es.enter_context(nc.semaphore(f"dsem{i}")), 0] for i in range(n_dma_sems)]
        self.dnext = 0
        self.nwaits = 0; self.nops = 0; self.ndma = 0

    def sb(self, name, shape, dt, es=None):
        return Buf((es or self.es).enter_context(self.nc.sbuf_tensor(name, list(shape), dt)), name)

    def ps(self, name, shape, dt):
        return Buf(self.es.enter_context(self.nc.psum_tensor(name, list(shape), dt)), name)

    def dram(self, name, shape, dt, kind="Internal"):
        return Buf(self.nc.dram_tensor(name, list(shape), dt, kind=kind), name)

    def view(self, buf, name=""):
        return Buf(buf.t, name or buf.name)

    def _wait(self, E, need, keep_last=False):
        pend = []
        for key, (sem, val) in need.items():
            if sem is E.sem and not E.strict_self:
                continue
            if E.waited.get(key, 0) >= val:
                continue
            pend.append((key, sem, val))
        last = None
        if keep_last and pend:
            last = pend.pop()
        for key, sem, val in pend:
            E.e.wait_ge(sem, val); E.waited[key] = val; self.nwaits += 1
        if last is not None:
            E.waited[last[0]] = last[2]
        return last

    @staticmethod
    def _merge(need, d):
        for kk, (s, v) in d.items():
            if kk not in need or need[kk][1] < v:
                need[kk] = (s, v)

    def _deps(self, reads, writes):
        need = {}
        for b in reads:
            self._merge(need, b.w)
        for b in writes:
            self._merge(need, b.w); self._merge(need, b.r)
        return need

    def op(self, E, fn, reads=(), writes=()):
        last = self._wait(E, self._deps(reads, writes), keep_last=True)
        ins = fn()
        if last is not None:
            ins._wait_ge(last[1], last[2])
        E.cnt += 1; self.nops += 1
        ins.then_inc(E.sem, 1)
        tok = (E.sem, E.cnt); key = id(E.sem)
        for b in reads:
            b.r[key] = tok
        for b in writes:
            b.w = {key: tok}; b.r = {}
        return ins

    def dma(self, E, out_ap, in_ap, reads=(), writes=(), fn=None, sems=None, **kw):
        need = self._deps(reads, writes)
        if sems is not None:
            ds = sems[0][sems[1][0] % len(sems[0])]; sems[1][0] += 1
        else:
            ds = self.dsems[self.dnext]; self.dnext = (self.dnext + 1) % len(self.dsems)
        if ds[1] > 0:
            self._merge(need, {id(ds[0]): (ds[0], ds[1])})
        self._wait(E, need)
        if fn is None:
            ins = E.e.dma_start(out=out_ap, in_=in_ap, **kw)
        else:
            ins = fn()
        ds[1] += 16; self.ndma += 1
        ins.then_inc(ds[0], 16)
        tok = (ds[0], ds[1]); key = id(ds[0])
        for b in reads:
            b.r[key] = tok
        for b in writes:
            b.w = {key: tok}; b.r = {}
        return ins

    def barrier(self):
        engs = [self.pe, self.act, self.dve, self.pool, self.sp]
        for E in engs:
            need = {}
            for F in engs:
                if F is not E and F.cnt > 0:
                    need[id(F.sem)] = (F.sem, F.cnt)
            for ds in self.dsems:
                if ds[1] > 0:
                    need[id(ds[0])] = (ds[0], ds[1])
            self._wait(E, need)

    def finish(self, bufs):
        need = {}
        for b in bufs:
            self._merge(need, b.w)
        self._wait(self.sp, need)


class Scope:
    def __init__(self, k):
        self.k = k; self.es = ExitStack()

    def __enter__(self):
        self.es.__enter__()
        return self.es

    def __exit__(self, *a):
        if a[0] is None:
            self.k.barrier()
        return self.es.__exit__(*a)


def _t5_bucket(n):
    n = np.maximum(n, 0)
    rel = np.log(np.maximum(n, 1).astype(np.float32) / np.float32(16)) / np.float32(math.log(128 / 16))
    large = 16 + (rel * np.float32(16)).astype(np.int32)
    large = np.minimum(large, 31)
    return np.where(n < 16, n, large)


def _consts():
    bf = ml_dtypes.bfloat16
    c = {}
    c["ident_bf"] = np.eye(128, dtype=np.float32).astype(bf)
    c["ident_f"] = np.eye(128, dtype=np.float32)
    i = np.arange(128)[:, None]; j = np.arange(128)[None, :]
    c["caus"] = np.where(i <= j, 0.0, NEG).astype(bf)
    c["w4m"] = np.where(i > j, 0.0, NEG).astype(bf)
    c["stri"] = (i < j).astype(np.float32).astype(bf)
    up = np.zeros((128, 4, 512), np.float32)
    for jo in range(4):
        for to in range(4):
            if jo < to:
                up[:, jo, to * 128:(to + 1) * 128] = 1.0
            elif jo == to:
                up[:, jo, to * 128:(to + 1) * 128] = (i <= j)
    c["upat"] = up
    m = np.arange(VLEN) - VOFF
    oh = np.zeros((33, VLEN), np.float32)
    bk = _t5_bucket(m)
    for idx in range(VLEN):
        if m[idx] >= 0:
            oh[bk[idx], idx] = 1.0
        else:
            oh[32, idx] = 1.0
    c["ohv"] = oh
    sel31 = np.zeros((32, 32), np.float32); sel31[31, :] = 1.0
    c["sel31"] = sel31
    am = np.zeros((128, 32), np.float32)
    for jj in range(32):
        for a in range(4):
            for b in range(2):
                cc = jj * 4 + a - b
                if 0 <= cc < 127:
                    am[cc, jj] += 1.0
    c["amat"] = am.astype(bf)
    em = np.zeros((32, 2048), np.float32)
    for jj in range(32):
        em[jj, jj * 64:(jj + 1) * 64] = 1.0
    c["emat"] = em.astype(bf)
    t = np.arange(1024, 2048)
    blk = np.arange(32)[None, :]
    cur = (t // 64)[:, None]
    valid = (blk * 64 <= t[:, None])
    forced = (blk == 0) | (blk == cur) | (blk == cur - 1)
    add = np.where(valid, np.where(forced, 1e4, 0.0), -1e30).astype(np.float32)
    c["selvalid"] = valid.astype(np.float32).reshape(8, 128, 32).transpose(1, 0, 2).copy()
    c["seladd"] = add.reshape(8, 128, 32).transpose(1, 0, 2).copy()
    c["ones_d"] = np.ones((3, 8, 2048), np.float32).astype(bf)
    c["iota_p"] = np.arange(128, dtype=np.float32).reshape(128, 1)
    return c


CONST_SPECS = [("ident_bf", [128, 128], BF16), ("ident_f", [128, 128], F32), ("caus", [128, 128], BF16),
               ("w4m", [128, 128], BF16), ("stri", [128, 128], BF16), ("upat", [128, 4, 512], F32),
               ("ohv", [33, VLEN], F32), ("sel31", [32, 32], F32), ("amat", [128, 32], BF16),
               ("emat", [32, 2048], BF16), ("selvalid", [128, 8, 32], F32), ("seladd", [128, 8, 32], F32),
               ("ones_d", [3, 8, 2048], BF16), ("iota_p", [128, 1], F32)]

PARAM_SPECS = [("attn_norm_w", [1, 2048]), ("w_in", [1, 2048, IN_COLS]), ("cmp_pe_k", [1, 32, 64]),
               ("cmp_pe_v", [1, 32, 64]), ("cmp_k_w1", [1, 2048, 128]), ("cmp_k_w2", [1, 128, 64]),
               ("cmp_v_w1", [1, 2048, 128]), ("cmp_v_w2", [1, 128, 64]), ("rel_bias_table", [32, 16]),
               ("fox_forget_b", [1, 16]), ("nsa_out_norm_w", [1, 1024]), ("fox_out_norm_w", [1, 1024]),
               ("w_out", [1, 2048, 2048]), ("ffn_norm_w", [1, 2048]), ("router_group_w", [1, 2048, 4]),
               ("router_group_b", [1, 4]), ("router_expert_w", [1, 2048, 32]), ("router_expert_b", [1, 32]),
               ("expert_w_gate", [32, 2048, 512]), ("expert_w_up", [32, 2048, 512]),
               ("expert_w_down", [32, 512, 2048]), ("final_norm_w", [2048])]


def build_nc(stop_after=None, debug=False):
    nc = bass.Bass("TRN2", target_bir_lowering=False)
    P = {}
    x_d = Buf(nc.dram_tensor("x", [T, D], F32, kind="ExternalInput"), "x")
    for name, shape in PARAM_SPECS:
        P[name] = Buf(nc.dram_tensor(name, shape, F32, kind="ExternalInput"), name)
    CD = {}
    for name, shape, dt in CONST_SPECS:
        CD[name] = Buf(nc.dram_tensor("c_" + name, shape, dt, kind="ExternalInput"), name)
    out_d = Buf(nc.dram_tensor("out", [T, D], F32, kind="ExternalOutput"), "out")
    dbg = {}
    V, S, G, TE = nc.vector, nc.scalar, nc.gpsimd, nc.tensor

    with ExitStack() as es:
        k = K(nc, es)
        pe, act, dve, pool, sp = k.pe, k.act, k.dve, k.pool, k.sp

        o_scr = k.dram("o_scr", [T, 2048], BF16)
        vscr = k.dram("vscr", [16, VLEN], BF16)
        brd = k.dram("brd", [16 * 128, VLEN], BF16)
        cscr = k.dram("cscr", [16, 6, T], BF16)
        h1_scr = k.dram("h1_scr", [T, D], F32)
        hn_scr = k.dram("hn_scr", [T, D], BF16)
        xslot = k.dram("xslot", [NSLOT * 128, D], BF16)
        yslot = k.dram("yslot", [NSLOT * 128, D], F32)
        if debug:
            for nm, shp, dt in [("d_o", [T, 2048], BF16), ("d_h1", [T, D], F32)]:
                dbg[nm] = Buf(nc.dram_tensor(nm, shp, dt, kind="ExternalOutput"), nm)

        wbf = {"g": k.dram("wbf_g", [32, 2048, 512], BF16), "u": k.dram("wbf_u", [32, 2048, 512], BF16),
               "d": k.dram("wbf_d", [32, 512, 2048], BF16)}
        pre_sems = ([[es.enter_context(nc.semaphore(f"presem{i}")), 0] for i in range(6)], [0])
        pre_list = []
        pre_views = []
        for e_ in range(32):
            for key_, pn_ in [("g", "expert_w_gate"), ("u", "expert_w_up"), ("d", "expert_w_down")]:
                vw = k.view(wbf[key_], f"wbf_{key_}{e_}")
                pre_views.append(vw)
                pre_list.append((vw, wbf[key_].t[e_], P[pn_], P[pn_].t[e_]))
        pre_pos = [0]

        def precast_step(n):
            for _ in range(n):
                if pre_pos[0] >= len(pre_list):
                    return
                vw, dst_ap, sb_, src_ap = pre_list[pre_pos[0]]; pre_pos[0] += 1
                k.dma(pool, dst_ap, src_ap, reads=[sb_], writes=[vw], sems=pre_sems)

        o_views = []

        def o_store(dst_ap, src_b, src_ap):
            v_ = k.view(o_scr, "o_st")
            k.dma(sp, dst_ap, src_ap, reads=[src_b], writes=[v_])
            o_views.append(v_)

        def done(bufs):
            k.finish(bufs)
            print("ops", k.nops, "waits", k.nwaits, "dmas", k.ndma, flush=True)
            return nc

        PB = [k.ps(f"pb{i}", [128, 512], F32) for i in range(7)]
        PT = k.ps("pt_bf", [128, 1024], BF16)

        ident_bf = k.sb("ident_bf", [128, 128], BF16)
        ident_f = k.sb("ident_f", [128, 128], F32)
        caus = k.sb("caus", [128, 128], BF16)
        for b_, nm in [(ident_bf, "ident_bf"), (ident_f, "ident_f"), (caus, "caus")]:
            k.dma(sp, b_[:], CD[nm][:], reads=[CD[nm]], writes=[b_])
        rstd_tmp = k.sb("rstd_tmp", [128, 4], F32)
        ssn = k.sb("ssn", [128, NT], F32)
        ssf = k.sb("ssf", [128, NT], F32)
        sstmp = k.sb("sstmp", [128, 2], F32)
        eps_t = k.sb("eps_t", [128, 1], F32)
        junk = k.sb("junk", [128, 2048], BF16)
        k.op(dve, lambda: V.memset(ssn[:], 0.0), writes=[ssn])
        k.op(dve, lambda: V.memset(ssf[:], 0.0), writes=[ssf])
        k.op(dve, lambda: V.memset(eps_t[:], 1e-6), writes=[eps_t])

        def evac(E, out_ap, in_ap, reads, writes, scale=None):
            if E is act:
                if scale is None:
                    return k.op(act, lambda: S.copy(out=out_ap, in_=in_ap), reads=reads, writes=writes)
                return k.op(act, lambda: S.activation(out=out_ap, in_=in_ap, func=AF.Copy, scale=scale), reads=reads, writes=writes)
            if scale is None:
                return k.op(dve, lambda: V.tensor_copy(out=out_ap, in_=in_ap), reads=reads, writes=writes)
            return k.op(dve, lambda: V.tensor_scalar(out=out_ap, in0=in_ap, scalar1=scale, scalar2=None, op0=ALU.mult),
                        reads=reads, writes=writes)

        def mm(out_b, out_ap, l_b, l_ap, r_b, r_ap, start, stop, extra_reads=()):
            return k.op(pe, lambda: TE.matmul(out_ap, l_ap, r_ap, start=start, stop=stop),
                        reads=[l_b, r_b] + list(extra_reads), writes=[out_b])

        def transpose(out_b, out_ap, in_b, in_ap, id_b, kp=128):
            return k.op(pe, lambda: TE.transpose(out_ap, in_ap, id_b[0:kp, 0:kp]), reads=[in_b, id_b], writes=[out_b])

        def rstd_from_ss(ss_ap, ss_b, out_ap, out_b, n):
            k.op(act, lambda: S.activation(out=rstd_tmp[:, 0:1], in_=ss_ap, func=AF.Ln, scale=1.0 / n, bias=eps_t[:, 0:1]),
                 reads=[ss_b, eps_t], writes=[rstd_tmp])
            k.op(act, lambda: S.activation(out=out_ap, in_=rstd_tmp[:, 0:1], func=AF.Exp, scale=-0.5),
                 reads=[rstd_tmp], writes=[out_b])

        def tt(out_b, out_ap, a_b, a_ap, b_b, b_ap, op, E=None):
            return k.op(E or dve, lambda: (E or dve).e.tensor_tensor(out=out_ap, in0=a_ap, in1=b_ap, op=op), reads=[a_b, b_b], writes=[out_b])

        def ts(out_b, out_ap, a_b, a_ap, s1, op0, s2=None, op1=None, sreads=()):
            if op1 is None:
                return k.op(dve, lambda: V.tensor_scalar(out=out_ap, in0=a_ap, scalar1=s1, scalar2=None, op0=op0),
                            reads=[a_b] + list(sreads), writes=[out_b])
            return k.op(dve, lambda: V.tensor_scalar(out=out_ap, in0=a_ap, scalar1=s1, scalar2=s2, op0=op0, op1=op1),
                        reads=[a_b] + list(sreads), writes=[out_b])

        def stt(out_b, out_ap, a_b, a_ap, sc, b_b, b_ap, op0, op1, sreads=()):
            return k.op(dve, lambda: V.scalar_tensor_tensor(out=out_ap, in0=a_ap, scalar=sc, in1=b_ap, op0=op0, op1=op1),
                        reads=[a_b, b_b] + list(sreads), writes=[out_b])

        with Scope(k) as esA:
            xnT = k.sb("xnT", [128, 16, T], BF16, es=esA)
            with Scope(k) as e1:
                anw_bc = k.sb("anw_bc", [128, D], F32, es=e1)
                k.dma(sp, anw_bc[:], bass.AP(P["attn_norm_w"].t, 0, [[0, 128], [1, D]]), reads=[P["attn_norm_w"]], writes=[anw_bc])
                xb = [k.sb(f"xb{i}", [128, D], F32, es=e1) for i in range(2)]
                xn_tm = [k.sb(f"xn_tm{i}", [128, D], BF16, es=e1) for i in range(2)]
                ssx = k.sb("ssx", [128, NT], F32, es=e1)
                rsx = k.sb("rsx", [128, NT], F32, es=e1)
                for i in range(NT):
                    xs = xb[i % 2]; xt = xn_tm[i % 2]
                    k.dma(sp, xs[:], x_d[i * 128:(i + 1) * 128, :], reads=[x_d], writes=[xs])
                    k.op(act, lambda: S.activation(out=junk[:], in_=xs[:], func=AF.Square, accum_out=ssx[:, i:i + 1]),
                         reads=[xs], writes=[junk, ssx])
                    rstd_from_ss(ssx[:, i:i + 1], ssx, rsx[:, i:i + 1], rsx, D)
                    stt(xt, xt[:], xs, xs[:], rsx[:, i:i + 1], anw_bc, anw_bc[:], ALU.mult, ALU.mult, sreads=[rsx])
                    for c4 in range(4):
                        for cc in range(4):
                            c = c4 * 4 + cc
                            transpose(PT, PT[:, cc * 128:(cc + 1) * 128], xt, xt[:, c * 128:(c + 1) * 128], ident_bf)
                        evac(act if c4 % 2 else dve, xnT[:, c4 * 4:(c4 + 1) * 4, i * 128:(i + 1) * 128],
                             PT[:, 0:512].rearrange("p (c t) -> p c t", c=4), [PT], [xnT])

            if stop_after == "xn":
                k.dma(sp, dbg["d_o"].t[:].rearrange("(p c) t -> p c t", c=16), xnT[:], reads=[xnT], writes=[dbg["d_o"]])
                return done([dbg["d_o"]])
            with Scope(k) as e2:
                tab = k.sb("tab", [33, 16], F32, es=e2)
                sel31 = k.sb("sel31", [32, 32], F32, es=e2)
                ohv = k.sb("ohv", [33, VLEN], F32, es=e2)
                vsb = k.sb("vsb", [16, VLEN], BF16, es=e2)
                k.dma(sp, tab[0:32, :], P["rel_bias_table"][:], reads=[P["rel_bias_table"]], writes=[tab])
                k.dma(sp, sel31[:], CD["sel31"][:], reads=[CD["sel31"]], writes=[sel31])
                k.dma(sp, ohv[:], CD["ohv"][:], reads=[CD["ohv"]], writes=[ohv])
                mm(PB[0], PB[0][0:32, 0:16], sel31, sel31[:], tab, tab[0:32, :], True, True)
                tt(tab, tab[0:32, :], tab, tab[0:32, :], PB[0], PB[0][0:32, 0:16], ALU.subtract)
                k.op(dve, lambda: V.memset(tab[32:33, :], NEG), writes=[tab])
                for q in range(VLEN // 512):
                    pb = PB[q % 2]
                    mm(pb, pb[0:16, :], tab, tab[:], ohv, ohv[:, q * 512:(q + 1) * 512], True, True)
                    evac(dve, vsb[:, q * 512:(q + 1) * 512], pb[0:16, :], [pb], [vsb])
                k.dma(sp, vscr[:], vsb[:], reads=[vsb], writes=[vscr])
                for h in range(16):
                    k.dma(sp, brd[h * 128:(h + 1) * 128, :], bass.AP(vscr.t, h * VLEN, [[0, 128], [1, VLEN]]), reads=[vscr], writes=[brd])

            wst_box = [None]

            def cast(E, out_b, out_ap, in_b, in_ap):
                return k.op(E, lambda: E.e.tensor_copy(out=out_ap, in_=in_ap), reads=[in_b], writes=[out_b])

            def load_w(buf, c0, col0, ncols):
                wst = wst_box[0]
                stv = wst[:, 0:16 * ncols].rearrange("p (c n) -> p c n", c=16)
                src = P["w_in"].t[0, :, col0:col0 + ncols].rearrange("(c p) n -> p c n", p=128)
                k.dma(sp, stv, src, reads=[P["w_in"]], writes=[wst])
                cast(pool, buf, buf[:, :, c0:c0 + ncols], wst, stv)

            rot = [0]

            def proj_fm(wbuf, ncols, dst_b, dst_fn, scale):
                for tc in range(4):
                    pb = PB[rot[0] % 3]; rot[0] += 1
                    for c in range(16):
                        mm(pb, pb[0:ncols, :], wbuf, wbuf[:, c, 0:ncols], xnT, xnT[:, c, tc * 512:(tc + 1) * 512], c == 0, c == 15)
                    evac(act if rot[0] % 2 else dve, dst_fn(tc), pb[0:ncols, :], [pb], [dst_b], scale)

            with Scope(k) as eC:
                wst_box[0] = k.sb("wstC", [128, 16 * 16], F32, es=eC)
                wf = k.sb("wf", [128, 16, 16], BF16, es=eC)
                fb_bc = k.sb("fb_bc", [128, 16], F32, es=eC)
                logf = k.sb("logf", [128, NT, 16], F32, es=eC)
                upat = k.sb("upat", [128, 4, 512], F32, es=eC)
                onesf = k.sb("onesf", [128, 512], F32, es=eC)
                cT = k.sb("cT", [16, T], F32, es=eC)
                r1 = k.sb("r1", [16, T], F32, es=eC)
                parts = k.sb("parts", [16, 6, T], BF16, es=eC)
                ztmp = k.sb("ztmp", [128, 16], F32, es=eC)
                load_w(wf, 0, C_FF, 16)
                k.dma(sp, fb_bc[:], bass.AP(P["fox_forget_b"].t, 0, [[0, 128], [1, 16]]), reads=[P["fox_forget_b"]], writes=[fb_bc])
                k.dma(sp, upat[:], CD["upat"][:], reads=[CD["upat"]], writes=[upat])
                k.op(dve, lambda: V.memset(onesf[:], 1.0), writes=[onesf])
                if stop_after == "wf":
                    k.dma(sp, dbg["d_o"].t[0:128, 0:256], wf[:].rearrange("p a b -> p (a b)"), reads=[wf], writes=[dbg["d_o"]])
                    k.dma(pool, dbg["d_o"].t[128:256, 0:16], fb_bc[:], reads=[fb_bc], writes=[dbg["d_o"]])
                    return done([dbg["d_o"]])
                for i in range(NT):
                    pb = PB[i % 2]
                    for c in range(16):
                        mm(pb, pb[:, 0:16], xnT, xnT[:, c, i * 128:(i + 1) * 128], wf, wf[:, c, :], c == 0, c == 15)
                    tt(ztmp, ztmp[:], pb, pb[:, 0:16], fb_bc, fb_bc[:], ALU.add)
                    k.op(act, lambda: S.activation(out=ztmp[:], in_=ztmp[:], func=AF.Exp, scale=-1.0), reads=[ztmp], writes=[ztmp])
                    k.op(act, lambda: S.activation(out=ztmp[:], in_=ztmp[:], func=AF.Ln, scale=1.0, bias=1.0), reads=[ztmp], writes=[ztmp])
                    ts(logf, logf[:, i, :], ztmp, ztmp[:], -1.0, ALU.mult)
                for q in range(4):
                    pb = PB[q % 2]
                    n = 4 * q + 4
                    for j in range(n):
                        jo = j - 4 * q
                        if jo >= 0:
                            mm(pb, pb[0:16, :], logf, logf[:, j, :], upat, upat[:, jo, :], j == 0, j == n - 1)
                        else:
                            mm(pb, pb[0:16, :], logf, logf[:, j, :], onesf, onesf[:], j == 0, False)
                    evac(dve, cT[:, q * 512:(q + 1) * 512], pb[0:16, :], [pb], [cT])
                if stop_after == "lf":
                    k.dma(sp, dbg["d_h1"].t[0:128, 0:256], logf[:].rearrange("p a b -> p (a b)"), reads=[logf], writes=[dbg["d_h1"]])
                    k.dma(sp, dbg["d_h1"].t[128:144, :], cT[:], reads=[cT], writes=[dbg["d_h1"]])
                    return done([dbg["d_h1"]])
                k.op(dve, lambda: V.tensor_copy(out=parts[:, 0, :], in_=cT[:]), reads=[cT], writes=[parts])
                tt(r1, r1[:], cT, cT[:], parts, parts[:, 0, :], ALU.subtract)
                k.op(dve, lambda: V.tensor_copy(out=parts[:, 1, :], in_=r1[:]), reads=[r1], writes=[parts])
                tt(r1, r1[:], r1, r1[:], parts, parts[:, 1, :], ALU.subtract)
                k.op(dve, lambda: V.tensor_copy(out=parts[:, 2, :], in_=r1[:]), reads=[r1], writes=[parts])
                ts(parts, parts[:, 3:6, :], parts, parts[:, 0:3, :], -1.0, ALU.mult)
                k.dma(sp, cscr[:], parts[:], reads=[parts], writes=[cscr])

            if stop_after == "cs":
                k.dma(sp, dbg["d_o"].t[0:96, :].rearrange("(h k) t -> h k t", k=6), cscr[:], reads=[cscr], writes=[dbg["d_o"]])
                return done([dbg["d_o"]])
            with Scope(k) as eF:
                NH = 4
                wst_box[0] = k.sb("wstF", [128, 16 * 256], F32, es=eF)
                FQ = k.sb("FQ", [128, NH, T], BF16, es=eF)
                FK = k.sb("FK", [128, NH, T], BF16, es=eF)
                k.op(pool, lambda: G.memset(FQ[:], 0.0), writes=[FQ])
                k.op(pool, lambda: G.memset(FK[:], 0.0), writes=[FK])
                FV = k.sb("FV", [128, NT, NH, 65], BF16, es=eF)
                OJ = k.sb("OJ", [128, NT, NH * 64], BF16, es=eF)
                wq = [k.sb(f"wq{i}", [128, 16, 128], BF16, es=eF) for i in range(2)]
                wv = k.sb("wv", [128, 16, NH * 64], BF16, es=eF)
                pt_sb = [k.sb(f"pt_sb{i}", [128, 512], BF16, es=eF) for i in range(3)]
                rz = k.sb("rz", [128, 8], F32, es=eF)
                of32s = [k.sb(f"of32_{i}", [128, 64], F32, es=eF) for i in range(2)]
                FQh = [k.view(FQ, f"FQ{h}") for h in range(NH)]
                FKh = [k.view(FK, f"FK{h}") for h in range(NH)]
                FQc = k.view(FQ, "FQc"); FKc = k.view(FK, "FKc")
                for v_ in FQh + [FQc]:
                    v_.w = dict(FQ.w)
                for v_ in FKh + [FKc]:
                    v_.w = dict(FK.w)
                k.op(dve, lambda: V.memset(FV[:, :, :, 64:65], 1.0), writes=[FV])
                POb = PB[3:7]
                for fp in range(16 // NH):
                    h0 = fp * NH
                    for pj in range(NH // 2):
                        h = h0 + 2 * pj
                        for wb, col, DST, DSTh, sc in [(wq[0], C_FQ, FQ, FQh, 0.125), (wq[1], C_FK, FK, FKh, None)]:
                            load_w(wb, 0, col + h * 64, 128)
                            for tc in range(4):
                                pb = PB[rot[0] % 3]; rot[0] += 1
                                for c in range(16):
                                    mm(pb, pb[:, :], wb, wb[:, c, :], xnT, xnT[:, c, tc * 512:(tc + 1) * 512], c == 0, c == 15)
                                evac(act, DST[0:64, 2 * pj, tc * 512:(tc + 1) * 512], pb[0:64, :], [pb], [DSTh[2 * pj]], sc)
                                evac(dve, DST[64:128, 2 * pj + 1, tc * 512:(tc + 1) * 512], pb[64:128, :], [pb], [DSTh[2 * pj + 1]], sc)
                    load_w(wv, 0, C_FV + h0 * 64, NH * 64)
                    for i in range(NT):
                        pb = PB[rot[0] % 3]; rot[0] += 1
                        for c in range(16):
                            mm(pb, pb[:, 0:NH * 64], xnT, xnT[:, c, i * 128:(i + 1) * 128], wv, wv[:, c, :], c == 0, c == 15)
                        evac(act if i % 2 else dve, FV[:, i, :, 0:64], pb[:, 0:NH * 64].rearrange("p (h d) -> p h d", h=NH), [pb], [FV])
                    for par, r0 in [(0, 64), (1, 0)]:
                        hs = slice(h0 + par, h0 + NH, 2); ls = slice(par, NH, 2); nh2 = NH // 2
                        k.dma(sp, FQ[r0:r0 + 3, ls, :], cscr.t[hs, 0:3, :].rearrange("h k t -> k h t"), reads=[cscr], writes=[FQc])
                        k.dma(sp, FQ[r0 + 3:r0 + 6, ls, :], CD["ones_d"].t[:, 0:nh2, :], reads=[CD["ones_d"]], writes=[FQc])
                        k.dma(sp, FK[r0:r0 + 3, ls, :], CD["ones_d"].t[:, 0:nh2, :], reads=[CD["ones_d"]], writes=[FKc])
                        k.dma(sp, FK[r0 + 3:r0 + 6, ls, :], cscr.t[hs, 3:6, :].rearrange("h k t -> k h t"), reads=[cscr], writes=[FKc])
                    recs = [(hl, qc, kb) for hl in range(NH) for qc in range(4) for kb in range(4 * qc + 4)]

                    def geom(rec):
                        hl, qc, kb = rec
                        dg = kb - 4 * qc
                        q0 = qc * 512 + (dg * 128 if dg > 0 else 0)
                        return hl, qc, kb, dg, q0, (qc + 1) * 512 - q0

                    def fox_qk(rec, slot):
                        hl, qc, kb, dg, q0, n = geom(rec)
                        pb = PB[slot % 3]
                        mm(pb, pb[:, 0:n], FKh[hl], FK[:, hl, kb * 128:(kb + 1) * 128], FQh[hl], FQ[:, hl, q0:q0 + n],
                           True, dg < 0, extra_reads=[FQc, FKc])
                        if dg >= 0:
                            mm(pb, pb[:, 0:128], ident_bf, ident_bf[:], caus, caus[:], False, True)

                    def fox_pv(rec, slot):
                        hl, qc, kb, dg, q0, n = geom(rec)
                        pb = PB[slot % 3]; ptb = pt_sb[slot % 3]
                        k.op(act, lambda: S.activation(out=ptb[:, 0:n], in_=pb[:, 0:n], func=AF.Exp), reads=[pb], writes=[ptb])
                        j0 = max(dg, 0)
                        for jq in range(j0, 4):
                            po = POb[jq]
                            cs = (jq - j0) * 128
                            mm(po, po[:, 0:65], ptb, ptb[:, cs:cs + 128], FV, FV[:, kb, hl, :], kb == 0, kb == 4 * qc + jq)
                        if kb == 4 * qc + 3:
                            for jq in range(4):
                                po = POb[jq]; i = qc * 4 + jq
                                k.op(dve, lambda: V.reciprocal(out=rz[:, jq:jq + 1], in_=po[:, 64:65]), reads=[po], writes=[rz])
                                of = of32s[jq % 2]
                                ts(of, of[:], po, po[:, 0:64], rz[:, jq:jq + 1], ALU.mult, sreads=[rz])
                                k.op(act, lambda: S.activation(out=junk[:, 0:64], in_=of[:], func=AF.Square, accum_out=sstmp[:, 0:1]),
                                     reads=[of], writes=[junk, sstmp])
                                tt(ssf, ssf[:, i:i + 1], ssf, ssf[:, i:i + 1], sstmp, sstmp[:, 0:1], ALU.add)
                                k.op(dve, lambda: V.tensor_copy(out=OJ[:, i, hl * 64:(hl + 1) * 64], in_=of[:]), reads=[of], writes=[OJ])
                            if qc % 2 == 1:
                                precast_step(1)

                    fox_qk(recs[0], 0)
                    for t_ in range(len(recs)):
                        if t_ + 1 < len(recs):
                            fox_qk(recs[t_ + 1], t_ + 1)
                        fox_pv(recs[t_], t_)
                    for i in range(NT):
                        c0 = 1024 + h0 * 64
                        o_store(o_scr[i * 128:(i + 1) * 128, c0:c0 + NH * 64], OJ, OJ[:, i, :])

            if stop_after == "fox":
                k.dma(sp, dbg["d_o"][:], o_scr[:], reads=o_views, writes=[dbg["d_o"]])
                return done([dbg["d_o"]])

            with Scope(k) as eN:
                wst_box[0] = k.sb("wstN", [128, 16 * 144], F32, es=eN)
                QT = k.sb("QT", [128, 4, T], BF16, es=eN)
                KTS = k.sb("KTS", [128, T], BF16, es=eN)
                KTW = k.sb("KTW", [128, T], BF16, es=eN)
                k.op(dve, lambda: V.memset(QT[:], 0.0), writes=[QT])
                for b_ in (KTS, KTW):
                    k.op(pool, lambda: G.memset(b_[:], 0.0), writes=[b_])
                CK = k.sb("CK", [64, T], BF16, es=eN)
                CV = k.sb("CV", [64, T], BF16, es=eN)
                VS = k.sb("VS", [128, NT, 65], BF16, es=eN)
                VW = k.sb("VW", [128, NT, 65], BF16, es=eN)
                GT = k.sb("GT", [128, NT, 12], F32, es=eN)
                W1K = k.sb("W1K", [64, 32, 128], BF16, es=eN)
                W1V = k.sb("W1V", [64, 32, 128], BF16, es=eN)
                W2K = k.sb("W2K", [128, 64], BF16, es=eN)
                W2V = k.sb("W2V", [128, 64], BF16, es=eN)
                pe_ld = k.sb("pe_ld", [32, 128], F32, es=eN)
                PET = k.sb("PET", [64, 2, 32], BF16, es=eN)
                b1 = k.sb("b1", [128, 2], F32, es=eN)
                HK = k.sb("HK", [128, 128], BF16, es=eN)
                HV = k.sb("HV", [128, 128], BF16, es=eN)
                KC = k.sb("KC", [128, 128], BF16, es=eN)
                VCX = k.sb("VCX", [128, 97], BF16, es=eN)
                B0 = k.sb("B0", [128, 4, 128], BF16, es=eN)
                B1 = k.sb("B1", [128, 4, 128], BF16, es=eN)
                W4X = k.sb("W4X", [128, 4, 128], BF16, es=eN)
                CB = [k.sb(f"CB{i}", [128, 4, 128], BF16, es=eN) for i in range(2)]
                NM4 = [k.sb(f"NM4{i}", [128, 4, 128], BF16, es=eN) for i in range(2)]
                emat = k.sb("emat", [128, T], BF16, es=eN)
                for b_ in NM4 + [emat]:
                    k.op(pool, lambda: G.memset(b_[:], 0.0), writes=[b_])
                selvalid = k.sb("selvalid", [128, 8, 32], F32, es=eN)
                seladd = k.sb("seladd", [128, 8, 32], F32, es=eN)
                OJn = k.sb("OJn", [128, NT, 256], BF16, es=eN)
                OA = k.sb("OA", [128, 4, 64], F32, es=eN)
                wqn = [k.sb(f"wqn{i}", [128, 16, 64], BF16, es=eN) for i in range(2)]
                wtm = k.sb("wtm", [128, 16, 144], BF16, es=eN)
                ptn = [k.sb(f"ptn{i}", [128, 512], BF16, es=eN) for i in range(3)]
                sm = k.sb("sm", [128, 64], F32, es=eN)
                imp = k.sb("imp", [128, 32], F32, es=eN)
                score = k.sb("score", [128, 32], F32, es=eN)
                score2 = k.sb("score2", [128, 32], F32, es=eN)
                mx = k.sb("mx", [128, 16], F32, es=eN)
                negmb = k.sb("negmb", [128, 32], BF16, es=eN)
                gtmp = k.sb("gtmp", [128, 12], F32, es=eN)
                k.dma(sp, emat[0:32, :], CD["emat"][:], reads=[CD["emat"]], writes=[emat])
                for b_, nm in [(selvalid, "selvalid"), (seladd, "seladd")]:
                    k.dma(sp, b_[:], CD[nm][:], reads=[CD[nm]], writes=[b_])
                k.dma(sp, W4X[:, 0, :], CD["w4m"][:], reads=[CD["w4m"]], writes=[W4X])
                for r in range(1, 4):
                    k.op(dve, lambda: V.tensor_copy(out=W4X[:, r, :], in_=W4X[:, 0, :]), reads=[W4X], writes=[W4X])
                wstn = wst_box[0]
                for (Wd, pn) in [(W1K, "cmp_k_w1"), (W1V, "cmp_v_w1")]:
                    for hf in range(2):
                        stv = wstn[0:64, 0:2048].rearrange("p (l h) -> p l h", l=16)
                        k.dma(sp, stv, P[pn].t[0, hf * 1024:(hf + 1) * 1024, :].rearrange("(l d) h -> d l h", d=64), reads=[P[pn]], writes=[wstn])
                        if hf == 0:
                            k.op(dve, lambda: V.tensor_copy(out=Wd[:, hf * 16:(hf + 1) * 16, :], in_=stv), reads=[wstn], writes=[Wd])
                        else:
                            k.op(act, lambda: S.copy(out=Wd[:, hf * 16:(hf + 1) * 16, :], in_=stv), reads=[wstn], writes=[Wd])
                for (Wd, pn) in [(W2K, "cmp_k_w2"), (W2V, "cmp_v_w2")]:
                    k.dma(sp, wstn[:, 0:64], P[pn].t[0], reads=[P[pn]], writes=[wstn])
                    cast(pool, Wd, Wd[:], wstn, wstn[:, 0:64])
                k.dma(sp, pe_ld[:, 0:64], P["cmp_pe_k"].t[0], reads=[P["cmp_pe_k"]], writes=[pe_ld])
                k.dma(sp, pe_ld[:, 64:128], P["cmp_pe_v"].t[0], reads=[P["cmp_pe_v"]], writes=[pe_ld])
                for kv in range(2):
                    transpose(PB[0], PB[0][0:64, 0:32], pe_ld, pe_ld[:, kv * 64:(kv + 1) * 64], ident_f, kp=32)
                    evac(dve, PET[:, kv, :], PB[0][0:64, 0:32], [PB[0]], [PET])
                    W1 = W1K if kv == 0 else W1V
                    for l in range(32):
                        mm(PB[1], PB[1][:, 0:1], W1, W1[:, l, :], PET, PET[:, kv, l:l + 1], l == 0, l == 31)
                    evac(dve, b1[:, kv:kv + 1], PB[1][:, 0:1], [PB[1]], [b1])
                k.op(dve, lambda: V.memset(VS[:, :, 64:65], 1.0), writes=[VS])
                k.op(dve, lambda: V.memset(VW[:, :, 64:65], 1.0), writes=[VW])
                k.op(dve, lambda: V.memset(KC[:], 0.0), writes=[KC])
                k.op(dve, lambda: V.memset(VCX[:], 0.0), writes=[VCX])
                k.op(dve, lambda: V.memset(VCX[:, 64:65], 1.0), writes=[VCX])
                k.dma(sp, VCX[:, 65:97], CD["amat"][:], reads=[CD["amat"]], writes=[VCX])
                POb = PB[3:7]

                if stop_after == "nsa0":
                    k.dma(sp, dbg["d_o"][:], o_scr[:], reads=o_views, writes=[dbg["d_o"]])
                    return done([dbg["d_o"]])
                for g in range(4):
                    for r in range(4):
                        wb = wqn[r % 2]
                        load_w(wb, 0, C_NQ + (4 * g + r) * 64, 64)
                        proj_fm(wb, 64, QT, lambda tc: QT[0:64, r, tc * 512:(tc + 1) * 512], 0.125)
                    for ii, (col, dst) in enumerate([(C_KSLC, KTS), (C_KWIN, KTW), (C_KCMP, CK), (C_VCMP, CV)]):
                        wb = wqn[ii % 2]
                        load_w(wb, 0, col + g * 64, 64)
                        proj_fm(wb, 64, dst, lambda tc: dst[0:64, tc * 512:(tc + 1) * 512], None)
                    if os.environ.get("NSA_SKIP") == "qk":
                        k.dma(sp, dbg["d_o"][:], o_scr[:], reads=o_views, writes=[dbg["d_o"]])
                        return done([dbg["d_o"]])
                    wstn_ = wst_box[0]
                    stv_all = wstn_[:, 0:16 * 144].rearrange("p (c n) -> p c n", c=16)
                    for (c0_, col_, n_) in [(0, C_VSLC + g * 64, 64), (64, C_VWIN + g * 64, 64), (128, C_GATE + g * 12, 16)]:
                        k.dma(sp, stv_all[:, :, c0_:c0_ + n_], P["w_in"].t[0, :, col_:col_ + n_].rearrange("(c p) n -> p c n", p=128),
                              reads=[P["w_in"]], writes=[wstn_])
                    cast(pool, wtm, wtm[:], wstn_, stv_all)
                    for i in range(NT):
                        pb = PB[rot[0] % 3]; rot[0] += 1
                        for c in range(16):
                            mm(pb, pb[:, 0:144], xnT, xnT[:, c, i * 128:(i + 1) * 128], wtm, wtm[:, c, :], c == 0, c == 15)
                        evac(dve, VS[:, i, 0:64], pb[:, 0:64], [pb], [VS])
                        evac(dve, VW[:, i, 0:64], pb[:, 64:128], [pb], [VW])
                        evac(dve, gtmp[:], pb[:, 128:140], [pb], [gtmp])
                        k.op(act, lambda: S.activation(out=gtmp[:], in_=gtmp[:], func=AF.Exp, scale=-1.0), reads=[gtmp], writes=[gtmp])
                        ts(gtmp, gtmp[:], gtmp, gtmp[:], 1.0, ALU.add)
                        k.op(dve, lambda: V.reciprocal(out=GT[:, i, :], in_=gtmp[:]), reads=[gtmp], writes=[GT])
                    if os.environ.get("NSA_SKIP") == "proj":
                        k.dma(sp, dbg["d_o"][:], o_scr[:], reads=o_views, writes=[dbg["d_o"]])
                        return done([dbg["d_o"]])
                    for kv, (SRC, W1, H) in enumerate([(CK, W1K, HK), (CV, W1V, HV)]):
                        pb = PB[rot[0] % 3]; rot[0] += 1
                        for l in range(32):
                            mm(pb, pb[:, 0:127], W1, W1[:, l, :], SRC, SRC[:, l:l + 2017:16], l == 0, l == 31)
                        k.op(act, lambda: S.activation(out=H[:, 0:127], in_=pb[:, 0:127], func=AF.Silu, bias=b1[:, kv:kv + 1]),
                             reads=[pb, b1], writes=[H])
                    pb = PB[rot[0] % 3]; rot[0] += 1
                    mm(pb, pb[0:64, 0:127], W2K, W2K[:], HK, HK[:, 0:127], True, True)
                    evac(dve, KC[0:64, 0:127], pb[0:64, 0:127], [pb], [KC])
                    pb = PB[rot[0] % 3]; rot[0] += 1
                    mm(pb, pb[0:127, 0:64], HV, HV[:, 0:127], W2V, W2V[:], True, True)
                    evac(dve, VCX[0:127, 0:64], pb[0:127, 0:64], [pb], [VCX])
                    base = (4 * g) * 128 * VLEN + VOFF
                    if os.environ.get("NSA_SKIP") == "cmp":
                        k.dma(sp, dbg["d_o"][:], o_scr[:], reads=o_views, writes=[dbg["d_o"]])
                        return done([dbg["d_o"]])
                    k.dma(sp, B0[:], bass.AP(brd.t, base, [[VLEN - 1, 128], [128 * VLEN, 4], [1, 128]]), reads=[brd], writes=[B0])
                    k.dma(sp, B1[:], bass.AP(brd.t, base + 128, [[VLEN - 1, 128], [128 * VLEN, 4], [1, 128]]), reads=[brd], writes=[B1])

                    if stop_after == "nsa1":
                        k.dma(sp, dbg["d_o"][:], o_scr[:], reads=o_views, writes=[dbg["d_o"]])
                        return done([dbg["d_o"]])

                    def cb_load(qb):
                        cb = CB[qb % 2]
                        k.dma(sp, cb[:], bass.AP(brd.t, base + 128 * qb - 31, [[VLEN - 16, 128], [128 * VLEN, 4], [1, 128]]), reads=[brd], writes=[cb])

                    def n_qk(rec, slot):
                        kind, qb, kb = rec
                        pb = PB[slot % 3]
                        qap = QT[:, :, qb * 128:(qb + 1) * 128]
                        if kind == "cmp":
                            if qb + 1 < NT:
                                cb_load(qb + 1)
                            cb = CB[qb % 2]
                            mm(pb, pb[:, :], KC, KC[:], QT, qap, True, False)
                            mm(pb, pb[:, :], ident_bf, ident_bf[:], cb, cb[:], False, True)
                            return
                        sel = kind == "slc"
                        KT = KTS if sel else KTW
                        extras = []
                        if kb == qb:
                            extras.append((ident_bf, ident_bf[:], B0, B0[:]))
                        elif kb == qb - 1:
                            extras.append((ident_bf, ident_bf[:], B1, B1[:]))
                        if (not sel) and kb == qb - 4:
                            extras.append((ident_bf, ident_bf[:], W4X, W4X[:]))
                        if sel and qb >= 8:
                            nm4 = NM4[qb % 2]
                            extras.append((emat, emat[:, kb * 128:(kb + 1) * 128], nm4, nm4[:]))
                        mm(pb, pb[:, :], KT, KT[:, kb * 128:(kb + 1) * 128], QT, qap, True, len(extras) == 0)
                        for ei, (lb, la, rb, ra) in enumerate(extras):
                            mm(pb, pb[:, :], lb, la, rb, ra, False, ei == len(extras) - 1)

                    def n_pv(rec, slot):
                        kind, qb, kb = rec
                        pb = PB[slot % 3]; ptb = ptn[slot % 3]
                        k.op(act, lambda: S.activation(out=ptb[:], in_=pb[:], func=AF.Exp), reads=[pb], writes=[ptb])
                        if kind == "cmp":
                            po = PB[(slot + 1) % 3]
                            for r in range(4):
                                mm(po, po[:, r * 97:(r + 1) * 97], ptb, ptb[:, r * 128:(r + 1) * 128], VCX, VCX[:], True, True)
                            ts(sm, sm[:, 0:4], po, po[:, 64:64 + 97 * 3 + 1:97], 1e-30, ALU.max)
                            k.op(dve, lambda: V.reciprocal(out=sm[:, 4:8], in_=sm[:, 0:4]), reads=[sm], writes=[sm])
                            tt(sm, sm[:, 8:12], sm, sm[:, 4:8], GT, GT[:, qb, 0:12:3], ALU.mult)
                            for r in range(4):
                                ts(OA, OA[:, r, :], po, po[:, r * 97:r * 97 + 64], sm[:, 8 + r:9 + r], ALU.mult, sreads=[sm])
                            if qb >= 8:
                                ts(imp, imp[:], po, po[:, 65:97], sm[:, 4:5], ALU.mult, sreads=[sm])
                                for r in range(1, 4):
                                    stt(imp, imp[:], po, po[:, r * 97 + 65:r * 97 + 97], sm[:, 4 + r:5 + r], imp, imp[:], ALU.mult, ALU.add, sreads=[sm])
                                tt(score, score[:], imp, imp[:], selvalid, selvalid[:, qb - 8, :], ALU.mult)
                                tt(score, score[:], score, score[:], seladd, seladd[:, qb - 8, :], ALU.add)
                                k.op(dve, lambda: V.max(out=mx[:, 0:8], in_=score[:]), reads=[score], writes=[mx])
                                k.op(dve, lambda: V.match_replace(out=score2[:], in_to_replace=mx[:, 0:8], in_values=score[:], imm_value=-3.0e38),
                                     reads=[score, mx], writes=[score2])
                                k.op(dve, lambda: V.max(out=mx[:, 8:16], in_=score2[:]), reads=[score2], writes=[mx])
                                ts(negmb, negmb[:], score, score[:], mx[:, 15:16], ALU.is_lt, NEG, ALU.mult, sreads=[mx])
                                transpose(PT, PT[0:32, 0:128], negmb, negmb[:], ident_bf)
                                nm4 = NM4[qb % 2]
                                for r in range(4):
                                    evac(dve if r % 2 else act, nm4[0:32, r, :], PT[0:32, 0:128], [PT], [nm4])
                            return
                        sel = kind == "slc"
                        VT = VS if sel else VW
                        kb_lo = 0 if sel else max(0, qb - 4)
                        for r in range(4):
                            mm(POb[r], POb[r][:, 0:65], ptb, ptb[:, r * 128:(r + 1) * 128], VT, VT[:, kb, :], kb == kb_lo, kb == qb)
                        if kb == qb:
                            gate_off = 1 if sel else 2
                            o0 = 16 if sel else 24
                            for r in range(4):
                                k.op(dve, lambda: V.reciprocal(out=sm[:, o0 + r:o0 + r + 1], in_=POb[r][:, 64:65]), reads=[POb[r]], writes=[sm])
                            tt(sm, sm[:, o0 + 4:o0 + 8], sm, sm[:, o0:o0 + 4], GT, GT[:, qb, gate_off:12:3], ALU.mult)
                            for r in range(4):
                                stt(OA, OA[:, r, :], POb[r], POb[r][:, 0:64], sm[:, o0 + 4 + r:o0 + 5 + r], OA, OA[:, r, :], ALU.mult, ALU.add, sreads=[sm])
                            if sel:
                                k.op(act, lambda: S.activation(out=junk[:, 0:256], in_=OA[:].rearrange("p r d -> p (r d)"), func=AF.Square,
                                                               accum_out=sstmp[:, 1:2]), reads=[OA], writes=[junk, sstmp])
                                tt(ssn, ssn[:, qb:qb + 1], ssn, ssn[:, qb:qb + 1], sstmp, sstmp[:, 1:2], ALU.add)
                                k.op(dve, lambda: V.tensor_copy(out=OJn[:, qb, :], in_=OA[:].rearrange("p r d -> p (r d)")), reads=[OA], writes=[OJn])
                                precast_step(1)

                    recs = []
                    for qb in range(NT):
                        recs.append(("cmp", qb, 0))
                        recs += [("win", qb, kb) for kb in range(max(0, qb - 4), qb + 1)]
                        recs += [("slc", qb, kb) for kb in range(0, qb + 1)]
                    slots = []
                    sl_ = 0
                    for rec in recs:
                        slots.append(sl_)
                        sl_ += 2 if rec[0] == "cmp" else 1
                    cb_load(0)
                    n_qk(recs[0], slots[0])
                    for t_ in range(len(recs)):
                        if t_ + 1 < len(recs):
                            n_qk(recs[t_ + 1], slots[t_ + 1])
                        n_pv(recs[t_], slots[t_])
                    for i in range(NT):
                        o_store(o_scr[i * 128:(i + 1) * 128, g * 256:(g + 1) * 256], OJn, OJn[:, i, :])

        if stop_after == "nsa":
            k.dma(sp, dbg["d_o"][:], o_scr[:], reads=o_views, writes=[dbg["d_o"]])
            return done([dbg["d_o"]])

        OH1a = k.sb("OH1a", [128, NT, 32], F32)
        OH2a = k.sb("OH2a", [128, NT, 32], F32)
        SELb = k.sb("SELb", [128, NT, 32], BF16)
        Wk = k.sb("Wk", [128, NT, 2], F32)
        ROWI = k.sb("ROWI", [128, NT, 2], I32)
        with Scope(k) as eO:
            WO = k.sb("WO", [128, 16, D], BF16, es=eO)
            onw_ld = k.sb("onw_ld", [16, 128], F32, es=eO)
            onw_col = k.sb("onw_col", [128, 16], F32, es=eO)
            rs_n = k.sb("rs_n", [128, NT], F32, es=eO)
            rs_f = k.sb("rs_f", [128, NT], F32, es=eO)
            fnw_bc = k.sb("fnw_bc", [128, D], F32, es=eO)
            WR = k.sb("WR", [128, 16, 36], F32, es=eO)
            RB = k.sb("RB", [128, 36], F32, es=eO)
            ot = [k.sb(f"ot{i}", [128, D], BF16, es=eO) for i in range(2)]
            oT = k.sb("oT", [128, 16, 128], BF16, es=eO)
            xs2 = [k.sb(f"xs2{i}", [128, D], F32, es=eO) for i in range(2)]
            h1t = k.sb("h1t", [128, D], F32, es=eO)
            hn32 = k.sb("hn32", [128, D], F32, es=eO)
            hnb = k.sb("hnb", [128, D], BF16, es=eO)
            hnT = k.sb("hnT", [128, 16, 128], F32, es=eO)
            lg = k.sb("lg", [128, 36], F32, es=eO)
            rt = k.sb("rt", [128, 64], F32, es=eO)
            elg = k.sb("elg", [128, 8], F32, es=eO)
            oh = k.sb("oh", [128, 24], F32, es=eO)
            wso = [k.sb(f"wso{i}", [128, D], F32, es=eO) for i in range(2)]
            WOv = [k.view(WO, f"WO{c}") for c in range(16)]
            for c in range(16):
                k.dma(sp, wso[c % 2][:], P["w_out"].t[0, c * 128:(c + 1) * 128, :], reads=[P["w_out"]], writes=[wso[c % 2]])
                if c % 3 == 0:
                    k.op(pool, lambda: G.tensor_copy(out=WO[:, c, :], in_=wso[c % 2][:]), reads=[wso[c % 2]], writes=[WOv[c]])
                elif c % 3 == 1:
                    k.op(dve, lambda: V.tensor_copy(out=WO[:, c, :], in_=wso[c % 2][:]), reads=[wso[c % 2]], writes=[WOv[c]])
                else:
                    k.op(act, lambda: S.copy(out=WO[:, c, :], in_=wso[c % 2][:]), reads=[wso[c % 2]], writes=[WOv[c]])
            k.dma(sp, onw_ld[0:8, :], P["nsa_out_norm_w"].t[0].rearrange("(c p) -> c p", p=128), reads=[P["nsa_out_norm_w"]], writes=[onw_ld])
            k.dma(sp, onw_ld[8:16, :], P["fox_out_norm_w"].t[0].rearrange("(c p) -> c p", p=128), reads=[P["fox_out_norm_w"]], writes=[onw_ld])
            transpose(PB[0], PB[0][:, 0:16], onw_ld, onw_ld[:], ident_f, kp=16)
            evac(dve, onw_col[:], PB[0][:, 0:16], [PB[0]], [onw_col])
            k.dma(sp, fnw_bc[:], bass.AP(P["ffn_norm_w"].t, 0, [[0, 128], [1, D]]), reads=[P["ffn_norm_w"]], writes=[fnw_bc])
            with nc.allow_non_contiguous_dma(reason="small router weights"):
                k.dma(sp, WR[:, :, 0:4], P["router_group_w"].t[0].rearrange("(c p) n -> p c n", p=128), reads=[P["router_group_w"]], writes=[WR])
                k.dma(sp, WR[:, :, 4:36], P["router_expert_w"].t[0].rearrange("(c p) n -> p c n", p=128), reads=[P["router_expert_w"]], writes=[WR])
            k.dma(sp, RB[:, 0:4], bass.AP(P["router_group_b"].t, 0, [[0, 128], [1, 4]]), reads=[P["router_group_b"]], writes=[RB])
            k.dma(sp, RB[:, 4:36], bass.AP(P["router_expert_b"].t, 0, [[0, 128], [1, 32]]), reads=[P["router_expert_b"]], writes=[RB])
            for i in range(NT):
                rstd_from_ss(ssn[:, i:i + 1], ssn, rs_n[:, i:i + 1], rs_n, 1024)
                rstd_from_ss(ssf[:, i:i + 1], ssf, rs_f[:, i:i + 1], rs_f, 1024)
            def oproj_load(i):
                k.dma(sp, ot[i % 2][:], o_scr[i * 128:(i + 1) * 128, :], reads=o_views, writes=[ot[i % 2]])
                k.dma(sp, xs2[i % 2][:], x_d[i * 128:(i + 1) * 128, :], reads=[x_d], writes=[xs2[i % 2]])

            oproj_load(0)
            for i in range(NT):
                o_t = ot[i % 2]; xs = xs2[i % 2]
                if i + 1 < NT:
                    oproj_load(i + 1)
                for c4 in range(4):
                    for cc in range(4):
                        c = c4 * 4 + cc
                        transpose(PT, PT[:, cc * 128:(cc + 1) * 128], o_t, o_t[:, c * 128:(c + 1) * 128], ident_bf)
                    for cc in range(4):
                        c = c4 * 4 + cc
                        ts(oT, oT[:, c, :], PT, PT[:, cc * 128:(cc + 1) * 128], onw_col[:, c:c + 1], ALU.mult, sreads=[onw_col])
                for dmb in range(4):
                    pn = PB[(2 * dmb) % 4]; pf = PB[(2 * dmb + 1) % 4]
                    for c in range(8):
                        mm(pn, pn[:], oT, oT[:, c, :], WOv[c], WO[:, c, dmb * 512:(dmb + 1) * 512], c == 0, c == 7)
                    for c in range(8, 16):
                        mm(pf, pf[:], oT, oT[:, c, :], WOv[c], WO[:, c, dmb * 512:(dmb + 1) * 512], c == 8, c == 15)
                    sl = slice(dmb * 512, (dmb + 1) * 512)
                    stt(h1t, h1t[:, sl], pn, pn[:], rs_n[:, i:i + 1], xs, xs[:, sl], ALU.mult, ALU.add, sreads=[rs_n])
                    stt(h1t, h1t[:, sl], pf, pf[:], rs_f[:, i:i + 1], h1t, h1t[:, sl], ALU.mult, ALU.add, sreads=[rs_f])
                k.dma(sp, h1_scr[i * 128:(i + 1) * 128, :], h1t[:], reads=[h1t], writes=[h1_scr])
                k.op(act, lambda: S.activation(out=junk[:], in_=h1t[:], func=AF.Square, accum_out=sstmp[:, 0:1]), reads=[h1t], writes=[junk, sstmp])
                rstd_from_ss(sstmp[:, 0:1], sstmp, rt[:, 0:1], rt, D)
                stt(hn32, hn32[:], h1t, h1t[:], rt[:, 0:1], fnw_bc, fnw_bc[:], ALU.mult, ALU.mult, sreads=[rt])
                k.op(act, lambda: S.copy(out=hnb[:], in_=hn32[:]), reads=[hn32], writes=[hnb])
                k.dma(sp, hn_scr[i * 128:(i + 1) * 128, :], hnb[:], reads=[hnb], writes=[hn_scr])
                for c4 in range(4):
                    pb = PB[4 + c4 % 2]
                    for cc in range(4):
                        c = c4 * 4 + cc
                        transpose(pb, pb[:, cc * 128:(cc + 1) * 128], hn32, hn32[:, c * 128:(c + 1) * 128], ident_f)
                    evac(act if c4 % 2 else dve, hnT[:, c4 * 4:(c4 + 1) * 4, :], pb[:].rearrange("p (c t) -> p c t", c=4), [pb], [hnT])
                pl = PB[6]
                for c in range(16):
                    mm(pl, pl[:, 0:36], hnT, hnT[:, c, :], WR, WR[:, c, :], c == 0, c == 15)
                tt(lg, lg[:], pl, pl[:, 0:36], RB, RB[:], ALU.add)
                k.op(dve, lambda: V.reduce_max(out=rt[:, 1:2], in_=lg[:, 0:4], axis=AX.X), reads=[lg], writes=[rt])
                ts(oh, oh[:, 0:4], lg, lg[:, 0:4], rt[:, 1:2], ALU.is_ge, sreads=[rt])
                ts(rt, rt[:, 2:3], rt, rt[:, 1:2], -1.0, ALU.mult)
                k.op(act, lambda: S.activation(out=rt[:, 8:12], in_=lg[:, 0:4], func=AF.Exp, bias=rt[:, 2:3], accum_out=rt[:, 3:4]),
                     reads=[lg, rt], writes=[rt])
                k.op(dve, lambda: V.reciprocal(out=rt[:, 4:5], in_=rt[:, 3:4]), reads=[rt], writes=[rt])
                ts(elg, elg[:], lg, lg[:, 4:12], oh[:, 0:1], ALU.mult, sreads=[oh])
                for gg in range(1, 4):
                    stt(elg, elg[:], lg, lg[:, 4 + gg * 8:12 + gg * 8], oh[:, gg:gg + 1], elg, elg[:], ALU.mult, ALU.add, sreads=[oh])
                k.op(dve, lambda: V.reduce_max(out=rt[:, 5:6], in_=elg[:], axis=AX.X), reads=[elg], writes=[rt])
                ts(oh, oh[:, 8:16], elg, elg[:], rt[:, 5:6], ALU.is_ge, sreads=[rt])
                stt(elg, elg[:], oh, oh[:, 8:16], -1.0e30, elg, elg[:], ALU.mult, ALU.add)
                k.op(dve, lambda: V.reduce_max(out=rt[:, 6:7], in_=elg[:], axis=AX.X), reads=[elg], writes=[rt])
                ts(oh, oh[:, 16:24], elg, elg[:], rt[:, 6:7], ALU.is_ge, sreads=[rt])
                tt(rt, rt[:, 7:8], rt, rt[:, 6:7], rt, rt[:, 5:6], ALU.subtract)
                k.op(act, lambda: S.activation(out=rt[:, 12:13], in_=rt[:, 7:8], func=AF.Exp), reads=[rt], writes=[rt])
                ts(rt, rt[:, 13:14], rt, rt[:, 12:13], 1.0, ALU.add)
                k.op(dve, lambda: V.reciprocal(out=rt[:, 14:15], in_=rt[:, 13:14]), reads=[rt], writes=[rt])
                tt(Wk, Wk[:, i, 0:1], rt, rt[:, 14:15], rt, rt[:, 4:5], ALU.mult)
                tt(rt, rt[:, 15:16], rt, rt[:, 14:15], rt, rt[:, 12:13], ALU.mult)
                tt(Wk, Wk[:, i, 1:2], rt, rt[:, 15:16], rt, rt[:, 4:5], ALU.mult)
                for gg in range(4):
                    ts(OH1a, OH1a[:, i, gg * 8:(gg + 1) * 8], oh, oh[:, 8:16], oh[:, gg:gg + 1], ALU.mult, sreads=[oh])
                    ts(OH2a, OH2a[:, i, gg * 8:(gg + 1) * 8], oh, oh[:, 16:24], oh[:, gg:gg + 1], ALU.mult, sreads=[oh])
                tt(SELb, SELb[:, i, :], OH1a, OH1a[:, i, :], OH2a, OH2a[:, i, :], ALU.add)

        if stop_after == "oproj":
            k.dma(sp, dbg["d_h1"][:], h1_scr[:], reads=[h1_scr], writes=[dbg["d_h1"]])
            return done([dbg["d_h1"]])

        IDXI = k.sb("IDXI", [128, NSLOT], I32)
        x_views = []
        with Scope(k) as eR:
            onesb = k.sb("onesb", [128, 128], BF16, es=eR)
            stri = k.sb("stri", [128, 128], BF16, es=eR)
            ncnt = k.sb("ncnt", [128, 32], F32, es=eR)
            tl = k.sb("tl", [128, 32], F32, es=eR)
            cA = k.sb("cA", [128, 32], F32, es=eR)
            cBb = k.sb("cBb", [128, 32], F32, es=eR)
            basef = k.sb("basef", [128, 32], F32, es=eR)
            rowf = k.sb("rowf", [128, 32], F32, es=eR)
            tmp32 = k.sb("tmp32", [128, 32], F32, es=eR)
            ROWF = k.sb("ROWF", [128, NT, 2], F32, es=eR)
            esl_f = k.sb("esl_f", [1, NSLOT + 1], F32, es=eR)
            nfl_f = k.sb("nfl_f", [1, NSLOT], F32, es=eR)
            k.op(dve, lambda: V.memset(onesb[:], 1.0), writes=[onesb])
            k.dma(sp, stri[:], CD["stri"][:], reads=[CD["stri"]], writes=[stri])
            pcnt = PB[0]
            for i in range(NT):
                mm(pcnt, pcnt[:, 0:32], onesb, onesb[:], SELb, SELb[:, i, :], i == 0, i == NT - 1)
            evac(dve, ncnt[:], pcnt[:, 0:32], [pcnt], [ncnt])
            k.op(dve, lambda: V.memset(tl[:], 0.0), writes=[tl])
            for j in range(16):
                stt(tl, tl[:], ncnt, ncnt[:], float(128 * j), tl, tl[:], ALU.is_gt, ALU.add)
            k.op(dve, lambda: V.tensor_copy(out=cA[:], in_=tl[:]), reads=[tl], writes=[cA])
            src, dst = cA, cBb
            for sft in [1, 2, 4, 8, 16]:
                k.op(dve, lambda: V.tensor_copy(out=dst[:, 0:sft], in_=src[:, 0:sft]), reads=[src], writes=[dst])
                tt(dst, dst[:, sft:32], src, src[:, sft:32], src, src[:, 0:32 - sft], ALU.add)
                src, dst = dst, src
            cum = src
            tt(basef, basef[:], cum, cum[:], tl, tl[:], ALU.subtract)
            ts(basef, basef[:], basef, basef[:], 128.0, ALU.mult)
            for i in range(NT):
                pp = PB[1 + i % 2]
                for j in range(i):
                    mm(pp, pp[:, 0:32], onesb, onesb[:], SELb, SELb[:, j, :], j == 0, False)
                mm(pp, pp[:, 0:32], stri, stri[:], SELb, SELb[:, i, :], i == 0, True)
                tt(rowf, rowf[:], pp, pp[:, 0:32], basef, basef[:], ALU.add)
                for kk, OH in enumerate([OH1a, OH2a]):
                    tt(tmp32, tmp32[:], rowf, rowf[:], OH, OH[:, i, :], ALU.mult)
                    k.op(dve, lambda: V.reduce_sum(out=ROWF[:, i, kk:kk + 1], in_=tmp32[:], axis=AX.X), reads=[tmp32], writes=[ROWF])
            k.op(dve, lambda: V.tensor_copy(out=ROWI[:], in_=ROWF[:]), reads=[ROWF], writes=[ROWI])
            k.op(dve, lambda: V.memset(esl_f[:], -1.0), writes=[esl_f])
            for s in range(NSLOT):
                k.op(dve, lambda: V.tensor_scalar(out=tmp32[0:1, :], in0=cum[0:1, :], scalar1=float(s), scalar2=None, op0=ALU.is_le,
                                                  op1=ALU.add, accum_out=esl_f[0:1, s + 1:s + 2]), reads=[cum], writes=[tmp32, esl_f])
            ts(esl_f, esl_f[0:1, 1:NSLOT + 1], esl_f, esl_f[0:1, 1:NSLOT + 1], 31.0, ALU.min)
            k.op(dve, lambda: V.memset(nfl_f[:], 1.0), writes=[nfl_f])
            tt(nfl_f, nfl_f[0:1, 2:NSLOT], esl_f, esl_f[0:1, 3:NSLOT + 1], esl_f, esl_f[0:1, 1:NSLOT - 1], ALU.not_equal)
            onesrow = k.sb("onesrow", [1, 128], F32, es=eR)
            iop = k.sb("iop", [128, 1], F32, es=eR)
            idxf = k.sb("idxf", [128, NSLOT], F32, es=eR)
            k.op(dve, lambda: V.memset(onesrow[:], 1.0), writes=[onesrow])
            k.dma(sp, iop[:], CD["iota_p"][:], reads=[CD["iota_p"]], writes=[iop])
            mm(PB[3], PB[3][:, 0:NSLOT], onesrow, onesrow[:], esl_f, esl_f[0:1, 1:NSLOT + 1], True, True)
            mm(PB[4], PB[4][:, 0:NSLOT], onesrow, onesrow[:], nfl_f, nfl_f[:], True, True)
            ts(idxf, idxf[:], PB[3], PB[3][:, 0:NSLOT], 128.0, ALU.mult, iop[:, 0:1], ALU.add, sreads=[iop])
            ts(idxf, idxf[:], idxf, idxf[:], -100000.0, ALU.add)
            tt(idxf, idxf[:], idxf, idxf[:], PB[4], PB[4][:, 0:NSLOT], ALU.mult)
            ts(idxf, idxf[:], idxf, idxf[:], 100000.0, ALU.add)
            k.op(dve, lambda: V.tensor_copy(out=IDXI[:], in_=idxf[:]), reads=[idxf], writes=[IDXI])
            hb = [k.sb(f"hb{i}", [128, D], BF16, es=eR) for i in range(2)]
            for i in range(NT):
                hbt = hb[i % 2]
                k.dma(sp, hbt[:], hn_scr[i * 128:(i + 1) * 128, :], reads=[hn_scr], writes=[hbt])
                for kk in range(2):
                    xv_ = k.view(xslot, "xs_st"); x_views.append(xv_)
                    k.dma(pool, None, None, reads=[hbt, ROWI], writes=[xv_],
                          fn=lambda: G.indirect_dma_start(out=xslot[:], out_offset=bass.IndirectOffsetOnAxis(ap=ROWI[:, i, kk:kk + 1], axis=0),
                                                          in_=hbt[:], in_offset=None))

        with Scope(k) as eM:
            WGs = [k.sb(f"WG{i}", [128, 16, 512], BF16, es=eM) for i in range(2)]
            WUs = [k.sb(f"WU{i}", [128, 16, 512], BF16, es=eM) for i in range(2)]
            WDs = [k.sb(f"WD{i}", [128, 4, D], BF16, es=eM) for i in range(2)]
            xgbs = [k.sb(f"xgb{i}", [128, D], BF16, es=eM) for i in range(2)]
            xgTs = [k.sb(f"xgT{i}", [128, 16, 128], BF16, es=eM) for i in range(2)]
            hTs = [k.sb(f"hT{i}", [128, 4, 128], BF16, es=eM) for i in range(2)]
            sgs = [k.sb(f"sg{i}", [128, 128], F32, es=eM) for i in range(2)]
            ybts = [k.sb(f"ybt{i}", [128, D], F32, es=eM) for i in range(2)]
            PTh = [k.view(PT, "PTa"), k.view(PT, "PTb")]
            precast_step(1000)
            bc_reg = G.to_reg(4095)

            def load_w_slot(s):
                for Wb, src in [(WGs[s % 2], wbf["g"]), (WUs[s % 2], wbf["u"]), (WDs[s % 2], wbf["d"])]:
                    src2d = src.t[:].rearrange("e (p c) f -> (e p) (c f)", p=128)
                    k.dma(pool, None, None, reads=pre_views + [IDXI], writes=[Wb],
                          fn=lambda: G.indirect_dma_start(out=Wb[:].rearrange("p c f -> p (c f)"), out_offset=None, in_=src2d,
                                                          in_offset=bass.IndirectOffsetOnAxis(ap=IDXI[:, s:s + 1], axis=0),
                                                          bounds_check=bc_reg, oob_is_err=False))

            def load_x_dma(s):
                xgb = xgbs[s % 2]
                k.dma(sp, xgb[:], xslot[s * 128:(s + 1) * 128, :], reads=x_views, writes=[xgb])

            def load_x_tr(s):
                xgb = xgbs[s % 2]; xgT = xgTs[s % 2]
                for c4 in range(4):
                    for cc in range(4):
                        c = c4 * 4 + cc
                        transpose(PT, PT[:, cc * 128:(cc + 1) * 128], xgb, xgb[:, c:D:16], ident_bf)
                    evac(act if c4 % 2 else dve, xgT[:, c4 * 4:(c4 + 1) * 4, :], PT[:, 0:512].rearrange("p (c t) -> p c t", c=4), [PT], [xgT])

            load_x_dma(0)
            load_x_dma(1)
            load_w_slot(0)
            load_x_tr(0)
            for s in range(NSLOT):
                xgT = xgTs[s % 2]; ybt = ybts[s % 2]; hT = hTs[s % 2]
                WG, WU, WD = WGs[s % 2], WUs[s % 2], WDs[s % 2]
                if s + 1 < NSLOT:
                    load_x_tr(s + 1)
                    if s + 2 < NSLOT:
                        load_x_dma(s + 2)
                    load_w_slot(s + 1)
                for c2 in range(4):
                    pg = PB[(2 * c2) % 4]; pu = PB[(2 * c2 + 1) % 4]; sg = sgs[c2 % 2]
                    for c in range(16):
                        mm(pg, pg[:, 0:128], WG, WG[:, c, c2:512:4], xgT, xgT[:, c, :], c == 0, c == 15)
                    for c in range(16):
                        mm(pu, pu[:, 0:128], WU, WU[:, c, c2:512:4], xgT, xgT[:, c, :], c == 0, c == 15)
                    k.op(act, lambda: S.activation(out=sg[:], in_=pg[:, 0:128], func=AF.Silu), reads=[pg], writes=[sg])
                    tt(hT, hT[:, c2, :], sg, sg[:], pu, pu[:, 0:128], ALU.mult)
                for dmb in range(4):
                    pd = PB[4 + dmb % 3]
                    for c2 in range(4):
                        mm(pd, pd[:], hT, hT[:, c2, :], WD, WD[:, c2, dmb * 512:(dmb + 1) * 512], c2 == 0, c2 == 3)
                    evac(act if dmb % 2 else dve, ybt[:, dmb * 512:(dmb + 1) * 512], pd[:], [pd], [ybt])
                k.dma(sp, yslot[s * 128:(s + 1) * 128, :], ybt[:], reads=[ybt], writes=[yslot])

        with Scope(k) as eZ:
            fw_bc = k.sb("fw_bc", [128, D], F32, es=eZ)
            k.dma(sp, fw_bc[:], bass.AP(P["final_norm_w"].t, 0, [[0, 128], [1, D]]), reads=[P["final_norm_w"]], writes=[fw_bc])
            h1b = [k.sb(f"h1b{i}", [128, D], F32, es=eZ) for i in range(2)]
            g0 = [k.sb(f"g0{i}", [128, D], F32, es=eZ) for i in range(2)]
            g1 = [k.sb(f"g1{i}", [128, D], F32, es=eZ) for i in range(2)]
            ob = [k.sb(f"ob{i}", [128, D], F32, es=eZ) for i in range(2)]
            rf = k.sb("rf", [128, 4], F32, es=eZ)
            def final_load(i):
                k.dma(sp, h1b[i % 2][:], h1_scr[i * 128:(i + 1) * 128, :], reads=[h1_scr], writes=[h1b[i % 2]])
                for kk, gb in enumerate([g0[i % 2], g1[i % 2]]):
                    k.dma(pool, None, None, reads=[yslot, ROWI], writes=[gb],
                          fn=lambda: G.indirect_dma_start(out=gb[:], out_offset=None, in_=yslot[:],
                                                          in_offset=bass.IndirectOffsetOnAxis(ap=ROWI[:, i, kk:kk + 1], axis=0)))

            final_load(0)
            for i in range(NT):
                hh = h1b[i % 2]; a0 = g0[i % 2]; a1 = g1[i % 2]; oo = ob[i % 2]
                if i + 1 < NT:
                    final_load(i + 1)
                stt(hh, hh[:], a0, a0[:], Wk[:, i, 0:1], hh, hh[:], ALU.mult, ALU.add, sreads=[Wk])
                stt(hh, hh[:], a1, a1[:], Wk[:, i, 1:2], hh, hh[:], ALU.mult, ALU.add, sreads=[Wk])
                k.op(act, lambda: S.activation(out=junk[:], in_=hh[:], func=AF.Square, accum_out=rf[:, 0:1]), reads=[hh], writes=[junk, rf])
                rstd_from_ss(rf[:, 0:1], rf, rf[:, 1:2], rf, D)
                stt(oo, oo[:], hh, hh[:], rf[:, 1:2], fw_bc, fw_bc[:], ALU.mult, ALU.mult, sreads=[rf])
                k.dma(sp, out_d[i * 128:(i + 1) * 128, :], oo[:], reads=[oo], writes=[out_d])
        return done([out_d])


_NC_CACHE = {}


def _in_maps(inputs, n_cores=8):
    consts = _consts()
    maps = []
    for c in range(n_cores):
        m = {"x": np.ascontiguousarray(inputs["x"][c])}
        for name, shape in PARAM_SPECS:
            m[name] = np.ascontiguousarray(np.asarray(inputs[name], dtype=np.float32).reshape(shape))
        for name, shape, dt in CONST_SPECS:
            m["c_" + name] = consts[name]
        maps.append(m)
    return maps


def kernel(**inputs):
    if "nc" not in _NC_CACHE:
        _NC_CACHE["nc"] = build_nc()
    nc = _NC_CACHE["nc"]
    res = run_bass_kernel_spmd(nc, _in_maps(inputs), core_ids=list(range(8)))
    return np.stack([np.asarray(r["out"], dtype=np.float32) for r in res.results], axis=0)
```
